# Optimizing a Trainium2 kernel written in Bass

```python
import jax
import jax.numpy as jnp
from jax import lax
import numpy as np

D_MODEL = 1024
BATCH = 4
SEQ = 8192
DEPTH = 2

MEM_TOKENS = 256
M_HEADS = 4
M_WIDTH = D_MODEL // 2
M_DV = M_WIDTH // M_HEADS
M_DQK = M_DV // 2
M_QK_WIDTH = M_HEADS * M_DQK
M_CONV = 4
M_CHUNK = 64
F_HEADS = 8
F_WIDTH = D_MODEL // 2
F_DH = F_WIDTH // F_HEADS
F_BLOCK = 128
R_HEADS = 8
R_WIDTH = D_MODEL // 2
R_DH = R_WIDTH // R_HEADS
R_LORA_W = 64
R_LORA_A = 64
R_LORA_V = 32
R_LORA_G = 128
R_GN_EPS = 64e-5
N_BRANCH = 3
X_HEADS = 4
X_DH = D_MODEL // X_HEADS
D_FF_DENSE = 128 * ((8 * D_MODEL // 3 + 127) // 128)
N_EXPERTS = 8
TOP_K = 2
D_FF_EXPERT = 7 * D_MODEL // 2
DEEPNORM_ALPHA = (2.0 * DEPTH) ** 0.25
DEEPNORM_BETA = (8.0 * DEPTH) ** -0.25
LN_EPS = 1e-5
HEAD_NORM_EPS = 1e-6

kernel_name = 'hybrid_mlstm_fox_rwkv7_moe_deepnorm'


def _in_layout(with_vres):
    cols = [('m_qk', 2 * M_QK_WIDTH), ('m_v', M_WIDTH), ('m_o', M_WIDTH), ('m_i', M_HEADS), ('m_f', M_HEADS),
            ('f_q', F_WIDTH), ('f_k', F_WIDTH), ('f_v', F_WIDTH), ('f_f', F_HEADS),
            ('gate', N_BRANCH * D_MODEL),
            ('r_r', R_WIDTH), ('r_k', R_WIDTH), ('r_v', R_WIDTH), ('r_w', R_LORA_W), ('r_a', R_LORA_A), ('r_g', R_LORA_G)]
    if with_vres:
        cols.append(('r_vres', R_LORA_V))
    layout, start = {}, 0
    for name, width in cols:
        layout[name] = (start, start + width)
        start += width
    return layout, start


def _layer_norm(x, g, b):
    x32 = x.astype(jnp.float32)
    mu = x32.mean(-1, keepdims=True)
    var = jnp.square(x32 - mu).mean(-1, keepdims=True)
    return ((x32 - mu) * lax.rsqrt(var + LN_EPS)).astype(x.dtype) * g + b


def _post_norm(x, sub, g, b):
    return _layer_norm(DEEPNORM_ALPHA * x + sub, g, b)


def _head_standardise(h, eps):
    h32 = h.astype(jnp.float32)
    mu = h32.mean(-1, keepdims=True)
    var = jnp.square(h32 - mu).mean(-1, keepdims=True)
    return ((h32 - mu) * lax.rsqrt(var + eps)).reshape(h.shape[:2] + (-1,))


def _head_l2_normalise(u):
    B, S, C = u.shape
    uh = u.astype(jnp.float32).reshape(B, S, R_HEADS, R_DH)
    uh = uh / jnp.maximum(jnp.sqrt(jnp.sum(uh * uh, -1, keepdims=True)), 1e-12)
    return uh.reshape(B, S, C).astype(u.dtype)


def _shift(u):
    return jnp.pad(u[:, :-1], ((0, 0), (1, 0), (0, 0)))


def _causal_conv(u, w):
    K, C = w.shape
    return lax.conv_general_dilated(u, w[:, None, :].astype(u.dtype), window_strides=(1,), padding=((K - 1, 0),),
                                    dimension_numbers=('NWC', 'WIO', 'NWC'), feature_group_count=C)


def _mlstm(q, k, v, i_pre, f_pre):
    B, S, H, DK = q.shape
    L = M_CHUNK
    NC = S // L
    f32 = jnp.float32

    def chunks(a):
        a = a.astype(f32).reshape((B, NC, L, H) + a.shape[3:])
        return jnp.moveaxis(a, (1, 3), (0, 2))

    xs = (chunks(q), chunks(k) * DK ** -0.5, chunks(v), chunks(i_pre), chunks(jax.nn.log_sigmoid(f_pre.astype(f32))))
    tri = jnp.tril(jnp.ones((L, L), dtype=bool))

    def step(carry, xs_c):
        C, n, m = carry
        qc, kc, vc, ic, lfc = xs_c
        b = jnp.cumsum(lfc, axis=-1)
        d_intra = jnp.where(tri, b[..., :, None] - b[..., None, :] + ic[..., None, :], -jnp.inf)
        d_inter = b + m[..., None]
        m_t = jnp.maximum(d_inter, d_intra.max(-1))
        w_intra = jnp.einsum('bhtd,bhsd->bhts', qc, kc) * jnp.exp(d_intra - m_t[..., None])
        w_inter = jnp.exp(d_inter - m_t)
        num = w_inter[..., None] * jnp.einsum('bhtd,bhde->bhte', qc, C) + jnp.einsum('bhts,bhse->bhte', w_intra, vc)
        den = w_inter * jnp.einsum('bhtd,bhd->bht', qc, n) + w_intra.sum(-1)
        h = num / jnp.maximum(jnp.abs(den), jnp.exp(-m_t))[..., None]
        g = b[..., -1:] - b + ic
        m_new = jnp.maximum(b[..., -1] + m, g.max(-1))
        decay = jnp.exp(b[..., -1] + m - m_new)
        ws = jnp.exp(g - m_new[..., None])
        C = decay[..., None, None] * C + jnp.einsum('bhs,bhsd,bhse->bhde', ws, kc, vc)
        n = decay[..., None] * n + jnp.einsum('bhs,bhsd->bhd', ws, kc)
        return (C, n, m_new), h

    init = (jnp.zeros((B, H, DK, v.shape[-1]), f32), jnp.zeros((B, H, DK), f32), jnp.zeros((B, H), f32))
    _, h = lax.scan(step, init, xs)
    return jnp.moveaxis(h, (0, 2), (1, 3)).reshape(B, S, H, -1)


def _forgetting_attention(q, k, v, f_pre):
    B, S, H, DH = q.shape
    NB = S // F_BLOCK
    cum = jnp.cumsum(jax.nn.log_sigmoid(f_pre.astype(jnp.float32)), axis=1).transpose(0, 2, 1)
    qh = q.transpose(0, 2, 1, 3) * DH ** -0.5
    kh = k.transpose(0, 2, 1, 3)
    vh = v.transpose(0, 2, 1, 3)
    q_blocks = qh.reshape(B, H, NB, F_BLOCK, DH).transpose(2, 0, 1, 3, 4)
    cq_blocks = cum.reshape(B, H, NB, F_BLOCK).transpose(2, 0, 1, 3)
    starts = jnp.arange(NB, dtype=jnp.int32) * F_BLOCK
    k_pos = jnp.arange(S, dtype=jnp.int32)

    def block(args):
        qb, cqb, start = args
        logits = jnp.einsum('bhtd,bhsd->bhts', qb, kh).astype(jnp.float32) + cqb[..., None] - cum[:, :, None, :]
        q_pos = start + jnp.arange(F_BLOCK, dtype=jnp.int32)
        logits = jnp.where(k_pos[None, :] <= q_pos[:, None], logits, -jnp.inf)
        probs = jax.nn.softmax(logits, axis=-1).astype(vh.dtype)
        return jnp.einsum('bhts,bhsd->bhtd', probs, vh)

    o = lax.map(block, (q_blocks, cq_blocks, starts))
    return o.transpose(1, 0, 3, 2, 4).reshape(B, S, H * DH)


def _rwkv7_scan(r, decay, k, v, kk, kk_a):
    B, S, _ = r.shape

    def heads(t):
        return t.astype(jnp.float32).reshape(B, S, R_HEADS, R_DH).transpose(1, 0, 2, 3)

    xs = (heads(r), heads(decay), heads(k), heads(v), heads(kk), heads(kk_a))

    def step(state, xs_t):
        r_t, w_t, k_t, v_t, kk_t, b_t = xs_t
        sk = jnp.einsum('bhvk,bhk->bhv', state, kk_t)
        state = state * w_t[:, :, None, :] - sk[..., None] * b_t[:, :, None, :] + v_t[..., None] * k_t[:, :, None, :]
        return state, jnp.einsum('bhvk,bhk->bhv', state, r_t)

    _, y = lax.scan(step, jnp.zeros((B, R_HEADS, R_DH, R_DH), jnp.float32), xs)
    return y.transpose(1, 0, 2, 3)


def _token_mixer(x, w_in, b_in, m_conv, m_norm, m_up, f_up, r_mu, r_wbias, r_wB, r_abias, r_aB, r_gB,
                 r_kk, r_ka, r_rk, r_ln_g, r_ln_b, r_up, w_out, vres):
    B, S, _ = x.shape
    layout, _ = _in_layout(vres is not None)
    p = x @ w_in + b_in

    def col(name):
        s, e = layout[name]
        return p[..., s:e]

    qk = jax.nn.silu(_causal_conv(col('m_qk'), m_conv))
    m_q = qk[..., :M_QK_WIDTH].reshape(B, S, M_HEADS, M_DQK)
    m_k = qk[..., M_QK_WIDTH:].reshape(B, S, M_HEADS, M_DQK)
    m_v = col('m_v').reshape(B, S, M_HEADS, M_DV)
    h_a = _mlstm(m_q, m_k, m_v, col('m_i'), col('m_f'))
    h_a = _head_standardise(h_a, HEAD_NORM_EPS).astype(x.dtype) * m_norm * jax.nn.sigmoid(col('m_o'))

    h_b = _forgetting_attention(col('f_q').reshape(B, S, F_HEADS, F_DH), col('f_k').reshape(B, S, F_HEADS, F_DH),
                                col('f_v').reshape(B, S, F_HEADS, F_DH), col('f_f'))

    r0 = layout['r_r'][0]
    pr = p[..., r0:]
    pr = pr + r_mu * (_shift(pr) - pr)

    def rcol(name):
        s, e = layout[name]
        return pr[..., s - r0:e - r0]

    r, k, v = rcol('r_r'), rcol('r_k'), rcol('r_v')
    w_log = -jax.nn.softplus(-(r_wbias + jnp.tanh(rcol('r_w')) @ r_wB)) - 0.5
    decay = jnp.exp(-jnp.exp(w_log.astype(jnp.float32)))
    a = jax.nn.sigmoid(r_abias + rcol('r_a') @ r_aB)
    v_own = v
    if vres is not None:
        v_first, r_vbias, r_vB = vres
        v = v + (v_first - v) * jax.nn.sigmoid(r_vbias + rcol('r_vres') @ r_vB)
    g = jax.nn.sigmoid(rcol('r_g')) @ r_gB
    kk = _head_l2_normalise(k * r_kk)
    k = k * (1.0 + (a - 1.0) * r_ka)
    y = _rwkv7_scan(r, decay, k, v, kk, kk * a)
    y = _head_standardise(y, R_GN_EPS).astype(x.dtype) * r_ln_g + r_ln_b
    bonus = jnp.sum((r * k * r_rk).reshape(B, S, R_HEADS, R_DH), axis=-1, keepdims=True)
    y = (y + (bonus * v.reshape(B, S, R_HEADS, R_DH)).reshape(B, S, R_WIDTH)) * g

    gates = jax.nn.sigmoid(col('gate')).reshape(B, S, N_BRANCH, D_MODEL)
    merged = gates[..., 0, :] * (h_a @ m_up) + gates[..., 1, :] * (h_b @ f_up) + gates[..., 2, :] * (y @ r_up)
    return merged @ w_out, v_own


def _cross_attention(x, mem_n, wq, wkv, wo):
    B, S, _ = x.shape
    M = mem_n.shape[1]
    q = (x @ wq).reshape(B, S, X_HEADS, X_DH)
    kv = (mem_n @ wkv).reshape(B, M, 2, X_HEADS, X_DH)
    logits = jnp.einsum('bthd,bmhd->bhtm', q, kv[:, :, 0]).astype(jnp.float32) * X_DH ** -0.5
    probs = jax.nn.softmax(logits, axis=-1).astype(x.dtype)
    o = jnp.einsum('bhtm,bmhd->bthd', probs, kv[:, :, 1]).reshape(B, S, D_MODEL)
    return o @ wo


def _swiglu(x, wgu, wd):
    gate, up = jnp.split(x @ wgu, 2, axis=-1)
    return (jax.nn.silu(gate) * up) @ wd


def _moe(x, router_w, router_b, ex_wgu, ex_wd):
    logits = (x @ router_w).astype(jnp.float32) + router_b
    top_v, top_i = lax.top_k(logits, TOP_K)
    top_w = jax.nn.softmax(top_v, axis=-1)
    comb = jnp.sum(jax.nn.one_hot(top_i, N_EXPERTS, dtype=jnp.float32) * top_w[..., None], axis=-2).astype(x.dtype)
    y = jnp.zeros_like(x)
    for e in range(N_EXPERTS):
        y = y + comb[..., e:e + 1] * _swiglu(x, ex_wgu[e], ex_wd[e])
    return y


def setup_inputs(seed: int = 0) -> dict:
    key = jax.random.key(seed)
    keys = iter(jax.random.split(key, 128))

    def nrm(shape, scale):
        return scale * jax.random.normal(next(keys), shape, jnp.float32)

    def unif(shape, lo, hi):
        return jax.random.uniform(next(keys), shape, jnp.float32, lo, hi)

    def gain(n):
        return 1.0 + nrm((n,), 0.02)

    inp = {}
    inp['x'] = nrm((BATCH, SEQ, D_MODEL), 1.0)
    inp['mem'] = nrm((BATCH, MEM_TOKENS, D_MODEL), 1.0)
    inp['mem_ln_g'] = gain(D_MODEL)
    inp['mem_ln_b'] = nrm((D_MODEL,), 0.02)
    for l in range(DEPTH):
        s = f'_{l}'
        layout, n_cols = _in_layout(l > 0)
        inp['w_in' + s] = nrm((D_MODEL, n_cols), D_MODEL ** -0.5)
        b_in = nrm((n_cols,), 0.02)
        b_in = b_in.at[layout['m_f'][0]:layout['m_f'][1]].add(jnp.linspace(3.0, 6.0, M_HEADS))
        b_in = b_in.at[layout['f_f'][0]:layout['f_f'][1]].add(jnp.linspace(1.0, 6.0, F_HEADS))
        inp['b_in' + s] = b_in
        inp['m_conv' + s] = nrm((M_CONV, 2 * M_QK_WIDTH), M_CONV ** -0.5)
        inp['m_norm' + s] = gain(M_WIDTH)
        inp['m_up' + s] = nrm((M_WIDTH, D_MODEL), M_WIDTH ** -0.5)
        inp['f_up' + s] = nrm((F_WIDTH, D_MODEL), F_WIDTH ** -0.5)
        inp['r_mu' + s] = unif((n_cols - layout['r_r'][0],), 0.0, 1.0)
        inp['r_wbias' + s] = unif((R_WIDTH,), -5.0, -1.0)
        inp['r_wB' + s] = nrm((R_LORA_W, R_WIDTH), R_LORA_W ** -0.5)
        inp['r_abias' + s] = nrm((R_WIDTH,), 0.1)
        inp['r_aB' + s] = nrm((R_LORA_A, R_WIDTH), R_LORA_A ** -0.5)
        if l > 0:
            inp['r_vbias' + s] = nrm((R_WIDTH,), 0.1)
            inp['r_vB' + s] = nrm((R_LORA_V, R_WIDTH), R_LORA_V ** -0.5)
        inp['r_gB' + s] = nrm((R_LORA_G, R_WIDTH), R_LORA_G ** -0.5)
        inp['r_kk' + s] = 0.85 + nrm((R_WIDTH,), 0.02)
        inp['r_ka' + s] = 1.0 + nrm((R_WIDTH,), 0.02)
        inp['r_rk' + s] = nrm((R_WIDTH,), 0.1)
        inp['r_ln_g' + s] = gain(R_WIDTH)
        inp['r_ln_b' + s] = nrm((R_WIDTH,), 0.02)
        inp['r_up' + s] = nrm((R_WIDTH, D_MODEL), R_WIDTH ** -0.5)
        inp['w_out' + s] = nrm((D_MODEL, D_MODEL), DEEPNORM_BETA * D_MODEL ** -0.5)
        inp['ln1_g' + s] = gain(D_MODEL)
        inp['ln1_b' + s] = nrm((D_MODEL,), 0.02)
        inp['x_wq' + s] = nrm((D_MODEL, D_MODEL), D_MODEL ** -0.5)
        inp['x_wkv' + s] = nrm((D_MODEL, 2 * D_MODEL), D_MODEL ** -0.5)
        inp['x_wo' + s] = nrm((D_MODEL, D_MODEL), DEEPNORM_BETA * D_MODEL ** -0.5)
        inp['ln2_g' + s] = gain(D_MODEL)
        inp['ln2_b' + s] = nrm((D_MODEL,), 0.02)
        if l % 2 == 0:
            inp['ff_wgu' + s] = nrm((D_MODEL, 2 * D_FF_DENSE), D_MODEL ** -0.5)
            inp['ff_wd' + s] = nrm((D_FF_DENSE, D_MODEL), DEEPNORM_BETA * D_FF_DENSE ** -0.5)
        else:
            inp['ex_router' + s] = nrm((D_MODEL, N_EXPERTS), D_MODEL ** -0.5)
            inp['ex_router_b' + s] = nrm((N_EXPERTS,), 0.01)
            inp['ex_wgu' + s] = nrm((N_EXPERTS, D_MODEL, 2 * D_FF_EXPERT), D_MODEL ** -0.5)
            inp['ex_wd' + s] = nrm((N_EXPERTS, D_FF_EXPERT, D_MODEL), DEEPNORM_BETA * D_FF_EXPERT ** -0.5)
        inp['ln3_g' + s] = gain(D_MODEL)
        inp['ln3_b' + s] = nrm((D_MODEL,), 0.02)
    return inp


def reference(x, mem, mem_ln_g, mem_ln_b,
              w_in_0, b_in_0, m_conv_0, m_norm_0, m_up_0, f_up_0, r_mu_0, r_wbias_0, r_wB_0, r_abias_0, r_aB_0,
              r_gB_0, r_kk_0, r_ka_0, r_rk_0, r_ln_g_0, r_ln_b_0, r_up_0, w_out_0, ln1_g_0, ln1_b_0,
              x_wq_0, x_wkv_0, x_wo_0, ln2_g_0, ln2_b_0, ff_wgu_0, ff_wd_0, ln3_g_0, ln3_b_0,
              w_in_1, b_in_1, m_conv_1, m_norm_1, m_up_1, f_up_1, r_mu_1, r_wbias_1, r_wB_1, r_abias_1, r_aB_1,
              r_vbias_1, r_vB_1,
              r_gB_1, r_kk_1, r_ka_1, r_rk_1, r_ln_g_1, r_ln_b_1, r_up_1, w_out_1, ln1_g_1, ln1_b_1,
              x_wq_1, x_wkv_1, x_wo_1, ln2_g_1, ln2_b_1, ex_router_1, ex_router_b_1, ex_wgu_1, ex_wd_1,
              ln3_g_1, ln3_b_1):
    mem_n = _layer_norm(mem, mem_ln_g, mem_ln_b)
    mixer_params = (
        (w_in_0, b_in_0, m_conv_0, m_norm_0, m_up_0, f_up_0, r_mu_0, r_wbias_0, r_wB_0, r_abias_0, r_aB_0,
         r_gB_0, r_kk_0, r_ka_0, r_rk_0, r_ln_g_0, r_ln_b_0, r_up_0, w_out_0),
        (w_in_1, b_in_1, m_conv_1, m_norm_1, m_up_1, f_up_1, r_mu_1, r_wbias_1, r_wB_1, r_abias_1, r_aB_1,
         r_gB_1, r_kk_1, r_ka_1, r_rk_1, r_ln_g_1, r_ln_b_1, r_up_1, w_out_1),
    )
    vres_params = (None, (r_vbias_1, r_vB_1))
    ln1 = ((ln1_g_0, ln1_b_0), (ln1_g_1, ln1_b_1))
    xattn = ((x_wq_0, x_wkv_0, x_wo_0), (x_wq_1, x_wkv_1, x_wo_1))
    ln2 = ((ln2_g_0, ln2_b_0), (ln2_g_1, ln2_b_1))
    ffn = ((ff_wgu_0, ff_wd_0), (ex_router_1, ex_router_b_1, ex_wgu_1, ex_wd_1))
    ln3 = ((ln3_g_0, ln3_b_0), (ln3_g_1, ln3_b_1))

    v_first = None
    for l in range(DEPTH):
        vres = None if l == 0 else (v_first, *vres_params[l])
        mix, v_own = _token_mixer(x, *mixer_params[l], vres)
        if l == 0:
            v_first = v_own
        x = _post_norm(x, mix, *ln1[l])
        x = _post_norm(x, _cross_attention(x, mem_n, *xattn[l]), *ln2[l])
        f = _swiglu(x, *ffn[l]) if l % 2 == 0 else _moe(x, *ffn[l])
        x = _post_norm(x, f, *ln3[l])
    return x
```

```python
import numpy as np
import concourse.bass as bass
import concourse.mybir as mybir
from concourse.bass_utils import run_bass_kernel_spmd

F32 = mybir.dt.float32
BF16 = mybir.dt.bfloat16
AF = mybir.ActivationFunctionType
ALU = mybir.AluOpType
AX = mybir.AxisListType

D = 1024
NCORES = 8
ALPHA = (2.0 * 2) ** 0.25
LN_EPS = 1e-5


class Buf:
    __slots__ = ("name", "w", "r", "psum")

    def __init__(self, name="", psum=False):
        self.name = name
        self.psum = psum
        self.w = None
        self.r = {}


class Ctx:
    ENGS = ("pe", "act", "dve", "pool", "sp")
    NDMASEM = 24

    def __init__(self, nc):
        self.nc = nc
        self.q = {e: [] for e in self.ENGS}
        self.cnt = {e: 0 for e in self.ENGS}
        self.seen = {e: {} for e in self.ENGS}
        self.sems = {}
        self.dma_uses = [0] * self.NDMASEM
        self.dma_rr = 0
        self.dma_rr_sw = 0
        self._stack = []
        self.ninst = 0

    def enter(self, cm):
        v = cm.__enter__()
        self._stack.append(cm)
        return v

    def close(self):
        while self._stack:
            self._stack.pop().__exit__(None, None, None)

    def alloc_sems(self):
        for e in self.ENGS:
            self.sems[e] = self.enter(self.nc.semaphore("s_" + e))
        for i in range(self.NDMASEM):
            self.sems[("dma", i)] = self.enter(self.nc.semaphore("s_dma%d" % i))

    def sbuf(self, name, shape, dt):
        return self.enter(self.nc.sbuf_tensor(name, list(shape), dt))

    def psum(self, name, shape, dt=F32):
        return self.enter(self.nc.psum_tensor(name, list(shape), dt))

    def _need(self, eng, tok, waits):
        if tok is None:
            return
        key, val = tok
        if key == "pe" and eng == "pe":
            return
        if self.seen[eng].get(key, 0) >= val:
            return
        waits[key] = max(waits.get(key, 0), val)

    def _deps(self, eng, reads, writes):
        waits = {}
        for b in reads:
            self._need(eng, b.w, waits)
            if b.psum:
                for key, val in b.r.items():
                    if key != eng:
                        self._need(eng, (key, val), waits)
        for b in writes:
            self._need(eng, b.w, waits)
            for key, val in b.r.items():
                if key == eng:
                    continue
                self._need(eng, (key, val), waits)
        for key, val in waits.items():
            self.seen[eng][key] = val
        return list(waits.items())

    def op(self, eng, fn, reads=(), writes=()):
        waits = self._deps(eng, reads, writes)
        self.cnt[eng] += 1
        tok = (eng, self.cnt[eng])
        for b in reads:
            if b.r.get(eng, 0) < tok[1]:
                b.r[eng] = tok[1]
        for b in writes:
            b.w = tok
            b.r = {}
        self.q[eng].append((waits, fn, (eng, 1)))
        self.ninst += 1
        return tok

    def dma(self, eng, out, in_, reads=(), writes=()):
        half = self.NDMASEM // 2
        if eng == "pool":
            s = half + self.dma_rr_sw
            self.dma_rr_sw = (self.dma_rr_sw + 1) % half
        else:
            s = self.dma_rr
            self.dma_rr = (self.dma_rr + 1) % half
        key = ("dma", s)
        waits = dict(self._deps(eng, reads, writes))
        prev = self.dma_uses[s] * 16
        if prev and self.seen[eng].get(key, 0) < prev:
            waits[key] = prev
            self.seen[eng][key] = prev
        self.dma_uses[s] += 1
        tok = (key, self.dma_uses[s] * 16)
        for b in reads:
            if b.r.get(key, 0) < tok[1]:
                b.r[key] = tok[1]
        for b in writes:
            b.w = tok
            b.r = {}
        self.q[eng].append((list(waits.items()), lambda e, o=out, i=in_: e.dma_start(out=o, in_=i), (key, 16)))
        self.ninst += 1
        return tok

    def wait_all_dma(self, eng):
        waits = []
        for s in range(self.NDMASEM):
            if self.dma_uses[s]:
                waits.append((("dma", s), self.dma_uses[s] * 16))
        self.q[eng].append((waits, None, None))

    def emit(self):
        nc = self.nc
        with nc.Block() as block:
            def run(e, name):
                for waits, fn, inc in self.q[name]:
                    for key, val in waits:
                        e.wait_ge(self.sems[key], val)
                    if fn is None:
                        continue
                    ins = fn(e)
                    ins.then_inc(self.sems[inc[0]], inc[1])

            @block.tensor
            def _(e):
                run(e, "pe")

            @block.scalar
            def _(e):
                run(e, "act")

            @block.vector
            def _(e):
                run(e, "dve")

            @block.gpsimd
            def _(e):
                run(e, "pool")

            @block.sync
            def _(e):
                run(e, "sp")


class Ring:
    def __init__(self, ctx, name, shape, dt, n, psum=False):
        self.tiles = []
        for i in range(n):
            t = ctx.psum("%s%d" % (name, i), shape, dt) if psum else ctx.sbuf("%s%d" % (name, i), shape, dt)
            self.tiles.append((t, Buf("%s%d" % (name, i), psum=psum)))
        self.i = 0

    def next(self):
        t = self.tiles[self.i]
        self.i = (self.i + 1) % len(self.tiles)
        return t


class WRing:
    SLOT = 4096

    def __init__(self, ctx, n, name="wr", dt=BF16, slot=None):
        self.ctx = ctx
        self.slot = slot or self.SLOT
        self.ring = Ring(ctx, name, [128, self.slot], dt, n)
        self.qi = 0

    def load(self, pieces, kc, ncols):
        t, b = self.ring.next()
        v = t[:, : kc * ncols].rearrange("p (k n) -> p k n", k=kc)
        for ap, off in pieces:
            w = ap.shape[1]
            src = ap.rearrange("(k p) n -> p k n", p=128)
            self.ctx.dma("pool", v[:, :, off:off + w], src, writes=[b])
        return v, b


def mm(ctx, ps, pb, lhsT, rhs, start, stop, reads):
    ctx.op("pe", lambda e: e.matmul(ps, lhsT, rhs, start=start, stop=stop), reads=reads, writes=[pb])


def layer_norm(ctx, z, zb, out, ob, gbc, bbc, pbuf, small, idx, F=1024):
    st, stb = small.next()
    nch = F // 512
    for c in range(nch):
        ctx.op("dve", lambda e, c=c: e.bn_stats(st[:, c * 6:(c + 1) * 6], z[:, c * 512:(c + 1) * 512]),
               reads=[zb], writes=[stb])
    ctx.op("dve", lambda e: e.bn_aggr(st[:, 16:18], st[:, 0:6 * nch]), reads=[stb], writes=[stb])
    A_(ctx, st[:, 18:19], st[:, 17:18], AF.Sqrt, [stb], [stb], bias=LN_EPS)
    ctx.op("dve", lambda e: e.reciprocal(st[:, 18:19], st[:, 18:19]), reads=[stb], writes=[stb])
    ctx.op("dve", lambda e: e.tensor_scalar(z, z, st[:, 16:17], st[:, 18:19], ALU.subtract, ALU.mult),
           reads=[zb, stb], writes=[zb])
    ctx.op("pool", lambda e: e.tensor_tensor(z, z, gbc, ALU.mult), reads=[zb, pbuf], writes=[zb])
    ctx.op("pool", lambda e: e.tensor_tensor(out, z, bbc, ALU.add), reads=[zb, pbuf], writes=[ob])


def transpose_to(ctx, psring, src_bf, srcb, dstT, dstb, ident_bf, cb, tok0, nk=8, evac=("act", "dve")):
    for h in range(nk // 4):
        ps, pb = psring.next()
        for q in range(4):
            k = h * 4 + q
            mm(ctx, ps[:, q * 128:(q + 1) * 128], pb, src_bf[:, k * 128:(k + 1) * 128], ident_bf, True, True,
               [srcb, cb])
        eng = evac[h % len(evac)]
        dst = dstT[:, h * 4:(h + 1) * 4, tok0:tok0 + 128]
        src = ps[:, :].rearrange("p (k t) -> p k t", k=4)
        if eng == "act":
            ctx.op("act", lambda e, d=dst, s=src: e.copy(d, s), reads=[pb], writes=[dstb])
        else:
            ctx.op("dve", lambda e, d=dst, s=src: e.tensor_copy(d, s), reads=[pb], writes=[dstb])


def A_(ctx, out, in_, func, reads, writes, bias=None, scale=None):
    kw = {}
    if bias is not None:
        kw["bias"] = bias
    if scale is not None:
        kw["scale"] = scale
    ctx.op("act", lambda e: e.activation(out, in_, func, **kw), reads=reads, writes=writes)


def TT(ctx, eng, out, in0, in1, op, reads, writes):
    ctx.op(eng, lambda e: e.tensor_tensor(out, in0, in1, op), reads=reads, writes=writes)


def TS(ctx, eng, out, in0, s1, s2, op0, op1, reads, writes):
    if s2 is None:
        ctx.op(eng, lambda e: e.tensor_scalar(out, in0, s1, None, op0), reads=reads, writes=writes)
    else:
        ctx.op(eng, lambda e: e.tensor_scalar(out, in0, s1, s2, op0, op1), reads=reads, writes=writes)


def STT(ctx, eng, out, in0, scalar, in1, op0, op1, reads, writes):
    ctx.op(eng, lambda e: e.scalar_tensor_tensor(out, in0, scalar, in1, op0, op1), reads=reads, writes=writes)


def CP(ctx, eng, out, in_, reads, writes):
    if eng == "act":
        ctx.op("act", lambda e: e.copy(out, in_), reads=reads, writes=writes)
    else:
        ctx.op(eng, lambda e: e.tensor_copy(out, in_), reads=reads, writes=writes)


def build_l2(moe, NT=4096, G=512, FF=None, NE=8):
    FF = FF or (3584 if moe else 2816)
    FC = FF // 128
    NG = NT // G
    nc = bass.Bass("TRN2", target_bir_lowering=False)
    ctx = Ctx(nc)

    def din(name, shape):
        return nc.dram_tensor(name, list(shape), F32, kind="ExternalInput").ap()

    x_tok = din("x_tok", [NT, D])
    xT_d = din("xT", [D, NT])
    hT_d = din("hT", [1536, NT])
    mem_d = din("mem", [256, D])
    cst_d = din("cst", [128, 256])
    vec_d = din("vecs", [8, D])
    wg_d = din("wg", [D, 3072])
    bg_d = din("bg", [1, 3072])
    wup_d = din("wup", [1536, D])
    wout_d = din("wout", [D, D])
    wq_d = din("wq", [D, D])
    wkv_d = din("wkv", [D, 2 * D])
    wo_d = din("wo", [D, D])
    if moe:
        rw_d = din("rw", [D, NE])
        rb_d = din("rb", [1, NE])
        wgu_d = din("wgu", [NE, D, 2 * FF])
        wd_d = din("wd", [NE, FF, D])
    else:
        wgu_d = din("wgu", [D, 2 * FF])
        wd_d = din("wd", [FF, D])
    out_d = nc.dram_tensor("x3", [NT, D], F32, kind="ExternalOutput").ap()

    ctx.alloc_sems()
    S = ctx.sbuf
    cst = S("cst_s", [128, 256], F32)
    cstbf = S("cstbf", [128, 256], BF16)
    cb = Buf("cst")
    ident_bf = cstbf[:, 0:128]
    ones_bf = cstbf[:, 128:256]
    ident_f = cst[:, 0:128]
    vecs = S("vecs_s", [128, 8, D], F32)
    bgbf = S("bgbf", [1, 3072], BF16)
    ctx.dma("sp", cst[:, :], cst_d, writes=[cb])
    ctx.dma("pool", cstbf[:, :], cst_d, writes=[cb])
    ctx.dma("sp", vecs[:, :, :].rearrange("p a d -> p (a d)"),
            vec_d.rearrange("a d -> (a d)").partition_broadcast(128), writes=[cb])
    ctx.dma("pool", bgbf[:, :], bg_d, writes=[cb])
    if moe:
        rw = S("rw_s", [128, 8, NE], F32)
        rb = S("rb_s", [1, NE], F32)
        ctx.dma("sp", rw[:, :, :], rw_d.rearrange("(k p) n -> p k n", p=128), writes=[cb])
        ctx.dma("sp", rb[:, :], rb_d, writes=[cb])

    psr = Ring(ctx, "ps", [128, 512], F32, 8, psum=True)
    wr = WRing(ctx, 5)
    small = Ring(ctx, "small", [128, 32], F32, 4)
    tmpr = Ring(ctx, "tmp", [128, 512], F32, 3)
    tmpb = Ring(ctx, "tmpb", [128, 1024], BF16, 2)

    memT = S("memT", [128, 8, 256], BF16)
    memTb = Buf("memT")
    kxT = S("kxT", [128, 8, 256], BF16)
    vx = S("vx", [128, 2, D], BF16)
    kvb = Buf("kv")
    zbuf = S("zbuf", [128, 4, D], F32); zbb = [Buf("z%d" % j) for j in range(4)]
    outr = Ring(ctx, "xo", [128, D], F32, 2)
    for mc in range(2):
        z, zb = zbuf[:, mc, :], zbb[mc]
        ctx.dma("sp", z[:, :], mem_d[mc * 128:(mc + 1) * 128, :], writes=[zb])
        o, ob = zbuf[:, 2 + mc, :], zbb[2 + mc]
        layer_norm(ctx, z[:, :], zb, o[:, :], ob, vecs[:, 0, :], vecs[:, 1, :], cb, small, 0)
        t, tb = tmpb.next()
        CP(ctx, "act", t[:, :], o[:, :], [ob], [tb])
        transpose_to(ctx, psr, t, tb, memT, memTb, ident_bf, cb, mc * 128)
    for s in range(4):
        w, wb = wr.load([(wkv_d[:, s * 512:(s + 1) * 512], 0)], 8, 512)
        if s < 2:
            for o4 in range(4):
                oc = s * 4 + o4
                ps, pb = psr.next()
                for k in range(8):
                    mm(ctx, ps[:, 0:256], pb, w[:, k, o4 * 128:(o4 + 1) * 128], memT[:, k, :], k == 0, k == 7,
                       [wb, memTb])
                CP(ctx, "dve", kxT[:, oc, :], ps[:, 0:256], [pb], [kvb])
        else:
            nb = s - 2
            for mc in range(2):
                ps, pb = psr.next()
                for k in range(8):
                    mm(ctx, ps[:, :], pb, memT[:, k, mc * 128:(mc + 1) * 128], w[:, k, :], k == 0, k == 7,
                       [wb, memTb])
                CP(ctx, "act", vx[:, mc, nb * 512:(nb + 1) * 512], ps[:, :], [pb], [kvb])

    xtok = S("xtok", [128, 4, D], F32); x1b = [Buf("x1%d" % j) for j in range(4)]
    big = S("big", [128, max(FC, 28), G], BF16); bigb = Buf("big")
    hT = big[:, 0:12, :]; qT = big[:, 12:20, :]; oT = big[:, 20:28, :]; hff = big[:, 0:FC, :]
    hTb = qTb = oTb = hfb = bigb
    merged = S("merged", [128, 4, D], F32); mgb = [Buf("mg%d" % j) for j in range(4)]
    x1 = xtok
    x2 = merged; x2b = mgb
    aT = S("aT", [128, 8, G], BF16); aTb = Buf("aT")
    xT = aT; xTb = aTb
    PT = Ring(ctx, "PT", [128, G], BF16, 4)
    if moe:
        yacc = zbuf; yb = zbb
        x2Tf = S("x2Tf", [128, 8, 128], F32); x2Tfb = Buf("x2Tf")
        comb = S("comb", [128, 4, NE], F32); combb = [Buf("comb%d" % j) for j in range(4)]

    def linear_tok(lhsT_tile, lhsT_buf, w_dram, K, ncols_total, consume, bias_row=None):
        KC = K // 128
        for nb in range(ncols_total // 512):
            w, wb = wr.load([(w_dram[:, nb * 512:(nb + 1) * 512], 0)], KC, 512)
            for j in range(4):
                ps, pb = psr.next()
                for k in range(KC):
                    mm(ctx, ps[:, :], pb, lhsT_tile[:, k, j * 128:(j + 1) * 128], w[:, k, :], k == 0,
                       k == KC - 1 and bias_row is None, [lhsT_buf, wb])
                if bias_row is not None:
                    mm(ctx, ps[:, :], pb, ones_bf[0:1, 0:128], bias_row[0:1, nb * 512:(nb + 1) * 512], False, True,
                       [cb])
                consume(j, nb, ps, pb)

    for g in range(NG):
        t0 = g * G
        ctx.dma("sp", xtok[:, :, :], x_tok[t0:t0 + G, :].rearrange("(j p) d -> p j d", p=128), writes=x1b)
        ctx.dma("pool", xT[:, :, :], xT_d.rearrange("(k p) t -> p k t", p=128)[:, :, t0:t0 + G], writes=[xTb])
        ctx.dma("pool", hT[:, :, :], hT_d.rearrange("(k p) t -> p k t", p=128)[:, :, t0:t0 + G], writes=[hTb])

        for nb in range(6):
            br, half = nb // 2, nb % 2
            wgs, wgb = wr.load([(wg_d[:, nb * 512:(nb + 1) * 512], 0)], 8, 512)
            wus, wub = wr.load([(wup_d[br * 512:(br + 1) * 512, half * 512:(half + 1) * 512], 0)], 4, 512)
            for j in range(4):
                psg, pgb = psr.next()
                for k in range(8):
                    mm(ctx, psg[:, :], pgb, xT[:, k, j * 128:(j + 1) * 128], wgs[:, k, :], k == 0, False, [xTb, wgb])
                mm(ctx, psg[:, :], pgb, ones_bf[0:1, 0:128], bgbf[0:1, nb * 512:(nb + 1) * 512], False, True, [cb])
                psu, pub = psr.next()
                for k in range(4):
                    mm(ctx, psu[:, :], pub, hT[:, br * 4 + k, j * 128:(j + 1) * 128], wus[:, k, :], k == 0, k == 3,
                       [hTb, wub])
                gt, gtb = tmpr.next()
                A_(ctx, gt[:, :], psg[:, :], AF.Sigmoid, [pgb], [gtb])
                dst = merged[:, j, half * 512:(half + 1) * 512]
                if br == 0:
                    TT(ctx, "dve", dst, gt[:, :], psu[:, :], ALU.mult, [gtb, pub], [mgb[j]])
                else:
                    TT(ctx, "dve", gt[:, :], gt[:, :], psu[:, :], ALU.mult, [gtb, pub], [gtb])
                    TT(ctx, "pool", dst, dst, gt[:, :], ALU.add, [gtb, mgb[j]], [mgb[j]])

        for j in range(4):
            t, tb = tmpb.next()
            CP(ctx, "act", t[:, :], merged[:, j, :], [mgb[j]], [tb])
            transpose_to(ctx, psr, t, tb, aT, aTb, ident_bf, cb, j * 128)
        zs = [(zbuf[:, j, :], zbb[j]) for j in range(4)]

        def cons1(j, nb, ps, pb):
            z, zb = zs[j]
            STT(ctx, "dve", z[:, nb * 512:(nb + 1) * 512], xtok[:, j, nb * 512:(nb + 1) * 512], ALPHA, ps[:, :],
                ALU.mult, ALU.add, [x1b[j], pb], [zb])
        linear_tok(aT, aTb, wout_d, D, D, cons1)
        for j in range(4):
            z, zb = zs[j]
            layer_norm(ctx, z[:, :], zb, x1[:, j, :], x1b[j], vecs[:, 2, :], vecs[:, 3, :], cb, small, 0)
            t, tb = tmpb.next()
            CP(ctx, "act", t[:, :], x1[:, j, :], [x1b[j]], [tb])
            transpose_to(ctx, psr, t, tb, aT, aTb, ident_bf, cb, j * 128)

        for s in range(2):
            w, wb = wr.load([(wq_d[:, s * 512:(s + 1) * 512], 0)], 8, 512)
            for o4 in range(4):
                oc = s * 4 + o4
                ps, pb = psr.next()
                for k in range(8):
                    mm(ctx, ps[:, :], pb, w[:, k, o4 * 128:(o4 + 1) * 128], aT[:, k, :], k == 0, k == 7, [wb, aTb])
                A_(ctx, qT[:, oc, :], ps[:, :], AF.Copy, [pb], [qTb], scale=1.0 / 16.0)
        for h in range(4):
            pts = []
            for mc in range(2):
                ps, pb = psr.next()
                for c in range(2):
                    mm(ctx, ps[:, :], pb, kxT[:, h * 2 + c, mc * 128:(mc + 1) * 128], qT[:, h * 2 + c, :], c == 0,
                       c == 1, [kvb, qTb])
                pt, ptb = PT.next()
                A_(ctx, pt[:, :], ps[:, :], AF.Exp, [pb], [ptb])
                pts.append((pt, ptb))
            psd, pdb = psr.next()
            for mc in range(2):
                mm(ctx, psd[:, :], pdb, ones_bf, pts[mc][0][:, :], mc == 0, mc == 1, [cb, pts[mc][1]])
            rd, rdb = tmpr.next()
            ctx.op("dve", lambda e, o=rd[:, :], i=psd[:, :]: e.reciprocal(o, i), reads=[pdb], writes=[rdb])
            for dc in range(2):
                pso, pob = psr.next()
                for mc in range(2):
                    mm(ctx, pso[:, :], pob, vx[:, mc, h * 256 + dc * 128:h * 256 + (dc + 1) * 128], pts[mc][0][:, :],
                       mc == 0, mc == 1, [kvb, pts[mc][1]])
                TT(ctx, "dve", oT[:, h * 2 + dc, :], pso[:, :], rd[:, :], ALU.mult, [pob, rdb], [oTb])
        zs = [(zbuf[:, j, :], zbb[j]) for j in range(4)]

        def cons2(j, nb, ps, pb):
            z, zb = zs[j]
            STT(ctx, "dve", z[:, nb * 512:(nb + 1) * 512], x1[:, j, nb * 512:(nb + 1) * 512], ALPHA, ps[:, :],
                ALU.mult, ALU.add, [x1b[j], pb], [zb])
        linear_tok(oT, oTb, wo_d, D, D, cons2)
        for j in range(4):
            z, zb = zs[j]
            layer_norm(ctx, z[:, :], zb, x2[:, j, :], x2b[j], vecs[:, 4, :], vecs[:, 5, :], cb, small, 0)
            t, tb = tmpb.next()
            CP(ctx, "act", t[:, :], x2[:, j, :], [x2b[j]], [tb])
            transpose_to(ctx, psr, t, tb, aT, aTb, ident_bf, cb, j * 128)
            if moe:
                for hh in range(2):
                    ps, pb = psr.next()
                    for q in range(4):
                        k = hh * 4 + q
                        mm(ctx, ps[:, q * 128:(q + 1) * 128], pb, x2[:, j, k * 128:(k + 1) * 128], ident_f, True, True,
                           [x2b[j], cb])
                    CP(ctx, "dve", x2Tf[:, hh * 4:(hh + 1) * 4, :], ps[:, :].rearrange("p (k t) -> p k t", k=4), [pb],
                       [x2Tfb])
                ps, pb = psr.next()
                for k in range(8):
                    mm(ctx, ps[:, 0:NE], pb, x2Tf[:, k, :], rw[:, k, :], k == 0, False, [x2Tfb, cb])
                mm(ctx, ps[:, 0:NE], pb, cst[0:1, 128:256], rb[0:1, :], False, True, [cb])
                sm, smb = small.next()
                lg = sm[:, 0:8]; m1 = sm[:, 8:9]; m2 = sm[:, 9:10]; k1 = sm[:, 10:18]; l2 = sm[:, 18:26]
                w1 = sm[:, 26:27]; w2 = sm[:, 27:28]
                CP(ctx, "dve", lg, ps[:, 0:NE], [pb], [smb])
                ctx.op("dve", lambda e, o=m1, i=lg: e.reduce_max(o, i, AX.X), reads=[smb], writes=[smb])
                TS(ctx, "dve", k1, lg, m1, None, ALU.is_equal, None, [smb], [smb])
                STT(ctx, "dve", l2, k1, -1e30, lg, ALU.mult, ALU.add, [smb], [smb])
                ctx.op("dve", lambda e, o=m2, i=l2: e.reduce_max(o, i, AX.X), reads=[smb], writes=[smb])
                TS(ctx, "dve", l2, l2, m2, None, ALU.is_equal, None, [smb], [smb])
                TT(ctx, "dve", w2, m2, m1, ALU.subtract, [smb], [smb])
                A_(ctx, w2, w2, AF.Exp, [smb], [smb])
                TS(ctx, "dve", w1, w2, 1.0, None, ALU.add, None, [smb], [smb])
                ctx.op("dve", lambda e, o=w1, i=w1: e.reciprocal(o, i), reads=[smb], writes=[smb])
                TT(ctx, "dve", w2, w2, w1, ALU.mult, [smb], [smb])
                TS(ctx, "dve", k1, k1, w1, None, ALU.mult, None, [smb], [smb])
                STT(ctx, "dve", comb[:, j, :], l2, w2, k1, ALU.mult, ALU.add, [smb], [combb[j]])

        for e_ in range(NE if moe else 1):
            wgu_e = wgu_d[e_] if moe else wgu_d
            wd_e = wd_d[e_] if moe else wd_d
            for fp in range(FC // 2):
                w, wb = wr.load([(wgu_e[:, fp * 256:(fp + 1) * 256], 0), (wgu_e[:, FF + fp * 256:FF + (fp + 1) * 256], 256)],
                                8, 512)
                for f2 in range(2):
                    fc = fp * 2 + f2
                    psg, pgb = psr.next()
                    for k in range(8):
                        mm(ctx, psg[:, :], pgb, w[:, k, f2 * 128:(f2 + 1) * 128], aT[:, k, :], k == 0, k == 7, [wb, aTb])
                    psu, pub = psr.next()
                    for k in range(8):
                        mm(ctx, psu[:, :], pub, w[:, k, 256 + f2 * 128:256 + (f2 + 1) * 128], aT[:, k, :], k == 0, k == 7,
                           [wb, aTb])
                    sg, sgb = tmpr.next()
                    A_(ctx, sg[:, :], psg[:, :], AF.Silu, [pgb], [sgb])
                    TT(ctx, "dve", hff[:, fc, :], sg[:, :], psu[:, :], ALU.mult, [sgb, pub], [hfb])
            parts = []
            k0 = 0
            while k0 < FC:
                parts.append((k0, min(8, FC - k0)))
                k0 += 8
            if not moe:
                zs = [(zbuf[:, j, :], zbb[j]) for j in range(4)]
            for nb in range(2):
                pss = [psr.next() for _ in range(4)]
                for pi, (k0, kn) in enumerate(parts):
                    w, wb = wr.load([(wd_e[k0 * 128:(k0 + kn) * 128, nb * 512:(nb + 1) * 512], 0)], kn, 512)
                    for j in range(4):
                        for k in range(kn):
                            mm(ctx, pss[j][0][:, :], pss[j][1], hff[:, k0 + k, j * 128:(j + 1) * 128], w[:, k, :],
                               k0 + k == 0, k0 + k == FC - 1, [hfb, wb])
                for j in range(4):
                    ps, pb = pss[j]
                    sl = slice(nb * 512, (nb + 1) * 512)
                    if not moe:
                        z, zb = zs[j]
                        STT(ctx, "dve", z[:, sl], x2[:, j, sl], ALPHA, ps[:, :], ALU.mult, ALU.add, [x2b[j], pb], [zb])
                    elif e_ == 0:
                        TS(ctx, "dve", yacc[:, j, sl], ps[:, :], comb[:, j, 0:1], None, ALU.mult, None,
                           [pb, combb[j]], [yb[j]])
                    else:
                        STT(ctx, "dve", yacc[:, j, sl], ps[:, :], comb[:, j, e_:e_ + 1], yacc[:, j, sl], ALU.mult, ALU.add,
                            [pb, combb[j], yb[j]], [yb[j]])
        if moe:
            zs = [(zbuf[:, j, :], zbb[j]) for j in range(4)]
            for j in range(4):
                z, zb = zs[j]
                STT(ctx, "dve", z[:, :], x2[:, j, :], ALPHA, yacc[:, j, :], ALU.mult, ALU.add, [x2b[j], yb[j]], [zb])
        for j in range(4):
            z, zb = zs[j]
            o, ob = outr.next()
            layer_norm(ctx, z[:, :], zb, o[:, :], ob, vecs[:, 6, :], vecs[:, 7, :], cb, small, 0)
            ctx.dma("sp", out_d[t0 + j * 128:t0 + (j + 1) * 128, :], o[:, :], reads=[ob])
    ctx.wait_all_dma("sp")
    ctx.emit()
    ctx.close()
    return nc, ctx


FGROUPS = ([("mq0", 64), ("mq1", 64), ("mk0", 64), ("mk1", 64), ("mo0", 128), ("mo1", 128),
            ("fq0", 128), ("fq1", 128), ("fk0", 128), ("fk1", 128)]
           + [("rr%d" % h, 64) for h in range(4)] + [("rk%d" % h, 64) for h in range(4)]
           + [("rv%d" % h, 64) for h in range(4)] + [("lw", 64), ("la", 64), ("lg", 128), ("lv", 32)])
FOFF = {}
_o = 0
for _n, _w in FGROUPS:
    FOFF[_n] = (_o, _w, len(FOFF))
    _o += _w
NF = _o
NTM = 520
PVN = (["conv%d_%s" % (j, g) for g in ("mq0", "mq1", "mk0", "mk1") for j in range(4)]
       + ["mnorm0", "mnorm1"]
       + ["%s_%d" % (n, h) for n in ("mu_r", "mu_k", "mu_v", "wbias", "abias", "vbias", "kkw", "ka", "rk", "lng", "lnb")
          for h in range(4)]
       + ["mu_lw", "mu_la", "mu_lg", "mu_lv"])
PVI = {n: i for i, n in enumerate(PVN)}


def build_l1(T=8192, vres=False, rw_level=2):
    NB = T // 512
    NQ = T // 128
    nc = bass.Bass("TRN2", target_bir_lowering=False)
    ctx = Ctx(nc)

    def din(name, shape):
        return nc.dram_tensor(name, list(shape), F32, kind="ExternalInput").ap()

    xT_d = din("xT", [D, T])
    wf_d = din("wf", [D, NF])
    wt_d = din("wt", [D, NTM])
    bf_d = din("bfm", [128, len(FGROUPS)])
    bt_d = din("btm", [1, NTM])
    pv_d = din("pv", [128, len(PVN)])
    cst_d = din("cst", [128, 1280])
    lora_d = din("lora", [128, 4, 256])
    if vres:
        vf_d = din("vfirst", [256, T])
    hout_d = nc.dram_tensor("hout", [768, T], F32, kind="ExternalOutput").ap()
    vown_d = nc.dram_tensor("vown", [256, T], F32, kind="ExternalOutput").ap()

    ctx.alloc_sems()
    S = ctx.sbuf
    cb = Buf("const")
    cst = S("cst_s", [128, 1280], F32)
    cstb = S("cst_b", [128, 1024], BF16)
    ctx.dma("sp", cst[:, :], cst_d, writes=[cb])
    ctx.dma("pool", cstb[:, :], cst_d[:, 0:1024], writes=[cb])
    mSU, mSL = cst[:, 1024:1152], cst[:, 1152:1280]
    ident_f, ones_f, mLT, utri, sel127, sel63, mneg, bdiag = [cst[:, i * 128:(i + 1) * 128] for i in range(8)]
    ident_b, ones_b = cstb[:, 0:128], cstb[:, 128:256]
    mneg_b = cstb[:, 768:896]
    wt = S("wt_s", [128, 8, NTM], BF16)
    wfr = WRing(ctx, 6, name="wfr", slot=1024)
    ctx.dma("pool", wt[:, :, :], wt_d.rearrange("(k p) n -> p k n", p=128), writes=[cb])
    bfm = S("bfm_s", [128, len(FGROUPS)], F32)
    btm = S("btm_s", [1, NTM], F32)
    pv = S("pv_s", [128, len(PVN)], F32)
    lora = S("lora_s", [128, 4, 256], F32)
    ctx.dma("sp", bfm[:, :], bf_d, writes=[cb])
    ctx.dma("sp", btm[:, :], bt_d, writes=[cb])
    ctx.dma("sp", pv[:, :], pv_d, writes=[cb])
    ctx.dma("sp", lora[:, :, :], lora_d, writes=[cb])

    def PV(name, w=128):
        i = PVI[name]
        return pv[0:w, i:i + 1]

    psr = Ring(ctx, "ps", [128, 512], F32, 5, psum=True)
    psacc = Ring(ctx, "pa", [128, 512], F32, 3, psum=True)
    xTr = Ring(ctx, "xTb", [128, 8, 512], BF16, 1)
    small = Ring(ctx, "small", [128, 64], F32, 6)

    fK = S("fK", [128, 2, T], BF16); fKb = Buf("fK")
    fV = S("fV", [128, NQ, 4 * 65], BF16); fVb = Buf("fV")
    fG = S("fG", [128, NQ, 4], F32); fGb = Buf("fG")
    ctx.op("pool", lambda e: e.memset(fV[:, :, :], 1.0), writes=[fVb])
    fQr = Ring(ctx, "fQ", [128, 2, 512], BF16, 2)
    PTr = Ring(ctx, "PT", [128, 128], BF16, 4)
    fbias = Ring(ctx, "fbias", [128, NQ], F32, 3)
    hbr = Ring(ctx, "hb", [128, 256], F32, 2)
    outT = Ring(ctx, "outT", [128, 256], F32, 3)

    LN8 = float(np.log(0.125))
    mU = [S("mU%d" % i, [64, 515], F32) for i in range(4)]; mUb = [Buf("mU%d" % i) for i in range(4)]
    for i in range(4):
        ctx.op("pool", lambda e, i=i: e.memset(mU[i][:, 0:3], 0.0), writes=[mUb[i]])
    mQK = [S("mQK%d" % i, [64, 512], F32) for i in range(4)]; mQKb = [Buf("mQK%d" % i) for i in range(4)]
    mVa = S("mVa", [128, 4, 2, 129], F32); mVab = [Buf("mVa%d" % j) for j in range(4)]
    ctx.op("pool", lambda e: e.memset(mVa[:, :, :, :].rearrange("p a b c -> p (a b c)"), 1.0), writes=mVab)
    mC = [S("mC%d" % i, [64, 129], F32) for i in range(2)]; mCb = [Buf("mC%d" % i) for i in range(2)]
    for i in range(2):
        ctx.op("pool", lambda e, i=i: e.memset(mC[i][:, :], 0.0), writes=[mCb[i]])
    mSig = [S("mSig%d" % i, [128, 512], F32) for i in range(2)]; mSigb = [Buf("mSig%d" % i) for i in range(2)]
    mWT = Ring(ctx, "mWT", [128, 128], F32, 2)
    mKg = Ring(ctx, "mKg", [128, 64], F32, 2)
    mH = Ring(ctx, "mH", [128, 128], F32, 2)
    mSc = Ring(ctx, "mSc", [128, 32], F32, 4)

    RG = [n for n, _ in FGROUPS if n[0] == "r" or n in ("lw", "la", "lg", "lv")]
    rHalo = S("rHalo", [128, len(RG)], F32); rHb = Buf("rHalo")
    ctx.op("pool", lambda e: e.memset(rHalo[:, :], 0.0), writes=[rHb])
    rP = Ring(ctx, "rP", [128, 513], F32, 3)
    rLo = [S("rLo%d" % i, [128, 512], F32) for i in range(4)]; rLob = [Buf("rLo%d" % i) for i in range(4)]
    NRX = 10
    rX = [[S("rX%d_%d" % (s_, i), [64, 512], F32) for i in range(NRX)] for s_ in range(1)]
    rXb = [[Buf("rX%d_%d" % (s_, i)) for i in range(NRX)] for s_ in range(1)]
    rM = [S("rM%d" % i, [64, 64], F32) for i in range(4)]; rMb = [Buf("rM%d" % i) for i in range(4)]
    for i in range(4):
        ctx.op("pool", lambda e, i=i: e.memset(rM[i][:, :], 0.0), writes=[rMb[i]])
    rE = Ring(ctx, "rE", [64, 5, 128], F32, 1)
    rF = Ring(ctx, "rF", [64, 6, 128], F32, 1)
    rTK = Ring(ctx, "rTK", [128, 256], F32, 2)
    rKX = Ring(ctx, "rKX", [128, 128], F32, 2)
    rA = Ring(ctx, "rA", [128, 4, 128], F32, 1)
    rN = Ring(ctx, "rN", [128, 128], F32, 6)
    rR = Ring(ctx, "rR", [128, 128], F32, 3)
    rWU = Ring(ctx, "rWU", [128, 128], F32, 2)
    rS64 = Ring(ctx, "rS64", [64, 256], F32, 3)
    rY = Ring(ctx, "rY", [128, 64], F32, 2)
    SIGC = 0.6065306597126334

    def fm_proj(xt, xtb, name, evac):
        off, w, gi = FOFF[name]
        wv, wvb = wfr.load([(wf_d[:, off:off + w], 0)], 8, w)
        ps, pb = psr.next()
        for k in range(8):
            mm(ctx, ps[0:w, :], pb, wv[:, k, :], xt[:, k, :], k == 0, k == 7, [wvb, xtb])
        evac(ps[0:w, :], pb, bfm[0:w, gi:gi + 1])

    for blk in range(NB):
        t0 = blk * 512
        xt, xtb = xTr.next()
        ctx.dma("pool", xt[:, :, :], xT_d.rearrange("(k p) t -> p k t", p=128)[:, :, t0:t0 + 512], writes=[xtb])

        tmv = []
        for j in range(4):
            ps, pb = psr.next()
            for k in range(8):
                mm(ctx, ps[:, :], pb, xt[:, k, j * 128:(j + 1) * 128], wt[:, k, 0:512], k == 0, False, [xtb, cb])
            mm(ctx, ps[:, :], pb, ones_f[0:1, 0:128], btm[0:1, 0:512], False, True, [cb])
            ps2, pb2 = psr.next()
            for k in range(8):
                mm(ctx, ps2[:, 0:8], pb2, xt[:, k, j * 128:(j + 1) * 128], wt[:, k, 512:520], k == 0, False, [xtb, cb])
            mm(ctx, ps2[:, 0:8], pb2, ones_f[0:1, 0:128], btm[0:1, 512:520], False, True, [cb])
            qb = blk * 4 + j
            CP(ctx, "act", fV[:, qb, :].rearrange("p (h c) -> p h c", c=65)[:, :, 0:64],
               ps[:, 256:512].rearrange("p (h c) -> p h c", c=64), [pb], [fVb])
            sm, smb = small.next()
            A_(ctx, sm[:, 0:8], ps2[:, 0:8], AF.Exp, [pb2], [smb], scale=-1.0)
            A_(ctx, sm[:, 8:16], sm[:, 0:8], AF.Ln, [smb], [smb], bias=1.0)
            psg, pgb = psr.next()
            mm(ctx, psg[:, 0:4], pgb, utri, sm[:, 12:16], True, qb == 0, [cb, smb])
            if qb > 0:
                mm(ctx, psg[:, 0:4], pgb, sel127, fG[:, qb - 1, :], False, True, [cb, fGb])
            CP(ctx, "dve", fG[:, qb, :], psg[:, 0:4], [pgb], [fGb])
            CP(ctx, "dve", mVa[:, j, :, 0:128], ps[:, 0:256].rearrange("p (h c) -> p h c", c=128), [pb], [mVab[j]])
            sc, scb = mSc.next()
            psm, pmb = psr.next()
            mm(ctx, psm[:, 0:2], pmb, utri, sm[:, 10:12], True, True, [cb, smb])
            mm(ctx, psm[:, 2:4], pmb, ones_f, sm[:, 10:12], True, True, [cb, smb])
            CP(ctx, "dve", sc[:, 0:4], psm[:, 0:4], [pmb], [scb])
            TT(ctx, "dve", sc[:, 12:14], ps2[:, 0:2], sc[:, 0:2], ALU.add, [pb2, scb], [scb])
            A_(ctx, sc[:, 4:6], sc[:, 12:14], AF.Exp, [scb], [scb], bias=LN8)
            A_(ctx, sc[:, 6:8], sc[:, 0:2], AF.Exp, [scb], [scb], scale=-1.0)
            TT(ctx, "dve", sc[:, 12:14], sc[:, 12:14], sc[:, 2:4], ALU.subtract, [scb], [scb])
            A_(ctx, sc[:, 8:10], sc[:, 12:14], AF.Exp, [scb], [scb], bias=LN8)
            A_(ctx, sc[:, 10:12], sc[:, 2:4], AF.Exp, [scb], [scb], scale=-1.0)
            tmv.append((sc, scb))

        for gi_, gname in enumerate(("mq0", "mq1", "mk0", "mk1")):
            U, Ub = mU[gi_], mUb[gi_]
            fm_proj(xt, xtb, gname, lambda ps, pb, bias, U=U, Ub=Ub: A_(ctx, U[:, 3:515], ps, AF.Identity, [pb, cb], [Ub], bias=bias))
            q_, qb_ = mQK[gi_], mQKb[gi_]
            TS(ctx, "dve", q_[:, :], U[:, 0:512], PV("conv0_" + gname, 64), None, ALU.mult, None, [Ub, cb], [qb_])
            for jj in range(1, 4):
                STT(ctx, "dve", q_[:, :], U[:, jj:jj + 512], PV("conv%d_%s" % (jj, gname), 64), q_[:, :], ALU.mult, ALU.add,
                    [Ub, cb, qb_], [qb_])
            A_(ctx, q_[:, :], q_[:, :], AF.Silu, [qb_], [qb_])
            CP(ctx, "pool", U[:, 0:3], U[:, 512:515], [Ub], [Ub])
        for i in range(2):
            fm_proj(xt, xtb, "mo%d" % i, lambda ps, pb, bias, i=i: A_(ctx, mSig[i][:, :], ps, AF.Sigmoid, [pb, cb], [mSigb[i]], bias=bias))
        for j in range(4):
            sc, scb = tmv[j]
            cs = slice(j * 128, (j + 1) * 128)
            for i in range(2):
                qT, qTb_, kT, kTb_ = mQK[i], mQKb[i], mQK[2 + i], mQKb[2 + i]
                psG, pGb = psr.next()
                mm(ctx, psG[:, 0:128], pGb, kT[:, cs], qT[:, cs], True, True, [kTb_, qTb_])
                wt_, wtb = mWT.next()
                STT(ctx, "dve", wt_[:, :], psG[:, 0:128], sc[:, 4 + i:5 + i], mLT, ALU.mult, ALU.mult, [pGb, scb, cb], [wtb])
                psN, pNb = psacc.next()
                mm(ctx, psN[:, 0:129], pNb, wt_[:, :], mVa[:, j, i, :], True, False, [wtb, mVab[j]])
                mm(ctx, psN[:, 0:129], pNb, qT[:, cs], mC[i][:, :], False, True, [qTb_, mCb[i]])
                psK, pKb = psr.next()
                mm(ctx, psK[:, 0:64], pKb, kT[:, cs], ident_f[0:64, 0:64], True, True, [kTb_, cb])
                kg, kgb = mKg.next()
                TS(ctx, "dve", kg[:, :], psK[:, 0:64], sc[:, 8 + i:9 + i], None, ALU.mult, None, [pKb, scb], [kgb])
                psC, pCb = psr.next()
                mm(ctx, psC[0:64, 0:129], pCb, kg[:, :], mVa[:, j, i, :], True, True, [kgb, mVab[j]])
                STT(ctx, "dve", mC[i][:, :], mC[i][:, :], sc[0:64, 10 + i:11 + i], psC[0:64, 0:129], ALU.mult, ALU.add,
                    [mCb[i], scb, pCb], [mCb[i]])
                s2, s2b = small.next()
                TT(ctx, "dve", s2[:, 0:1], psN[:, 128:129], sc[:, 6 + i:7 + i], ALU.mult, [pNb, scb], [s2b])
                A_(ctx, s2[:, 0:1], s2[:, 0:1], AF.Abs, [s2b], [s2b])
                TS(ctx, "dve", s2[:, 0:1], s2[:, 0:1], 1.0, None, ALU.max, None, [s2b], [s2b])
                ctx.op("dve", lambda e, o=s2[:, 1:2], i_=s2[:, 0:1]: e.reciprocal(o, i_), reads=[s2b], writes=[s2b])
                TT(ctx, "dve", s2[:, 1:2], s2[:, 1:2], sc[:, 6 + i:7 + i], ALU.mult, [s2b, scb], [s2b])
                hh_, hhb = mH.next()
                TS(ctx, "dve", hh_[:, :], psN[:, 0:128], s2[:, 1:2], None, ALU.mult, None, [pNb, s2b], [hhb])
                ctx.op("dve", lambda e, o=s2[:, 8:14], i_=hh_[:, :]: e.bn_stats(o, i_), reads=[hhb], writes=[s2b])
                ctx.op("dve", lambda e, o=s2[:, 16:18], i_=s2[:, 8:14]: e.bn_aggr(o, i_), reads=[s2b], writes=[s2b])
                A_(ctx, s2[:, 18:19], s2[:, 17:18], AF.Sqrt, [s2b], [s2b], bias=1e-6)
                ctx.op("dve", lambda e, o=s2[:, 18:19]: e.reciprocal(o, o), reads=[s2b], writes=[s2b])
                TS(ctx, "dve", hh_[:, :], hh_[:, :], s2[:, 16:17], s2[:, 18:19], ALU.subtract, ALU.mult, [hhb, s2b], [hhb])
                psT, pTb = psr.next()
                mm(ctx, psT[:, 0:128], pTb, hh_[:, :], ident_f, True, True, [hhb, cb])
                ot, otb = outT.next()
                STT(ctx, "dve", ot[:, 0:128], psT[:, 0:128], PV("mnorm%d" % i), mSig[i][:, cs], ALU.mult, ALU.mult,
                    [pTb, cb, mSigb[i]], [otb])
                ctx.dma("sp", hout_d[i * 128:(i + 1) * 128, t0 + j * 128:t0 + (j + 1) * 128], ot[:, 0:128], reads=[otb])

        def lerp_group(name, dst, dstb, post=None):
            off, w, gi = FOFF[name]
            hi = RG.index(name)
            P, Pb = rP.next()
            CP(ctx, "pool", P[0:w, 0:1], rHalo[0:w, hi:hi + 1], [rHb], [Pb])
            fm_proj(xt, xtb, name, lambda ps, pb, bias: A_(ctx, P[0:w, 1:513], ps, AF.Identity, [pb, cb], [Pb], bias=bias))
            CP(ctx, "pool", rHalo[0:w, hi:hi + 1], P[0:w, 512:513], [Pb], [rHb])
            Dd, Ddb = rP.next()
            TT(ctx, "pool", Dd[0:w, 0:512], P[0:w, 0:512], P[0:w, 1:513], ALU.subtract, [Pb], [Ddb])
            muname = {"lw": "mu_lw", "la": "mu_la", "lg": "mu_lg", "lv": "mu_lv"}.get(name) or "mu_%s_%s" % (name[1], name[2])
            STT(ctx, "dve", dst, Dd[0:w, 0:512], PV(muname, w), P[0:w, 1:513], ALU.mult, ALU.add, [Pb, Ddb, cb], [dstb])
            if post is not None:
                A_(ctx, dst, dst, post, [dstb], [dstb])

        lerp_group("lw", rLo[0][0:64, :], rLob[0], AF.Tanh)
        lerp_group("la", rLo[1][0:64, :], rLob[1])
        lerp_group("lg", rLo[2][0:128, :], rLob[2], AF.Sigmoid)
        if vres:
            lerp_group("lv", rLo[3][0:32, :], rLob[3])
        for i in range(4 if rw_level >= 1 else 0):
            X, Xb = rX[0], rXb[0]
            r_, k_, v_, nlw, a_, g_, kk, k2, bb, t1 = [X[q][:, :] for q in range(NRX)]
            r_b, k_b, v_b, nlwb, a_b, g_b, kkb, k2b, bbb, t1b = Xb
            bv, bvb = k_, k_b
            t2, t2b = kk, kkb
            lerp_group("rr%d" % i, r_, r_b)
            lerp_group("rk%d" % i, k_, k_b)
            lerp_group("rv%d" % i, v_, v_b)
            ctx.dma("sp", vown_d[i * 64:(i + 1) * 64, t0:t0 + 512], v_, reads=[v_b])
            ic = slice(i * 64, (i + 1) * 64)
            ps, pb = psr.next()
            mm(ctx, ps[0:64, :], pb, lora[0:64, 0, ic], rLo[0][0:64, :], True, True, [cb, rLob[0]])
            A_(ctx, nlw, ps[0:64, :], AF.Sigmoid, [pb, cb], [nlwb], bias=PV("wbias_%d" % i, 64))
            TS(ctx, "pool", nlw, nlw, SIGC, None, ALU.mult, None, [nlwb], [nlwb])
            ps, pb = psr.next()
            mm(ctx, ps[0:64, :], pb, lora[0:64, 1, ic], rLo[1][0:64, :], True, True, [cb, rLob[1]])
            A_(ctx, a_, ps[0:64, :], AF.Sigmoid, [pb, cb], [a_b], bias=PV("abias_%d" % i, 64))
            ps, pb = psr.next()
            mm(ctx, ps[0:64, :], pb, lora[0:128, 2, ic], rLo[2][0:128, :], True, True, [cb, rLob[2]])
            CP(ctx, "act", g_, ps[0:64, :], [pb], [g_b])
            if vres:
                ps, pb = psr.next()
                mm(ctx, ps[0:64, :], pb, lora[0:32, 3, ic], rLo[3][0:32, :], True, True, [cb, rLob[3]])
                A_(ctx, t1, ps[0:64, :], AF.Sigmoid, [pb, cb], [t1b], bias=PV("vbias_%d" % i, 64))
                ctx.dma("sp", t2, vf_d[i * 64:(i + 1) * 64, t0:t0 + 512], writes=[t2b])
                TT(ctx, "pool", t2, t2, v_, ALU.subtract, [t2b, v_b], [t2b])
                TT(ctx, "pool", t2, t2, t1, ALU.mult, [t2b, t1b], [t2b])
                TT(ctx, "pool", v_, v_, t2, ALU.add, [v_b, t2b], [v_b])
            TS(ctx, "pool", kk, k_, PV("kkw_%d" % i, 64), None, ALU.mult, None, [k_b, cb], [kkb])
            TT(ctx, "pool", t1, kk, kk, ALU.mult, [kkb], [t1b])
            ps, pb = psr.next()
            mm(ctx, ps[0:64, :], pb, ones_f[0:64, 0:64], t1, True, True, [cb, t1b])
            A_(ctx, t1, ps[0:64, :], AF.Sqrt, [pb], [t1b])
            TS(ctx, "dve", t1, t1, 1e-12, None, ALU.max, None, [t1b], [t1b])
            ctx.op("dve", lambda e, o=t1: e.reciprocal(o, o), reads=[t1b], writes=[t1b])
            TT(ctx, "pool", kk, kk, t1, ALU.mult, [kkb, t1b], [kkb])
            TS(ctx, "dve", t1, a_, 1.0, PV("ka_%d" % i, 64), ALU.subtract, ALU.mult, [a_b, cb], [t1b])
            STT(ctx, "dve", k2, t1, 1.0, k_, ALU.add, ALU.mult, [t1b, k_b], [k2b])
            TT(ctx, "pool", bb, kk, a_, ALU.mult, [kkb, a_b], [bbb])
            STT(ctx, "dve", t1, r_, PV("rk_%d" % i, 64), k2, ALU.mult, ALU.mult, [r_b, cb, k2b], [t1b])
            ps, pb = psr.next()
            mm(ctx, ps[0:64, :], pb, ones_f[0:64, 0:64], t1, True, True, [cb, t1b])
            TT(ctx, "dve", bv, ps[0:64, :], v_, ALU.mult, [pb, v_b], [bvb])
            M, Mb = rM[i], rMb[i]
            for j in range(4 if rw_level >= 2 else 0):
                cs = slice(j * 128, (j + 1) * 128)
                E, Eb = rE.next()
                ncl, eg, egm, ei, egl = [E[:, q, :] for q in range(5)]
                ctx.op("dve", lambda e, o=ncl, d1=nlw[:, cs]: e.tensor_tensor_scan(o, ones_f[0:64, 0:128], d1, 0.0, ALU.mult, ALU.add),
                       reads=[cb, nlwb], writes=[Eb])
                sm, smb = small.next()
                TS(ctx, "pool", sm[0:64, 0:1], ncl[:, 127:128], -1.0, None, ALU.mult, None, [Eb], [smb])
                TT(ctx, "pool", egm, nlw[:, cs], ncl, ALU.subtract, [nlwb, Eb], [Eb])
                A_(ctx, eg, ncl, AF.Exp, [Eb], [Eb], scale=-1.0)
                A_(ctx, egm, egm, AF.Exp, [Eb], [Eb])
                A_(ctx, ei, ncl, AF.Exp, [Eb], [Eb])
                A_(ctx, egl, ncl, AF.Exp, [Eb, smb], [Eb], bias=sm[0:64, 0:1])
                Fm, Fb = rF.next()
                KR = Fm[:, 0:2, :]
                BiT, KiT, KtT, NBtT = [Fm[:, q, :] for q in range(2, 6)]
                TT(ctx, "pool", Fm[:, 0, :], kk[:, cs], egm, ALU.mult, [kkb, Eb], [Fb])
                TT(ctx, "pool", Fm[:, 1, :], r_[:, cs], eg, ALU.mult, [r_b, Eb], [Fb])
                TT(ctx, "pool", BiT, bb[:, cs], ei, ALU.mult, [bbb, Eb], [Fb])
                TT(ctx, "pool", KiT, k2[:, cs], ei, ALU.mult, [k2b, Eb], [Fb])
                TT(ctx, "pool", KtT, k2[:, cs], egl, ALU.mult, [k2b, Eb], [Fb])
                STT(ctx, "dve", NBtT, bb[:, cs], -1.0, egl, ALU.mult, ALU.mult, [bbb, Eb], [Fb])
                if rw_level == 3:
                    continue
                psT, pTb = psr.next()
                SUB = 9
                for q, src, srcb in ((0, Fm[:, 0, :], Fb), (1, Fm[:, 1, :], Fb), (2, v_[:, cs], v_b), (3, KtT, Fb), (4, NBtT, Fb)):
                    if SUB == 1 and q >= 2:
                        continue
                    mm(ctx, psT[:, q * 64:(q + 1) * 64], pTb, src, ident_f[0:64, 0:64], True, True, [srcb, cb])
                TK, TKb = rTK.next()
                KX, KXb = rKX.next()
                if SUB <= 2:
                    continue
                CP(ctx, "act", TK[:, :], psT[:, 64:320], [pTb], [TKb])
                if SUB <= 3:
                    continue
                CP(ctx, "dve", KX[:, 0:64], psT[:, 0:64], [pTb], [KXb])
                Rg_tok, V_tok, Kt_tok, NBt_tok = [TK[:, q * 64:(q + 1) * 64] for q in range(4)]
                if rw_level == 4:
                    continue
                Am, Ab = rA.next()
                NT_, NArbT, AkkT, ArkT = [Am[:, q, :] for q in range(4)]
                KRf = KR.rearrange("p a t -> p (a t)")
                ps1, p1b = psr.next()
                mm(ctx, ps1[:, 0:256], p1b, BiT, KRf, True, True, [Fb])
                STT(ctx, "dve", NT_, ps1[:, 0:128], -1.0, mSU, ALU.mult, ALU.mult, [p1b, cb], [Ab])
                STT(ctx, "dve", NArbT, ps1[:, 128:256], -1.0, mLT, ALU.mult, ALU.mult, [p1b, cb], [Ab])
                ps2_, p2b = psr.next()
                mm(ctx, ps2_[:, 0:256], p2b, KiT, KRf, True, True, [Fb])
                TT(ctx, "dve", AkkT, ps2_[:, 0:128], mSU, ALU.mult, [p2b, cb], [Ab])
                TT(ctx, "dve", ArkT, ps2_[:, 128:256], mLT, ALU.mult, [p2b, cb], [Ab])
                ps3, p3b = psr.next()
                mm(ctx, ps3[:, 0:128], p3b, Fm[:, 0, :], BiT, True, True, [Fb])
                A_cur, A_curb = rN.next()
                STT(ctx, "dve", A_cur[:, :], ps3[:, 0:128], -1.0, mSL, ALU.mult, ALU.mult, [p3b, cb], [A_curb])
                if rw_level == 5:
                    continue
                AT_cur, AT_curb = NT_, Ab
                RT, RTb = rR.next()
                TT(ctx, "pool", RT[:, :], NT_, ident_f, ALU.add, [Ab, cb], [RTb])
                for lvl in range(6):
                    psa, pab = psr.next()
                    mm(ctx, psa[:, 0:128], pab, AT_cur, A_cur[:, :], True, True, [AT_curb, A_curb])
                    A2, A2b = rN.next()
                    CP(ctx, "act", A2[:, :], psa[:, 0:128], [pab], [A2b])
                    if lvl < 5:
                        psb_, pbb = psr.next()
                        mm(ctx, psb_[:, 0:128], pbb, A_cur[:, :], AT_cur, True, True, [AT_curb, A_curb])
                        A2T, A2Tb = rN.next()
                        CP(ctx, "act", A2T[:, :], psb_[:, 0:128], [pbb], [A2Tb])
                    psc, pcb = psr.next()
                    mm(ctx, psc[:, 0:128], pcb, A2[:, :], RT[:, :], True, True, [A2b, RTb])
                    RT2, RT2b = rR.next()
                    TT(ctx, "dve", RT2[:, :], psc[:, 0:128], RT[:, :], ALU.add, [pcb, RTb], [RT2b])
                    RT, RTb = RT2, RT2b
                    A_cur, A_curb = A2, A2b
                    if lvl < 5:
                        AT_cur, AT_curb = A2T[:, :], A2Tb
                if rw_level == 6:
                    continue
                ps4, p4b = psr.next()
                mm(ctx, ps4[:, 0:64], p4b, AkkT, V_tok, True, True, [Ab, TKb])
                CP(ctx, "act", KX[:, 64:128], ps4[:, 0:64], [p4b], [KXb])
                ps5, p5b = psr.next()
                mm(ctx, ps5[:, 0:128], p5b, RT[:, :], KX[:, :], True, True, [RTb, KXb])
                WU, WUb = rWU.next()
                CP(ctx, "act", WU[:, :], ps5[:, 0:128], [p5b], [WUb])
                S64, S64b = rS64.next()
                RyT, PTs = S64[:, 0:128], S64[:, 128:192]
                ps6, p6b = psr.next()
                mm(ctx, ps6[0:64, 0:128], p6b, WU[:, 0:64], NArbT, True, False, [WUb, Ab])
                mm(ctx, ps6[0:64, 0:128], p6b, Rg_tok, ident_f, False, True, [TKb, cb])
                CP(ctx, "act", RyT, ps6[0:64, 0:128], [p6b], [S64b])
                ps7, p7b = psr.next()
                mm(ctx, ps7[0:64, 0:64], p7b, WU[:, 0:64], NBt_tok, True, True, [WUb, TKb])
                STT(ctx, "dve", PTs, ident_f[0:64, 0:64], eg[:, 127:128], ps7[0:64, 0:64], ALU.mult, ALU.add, [cb, Eb, p7b], [S64b])
                if rw_level == 7:
                    continue
                ps9, p9b = psacc.next()
                mm(ctx, ps9[:, 0:64], p9b, ArkT, V_tok, True, False, [Ab, TKb])
                mm(ctx, ps9[:, 0:64], p9b, NArbT, WU[:, 64:128], False, False, [Ab, WUb])
                mm(ctx, ps9[:, 0:64], p9b, RyT, M[:, :], False, True, [S64b, Mb])
                ps8, p8b = psacc.next()
                mm(ctx, ps8[0:64, 0:64], p8b, Kt_tok, V_tok, True, False, [TKb])
                mm(ctx, ps8[0:64, 0:64], p8b, NBt_tok, WU[:, 64:128], False, False, [TKb, WUb])
                mm(ctx, ps8[0:64, 0:64], p8b, PTs, M[:, :], False, True, [S64b, Mb])
                CP(ctx, "act", M[:, :], ps8[0:64, 0:64], [p8b], [Mb])
                if rw_level == 8:
                    continue
                s2, s2b = small.next()
                yt, ytb = rY.next()
                CP(ctx, "dve", yt[:, :], ps9[:, 0:64], [p9b], [ytb])
                ctx.op("dve", lambda e, o=s2[:, 8:14], i_=yt[:, :]: e.bn_stats(o, i_), reads=[ytb], writes=[s2b])
                ctx.op("dve", lambda e, o=s2[:, 16:18], i_=s2[:, 8:14]: e.bn_aggr(o, i_), reads=[s2b], writes=[s2b])
                A_(ctx, s2[:, 18:19], s2[:, 17:18], AF.Sqrt, [s2b], [s2b], bias=64e-5)
                ctx.op("dve", lambda e, o=s2[:, 18:19]: e.reciprocal(o, o), reads=[s2b], writes=[s2b])
                TS(ctx, "dve", yt[:, :], yt[:, :], s2[:, 16:17], s2[:, 18:19], ALU.subtract, ALU.mult, [ytb, s2b], [ytb])
                psy, pyb = psr.next()
                mm(ctx, psy[0:64, 0:128], pyb, yt[:, :], ident_f, True, True, [ytb, cb])
                ot, otb = outT.next()
                TS(ctx, "dve", ot[0:64, 0:128], psy[0:64, 0:128], PV("lng_%d" % i, 64), PV("lnb_%d" % i, 64), ALU.mult, ALU.add,
                   [pyb, cb], [otb])
                TT(ctx, "pool", ot[0:64, 0:128], ot[0:64, 0:128], bv[:, cs], ALU.add, [otb, bvb], [otb])
                TT(ctx, "pool", ot[0:64, 0:128], ot[0:64, 0:128], g_[:, cs], ALU.mult, [otb, g_b], [otb])
                ctx.dma("sp", hout_d[512 + i * 64:512 + (i + 1) * 64, t0 + j * 128:t0 + (j + 1) * 128], ot[0:64, 0:128], reads=[otb])

        fq, fqb = fQr.next()
        for pr_ in range(2):
            fm_proj(xt, xtb, "fq%d" % pr_,
                    lambda ps, pb, bias, pr_=pr_: A_(ctx, fq[:, pr_, :], ps, AF.Identity, [pb, cb], [fqb], bias=bias, scale=1.0))
            fm_proj(xt, xtb, "fk%d" % pr_,
                    lambda ps, pb, bias, pr_=pr_: A_(ctx, fK[:, pr_, t0:t0 + 512], ps, AF.Identity, [pb, cb], [fKb], bias=bias))

        for j in range(4):
            qb = blk * 4 + j
            pso, pob = psacc.next()
            for h in range(4):
                hs = slice((h % 2) * 64, (h % 2) * 64 + 64)
                psr_, prb = psr.next()
                mm(ctx, psr_[:, 0:1], prb, sel63, fG[:, qb, h:h + 1], True, True, [cb, fGb])
                sm, smb = small.next()
                CP(ctx, "dve", sm[:, 0:1], psr_[:, 0:1], [prb], [smb])
                fb, fbb = fbias.next()
                TS(ctx, "dve", fb[:, 0:qb + 1], fG[:, 0:qb + 1, h], sm[:, 0:1], None, ALU.subtract, None, [fGb, smb], [fbb])
                for kb in range(qb + 1):
                    ps, pb = psr.next()
                    mm(ctx, ps[:, 0:128], pb, fK[hs, h // 2, kb * 128:(kb + 1) * 128], fq[hs, h // 2, j * 128:(j + 1) * 128],
                       True, kb != qb, [fKb, fqb])
                    if kb == qb:
                        mm(ctx, ps[:, 0:128], pb, ident_b, mneg_b, False, True, [cb])
                    pt, ptb = PTr.next()
                    A_(ctx, pt[:, :], ps[:, 0:128], AF.Exp, [pb, fbb], [ptb], bias=fb[:, kb:kb + 1], scale=0.125)
                    mm(ctx, pso[:, h * 65:(h + 1) * 65], pob, pt[:, :], fV[:, kb, h * 65:(h + 1) * 65], kb == 0, kb == qb,
                       [ptb, fVb])
            sm, smb = small.next()
            ctx.op("dve", lambda e, o=sm[:, 0:4], i=pso[:, 0:260].rearrange("p (h c) -> p h c", c=65)[:, :, 64]: e.reciprocal(o, i),
                   reads=[pob], writes=[smb])
            hb, hbb = hbr.next()
            for h in range(4):
                TS(ctx, "dve", hb[:, h * 64:(h + 1) * 64], pso[:, h * 65:h * 65 + 64], sm[:, h:h + 1], None, ALU.mult, None,
                   [pob, smb], [hbb])
            pst, ptb2 = psr.next()
            for c in range(2):
                mm(ctx, pst[:, c * 128:(c + 1) * 128], ptb2, hb[:, c * 128:(c + 1) * 128], ident_f, True, True, [hbb, cb])
            ot, otb = outT.next()
            CP(ctx, "act", ot[:, 0:256], pst[:, 0:256], [ptb2], [otb])
            for c in range(2):
                ctx.dma("sp", hout_d[256 + c * 128:256 + (c + 1) * 128, qb * 128:(qb + 1) * 128], ot[:, c * 128:(c + 1) * 128],
                        reads=[otb])
    ctx.wait_all_dma("sp")
    ctx.emit()
    ctx.close()
    return nc, ctx


def _layout(vres):
    cols = [("m_qk", 512), ("m_v", 512), ("m_o", 512), ("m_i", 4), ("m_f", 4), ("f_q", 512), ("f_k", 512), ("f_v", 512),
            ("f_f", 8), ("gate", 3072), ("r_r", 512), ("r_k", 512), ("r_v", 512), ("r_w", 64), ("r_a", 64), ("r_g", 128)]
    if vres:
        cols.append(("r_vres", 32))
    lay, st = {}, 0
    for n, w in cols:
        lay[n] = st
        st += w
    return lay, st


def l1_consts():
    p = np.arange(128)
    ident = np.eye(128)
    ones = np.ones((128, 128))
    mLT = (p[:, None] <= p[None, :]).astype(np.float64)
    utri = mLT.copy()
    sel127 = np.zeros((128, 128)); sel127[127, :] = 1
    sel63 = np.zeros((128, 128)); sel63[63, :] = 1
    mneg = np.where(p[:, None] <= p[None, :], 0.0, -30000.0)
    bd = (p[:, None] // 64 == p[None, :] // 64).astype(np.float64)
    mSU = (p[:, None] < p[None, :]).astype(np.float64)
    mSL = (p[:, None] > p[None, :]).astype(np.float64)
    return np.concatenate([ident, ones, mLT, utri, sel127, sel63, mneg, bd, mSU, mSL], 1).astype(np.float32)


def pack_l1(inp, l, hh):
    vres = l > 0
    lay, ncol = _layout(vres)
    W = inp["w_in_%d" % l]
    Bv = inp["b_in_%d" % l]
    g = lambda n: inp["%s_%d" % (n, l)]
    mh = [2 * hh, 2 * hh + 1]
    fh = [4 * hh + i for i in range(4)]
    cols = {}
    for i, h in enumerate(mh):
        cols["mq%d" % i] = np.arange(lay["m_qk"] + h * 64, lay["m_qk"] + (h + 1) * 64)
        cols["mk%d" % i] = np.arange(lay["m_qk"] + 256 + h * 64, lay["m_qk"] + 256 + (h + 1) * 64)
        cols["mo%d" % i] = np.arange(lay["m_o"] + h * 128, lay["m_o"] + (h + 1) * 128)
    for pr in range(2):
        cols["fq%d" % pr] = np.arange(lay["f_q"] + fh[2 * pr] * 64, lay["f_q"] + (fh[2 * pr] + 2) * 64)
        cols["fk%d" % pr] = np.arange(lay["f_k"] + fh[2 * pr] * 64, lay["f_k"] + (fh[2 * pr] + 2) * 64)
    for i, h in enumerate(fh):
        cols["rr%d" % i] = np.arange(lay["r_r"] + h * 64, lay["r_r"] + (h + 1) * 64)
        cols["rk%d" % i] = np.arange(lay["r_k"] + h * 64, lay["r_k"] + (h + 1) * 64)
        cols["rv%d" % i] = np.arange(lay["r_v"] + h * 64, lay["r_v"] + (h + 1) * 64)
    cols["lw"] = np.arange(lay["r_w"], lay["r_w"] + 64)
    cols["la"] = np.arange(lay["r_a"], lay["r_a"] + 64)
    cols["lg"] = np.arange(lay["r_g"], lay["r_g"] + 128)
    cols["lv"] = np.arange(lay["r_vres"], lay["r_vres"] + 32) if vres else None
    wf = np.zeros((D, NF), np.float32)
    bfm = np.zeros((128, len(FGROUPS)), np.float32)
    for n, w in FGROUPS:
        off, _, gi = FOFF[n]
        if cols[n] is None:
            continue
        wf[:, off:off + w] = W[:, cols[n]]
        bfm[:w, gi] = Bv[cols[n]]
    tcols = np.concatenate([np.arange(lay["m_v"] + mh[0] * 128, lay["m_v"] + (mh[1] + 1) * 128),
                            np.arange(lay["f_v"] + fh[0] * 64, lay["f_v"] + (fh[3] + 1) * 64),
                            lay["m_i"] + np.array(mh), lay["m_f"] + np.array(mh), lay["f_f"] + np.array(fh)])
    wt = np.ascontiguousarray(W[:, tcols])
    btm = np.ascontiguousarray(Bv[tcols])[None, :]
    pv = np.zeros((128, len(PVN)), np.float32)
    r0 = lay["r_r"]
    mu = g("r_mu")
    mc = g("m_conv")
    for gname in ("mq0", "mq1", "mk0", "mk1"):
        for j in range(4):
            pv[:64, PVI["conv%d_%s" % (j, gname)]] = mc[j, cols[gname] - lay["m_qk"]]
    for i, h in enumerate(mh):
        pv[:128, PVI["mnorm%d" % i]] = g("m_norm")[h * 128:(h + 1) * 128]
    for i, h in enumerate(fh):
        sl = slice(h * 64, (h + 1) * 64)
        pv[:64, PVI["mu_r_%d" % i]] = mu[cols["rr%d" % i] - r0]
        pv[:64, PVI["mu_k_%d" % i]] = mu[cols["rk%d" % i] - r0]
        pv[:64, PVI["mu_v_%d" % i]] = mu[cols["rv%d" % i] - r0]
        pv[:64, PVI["wbias_%d" % i]] = g("r_wbias")[sl]
        pv[:64, PVI["abias_%d" % i]] = g("r_abias")[sl]
        if vres:
            pv[:64, PVI["vbias_%d" % i]] = g("r_vbias")[sl]
        pv[:64, PVI["kkw_%d" % i]] = g("r_kk")[sl]
        pv[:64, PVI["ka_%d" % i]] = g("r_ka")[sl]
        pv[:64, PVI["rk_%d" % i]] = g("r_rk")[sl]
        pv[:64, PVI["lng_%d" % i]] = g("r_ln_g")[sl]
        pv[:64, PVI["lnb_%d" % i]] = g("r_ln_b")[sl]
    pv[:64, PVI["mu_lw"]] = mu[cols["lw"] - r0]
    pv[:64, PVI["mu_la"]] = mu[cols["la"] - r0]
    pv[:128, PVI["mu_lg"]] = mu[cols["lg"] - r0]
    if vres:
        pv[:32, PVI["mu_lv"]] = mu[cols["lv"] - r0]
    lora = np.zeros((128, 4, 256), np.float32)
    csl = slice(fh[0] * 64, (fh[3] + 1) * 64)
    lora[:64, 0] = g("r_wB")[:, csl]
    lora[:64, 1] = g("r_aB")[:, csl]
    lora[:128, 2] = g("r_gB")[:, csl]
    if vres:
        lora[:32, 3] = g("r_vB")[:, csl]
    return dict(wf=wf, wt=wt, bfm=bfm, btm=btm, pv=pv, lora=lora, cst=l1_consts())


_PROGS = {}
SEQ = 8192
NBATCH = 4


def _prog(kind, **kw):
    key = (kind,) + tuple(sorted(kw.items()))
    if key not in _PROGS:
        if kind == "l1":
            _PROGS[key] = build_l1(T=SEQ, vres=kw["vres"])[0]
        else:
            _PROGS[key] = build_l2(kw["moe"], NT=SEQ // 2)[0]
    return _PROGS[key]


def run_l1(inp, l, cur):
    nc = _prog("l1", vres=l > 0)
    packs = [pack_l1(inp, l, hh) for hh in range(2)]
    xTs = [np.ascontiguousarray(cur[b].T) for b in range(NBATCH)]
    maps = []
    for c in range(NCORES):
        b, hh = c // 2, c % 2
        m = dict(packs[hh])
        m["xT"] = xTs[b]
        if l > 0:
            m["vfirst"] = inp["_vfirst"][c]
        maps.append(m)
    res = run_bass_kernel_spmd(nc, maps, core_ids=list(range(NCORES)))
    hT = []
    for b in range(NBATCH):
        o0, o1 = res.results[2 * b]["hout"], res.results[2 * b + 1]["hout"]
        hT.append(np.concatenate([o0[0:256], o1[0:256], o0[256:512], o1[256:512], o0[512:768], o1[512:768]], 0))
    vown = [np.ascontiguousarray(res.results[c]["vown"]) for c in range(NCORES)]
    return hT, vown


def run_l2(inp, l, cur, hT):
    moe = l % 2 == 1
    nc = _prog("l2", moe=moe)
    NT = SEQ // 2
    g = lambda n: inp["%s_%d" % (n, l)]
    lay, _ = _layout(l > 0)
    g0 = lay["gate"]
    W = g("w_in")
    shared = dict(
        cst=np.concatenate([np.eye(128), np.ones((128, 128))], 1).astype(np.float32),
        vecs=np.stack([inp["mem_ln_g"], inp["mem_ln_b"], g("ln1_g"), g("ln1_b"), g("ln2_g"), g("ln2_b"),
                       g("ln3_g"), g("ln3_b")]).astype(np.float32),
        wg=np.ascontiguousarray(W[:, g0:g0 + 3072]),
        bg=np.ascontiguousarray(g("b_in")[g0:g0 + 3072])[None, :],
        wup=np.concatenate([g("m_up"), g("f_up"), g("r_up")], 0),
        wout=g("w_out"), wq=g("x_wq"), wkv=g("x_wkv"), wo=g("x_wo"))
    if moe:
        shared.update(rw=g("ex_router"), rb=g("ex_router_b")[None, :], wgu=g("ex_wgu"), wd=g("ex_wd"))
    else:
        shared.update(wgu=g("ff_wgu"), wd=g("ff_wd"))
    maps = []
    for c in range(NCORES):
        b, half = c // 2, c % 2
        sl = slice(half * NT, (half + 1) * NT)
        m = dict(shared)
        m["x_tok"] = np.ascontiguousarray(cur[b, sl])
        m["xT"] = np.ascontiguousarray(cur[b, sl].T)
        m["hT"] = np.ascontiguousarray(hT[b][:, sl])
        m["mem"] = np.ascontiguousarray(inp["mem"][b])
        maps.append(m)
    res = run_bass_kernel_spmd(nc, maps, core_ids=list(range(NCORES)))
    out = np.empty((NBATCH, SEQ, D), np.float32)
    for c in range(NCORES):
        b, half = c // 2, c % 2
        out[b, half * NT:(half + 1) * NT] = res.results[c]["x3"]
    return out


def kernel(**inputs):
    inp = {k: np.asarray(v, dtype=np.float32) for k, v in inputs.items()}
    cur = inp["x"]
    for l in range(2):
        hT, vown = run_l1(inp, l, cur)
        if l == 0:
            inp["_vfirst"] = vown
        cur = run_l2(inp, l, cur, hT)
    return cur
```

```python
import numpy as np
import concourse.bass as bass
import concourse.mybir as mybir
from concourse.bass_utils import run_bass_kernel_spmd

F32 = mybir.dt.float32
BF16 = mybir.dt.bfloat16
AF = mybir.ActivationFunctionType
ALU = mybir.AluOpType
AX = mybir.AxisListType

D = 1024
NCORES = 8
ALPHA = (2.0 * 2) ** 0.25
LN_EPS = 1e-5


class Buf:
    __slots__ = ("name", "w", "r", "psum")

    def __init__(self, name="", psum=False):
        self.name = name
        self.psum = psum
        self.w = None
        self.r = {}


class Ctx:
    ENGS = ("pe", "act", "dve", "pool", "sp")
    NDMASEM = 24

    def __init__(self, nc):
        self.nc = nc
        self.q = {e: [] for e in self.ENGS}
        self.cnt = {e: 0 for e in self.ENGS}
        self.seen = {e: {} for e in self.ENGS}
        self.sems = {}
        self.dma_uses = [0] * self.NDMASEM
        self.dma_rr = 0
        self.dma_rr_sw = 0
        self._stack = []
        self.ninst = 0

    def enter(self, cm):
        v = cm.__enter__()
        self._stack.append(cm)
        return v

    def close(self):
        while self._stack:
            self._stack.pop().__exit__(None, None, None)

    def alloc_sems(self):
        for e in self.ENGS:
            self.sems[e] = self.enter(self.nc.semaphore("s_" + e))
        for i in range(self.NDMASEM):
            self.sems[("dma", i)] = self.enter(self.nc.semaphore("s_dma%d" % i))

    def sbuf(self, name, shape, dt):
        return self.enter(self.nc.sbuf_tensor(name, list(shape), dt))

    def psum(self, name, shape, dt=F32):
        return self.enter(self.nc.psum_tensor(name, list(shape), dt))

    def _need(self, eng, tok, waits):
        if tok is None:
            return
        key, val = tok
        if key == "pe" and eng == "pe":
            return
        if self.seen[eng].get(key, 0) >= val:
            return
        waits[key] = max(waits.get(key, 0), val)

    def _deps(self, eng, reads, writes):
        waits = {}
        for b in reads:
            self._need(eng, b.w, waits)
            if b.psum:
                for key, val in b.r.items():
                    if key != eng:
                        self._need(eng, (key, val), waits)
        for b in writes:
            self._need(eng, b.w, waits)
            for key, val in b.r.items():
                if key == eng and eng != "pool":
                    continue
                self._need(eng, (key, val), waits)
        for key, val in waits.items():
            self.seen[eng][key] = val
        return list(waits.items())

    def op(self, eng, fn, reads=(), writes=()):
        waits = self._deps(eng, reads, writes)
        self.cnt[eng] += 1
        tok = (eng, self.cnt[eng])
        for b in reads:
            if b.r.get(eng, 0) < tok[1]:
                b.r[eng] = tok[1]
        for b in writes:
            b.w = tok
            b.r = {}
        self.q[eng].append((waits, fn, (eng, 1)))
        self.ninst += 1
        return tok

    def dma(self, eng, out, in_, reads=(), writes=()):
        half = self.NDMASEM // 2
        if eng == "pool":
            s = half + self.dma_rr_sw
            self.dma_rr_sw = (self.dma_rr_sw + 1) % half
        else:
            s = self.dma_rr
            self.dma_rr = (self.dma_rr + 1) % half
        key = ("dma", s)
        waits = dict(self._deps(eng, reads, writes))
        prev = self.dma_uses[s] * 16
        if prev and self.seen[eng].get(key, 0) < prev:
            waits[key] = prev
            self.seen[eng][key] = prev
        self.dma_uses[s] += 1
        tok = (key, self.dma_uses[s] * 16)
        for b in reads:
            if b.r.get(key, 0) < tok[1]:
                b.r[key] = tok[1]
        for b in writes:
            b.w = tok
            b.r = {}
        self.q[eng].append((list(waits.items()), lambda e, o=out, i=in_: e.dma_start(out=o, in_=i), (key, 16)))
        self.ninst += 1
        return tok

    def wait_all_dma(self, eng):
        waits = []
        for s in range(self.NDMASEM):
            if self.dma_uses[s]:
                waits.append((("dma", s), self.dma_uses[s] * 16))
        self.q[eng].append((waits, None, None))

    def emit(self):
        nc = self.nc
        with nc.Block() as block:
            def run(e, name):
                for waits, fn, inc in self.q[name]:
                    for key, val in waits:
                        e.wait_ge(self.sems[key], val)
                    if fn is None:
                        continue
                    ins = fn(e)
                    ins.then_inc(self.sems[inc[0]], inc[1])

            @block.tensor
            def _(e):
                run(e, "pe")

            @block.scalar
            def _(e):
                run(e, "act")

            @block.vector
            def _(e):
                run(e, "dve")

            @block.gpsimd
            def _(e):
                run(e, "pool")

            @block.sync
            def _(e):
                run(e, "sp")


class Ring:
    def __init__(self, ctx, name, shape, dt, n, psum=False):
        self.tiles = []
        for i in range(n):
            t = ctx.psum("%s%d" % (name, i), shape, dt) if psum else ctx.sbuf("%s%d" % (name, i), shape, dt)
            self.tiles.append((t, Buf("%s%d" % (name, i), psum=psum)))
        self.i = 0

    def next(self):
        t = self.tiles[self.i]
        self.i = (self.i + 1) % len(self.tiles)
        return t


class WRing:
    SLOT = 4096

    def __init__(self, ctx, n, name="wr", dt=BF16, slot=None):
        self.ctx = ctx
        self.slot = slot or self.SLOT
        self.ring = Ring(ctx, name, [128, self.slot], dt, n)
        self.qi = 0

    def load(self, pieces, kc, ncols):
        t, b = self.ring.next()
        v = t[:, : kc * ncols].rearrange("p (k n) -> p k n", k=kc)
        for ap, off in pieces:
            w = ap.shape[1]
            src = ap.rearrange("(k p) n -> p k n", p=128)
            self.ctx.dma("pool", v[:, :, off:off + w], src, writes=[b])
        return v, b


def mm(ctx, ps, pb, lhsT, rhs, start, stop, reads):
    ctx.op("pe", lambda e: e.matmul(ps, lhsT, rhs, start=start, stop=stop), reads=reads, writes=[pb])


def layer_norm(ctx, z, zb, out, ob, gbc, bbc, pbuf, small, idx, F=1024):
    st, stb = small.next()
    nch = F // 512
    for c in range(nch):
        ctx.op("dve", lambda e, c=c: e.bn_stats(st[:, c * 6:(c + 1) * 6], z[:, c * 512:(c + 1) * 512]),
               reads=[zb], writes=[stb])
    ctx.op("dve", lambda e: e.bn_aggr(st[:, 16:18], st[:, 0:6 * nch]), reads=[stb], writes=[stb])
    A_(ctx, st[:, 18:19], st[:, 17:18], AF.Sqrt, [stb], [stb], bias=LN_EPS)
    ctx.op("dve", lambda e: e.reciprocal(st[:, 18:19], st[:, 18:19]), reads=[stb], writes=[stb])
    ctx.op("dve", lambda e: e.tensor_scalar(z, z, st[:, 16:17], st[:, 18:19], ALU.subtract, ALU.mult),
           reads=[zb, stb], writes=[zb])
    ctx.op("pool", lambda e: e.tensor_tensor(z, z, gbc, ALU.mult), reads=[zb, pbuf], writes=[zb])
    ctx.op("pool", lambda e: e.tensor_tensor(out, z, bbc, ALU.add), reads=[zb, pbuf], writes=[ob])


def transpose_to(ctx, psring, src_bf, srcb, dstT, dstb, ident_bf, cb, tok0, nk=8, evac=("act", "dve")):
    for h in range(nk // 4):
        ps, pb = psring.next()
        for q in range(4):
            k = h * 4 + q
            mm(ctx, ps[:, q * 128:(q + 1) * 128], pb, src_bf[:, k * 128:(k + 1) * 128], ident_bf, True, True,
               [srcb, cb])
        eng = evac[h % len(evac)]
        dst = dstT[:, h * 4:(h + 1) * 4, tok0:tok0 + 128]
        src = ps[:, :].rearrange("p (k t) -> p k t", k=4)
        if eng == "act":
            ctx.op("act", lambda e, d=dst, s=src: e.copy(d, s), reads=[pb], writes=[dstb])
        else:
            ctx.op("dve", lambda e, d=dst, s=src: e.tensor_copy(d, s), reads=[pb], writes=[dstb])


def A_(ctx, out, in_, func, reads, writes, bias=None, scale=None):
    kw = {}
    if bias is not None:
        kw["bias"] = bias
    if scale is not None:
        kw["scale"] = scale
    ctx.op("act", lambda e: e.activation(out, in_, func, **kw), reads=reads, writes=writes)


def TT(ctx, eng, out, in0, in1, op, reads, writes):
    ctx.op(eng, lambda e: e.tensor_tensor(out, in0, in1, op), reads=reads, writes=writes)


def TS(ctx, eng, out, in0, s1, s2, op0, op1, reads, writes):
    if s2 is None:
        ctx.op(eng, lambda e: e.tensor_scalar(out, in0, s1, None, op0), reads=reads, writes=writes)
    else:
        ctx.op(eng, lambda e: e.tensor_scalar(out, in0, s1, s2, op0, op1), reads=reads, writes=writes)


def STT(ctx, eng, out, in0, scalar, in1, op0, op1, reads, writes):
    ctx.op(eng, lambda e: e.scalar_tensor_tensor(out, in0, scalar, in1, op0, op1), reads=reads, writes=writes)


def CP(ctx, eng, out, in_, reads, writes):
    if eng == "act":
        ctx.op("act", lambda e: e.copy(out, in_), reads=reads, writes=writes)
    else:
        ctx.op(eng, lambda e: e.tensor_copy(out, in_), reads=reads, writes=writes)


def build_l2(moe, NT=4096, G=512, FF=None, NE=8):
    FF = FF or (3584 if moe else 2816)
    FC = FF // 128
    NG = NT // G
    nc = bass.Bass("TRN2", target_bir_lowering=False)
    ctx = Ctx(nc)

    def din(name, shape):
        return nc.dram_tensor(name, list(shape), F32, kind="ExternalInput").ap()

    x_tok = din("x_tok", [NT, D])
    xT_d = din("xT", [D, NT])
    hT_d = din("hT", [1536, NT])
    mem_d = din("mem", [256, D])
    cst_d = din("cst", [128, 256])
    vec_d = din("vecs", [8, D])
    wg_d = din("wg", [D, 3072])
    bg_d = din("bg", [1, 3072])
    wup_d = din("wup", [1536, D])
    wout_d = din("wout", [D, D])
    wq_d = din("wq", [D, D])
    wkv_d = din("wkv", [D, 2 * D])
    wo_d = din("wo", [D, D])
    if moe:
        rw_d = din("rw", [D, NE])
        rb_d = din("rb", [1, NE])
        wgu_d = din("wgu", [NE, D, 2 * FF])
        wd_d = din("wd", [NE, FF, D])
    else:
        wgu_d = din("wgu", [D, 2 * FF])
        wd_d = din("wd", [FF, D])
    out_d = nc.dram_tensor("x3", [NT, D], F32, kind="ExternalOutput").ap()

    ctx.alloc_sems()
    S = ctx.sbuf
    cst = S("cst_s", [128, 256], F32)
    cstbf = S("cstbf", [128, 256], BF16)
    cb = Buf("cst")
    ident_bf = cstbf[:, 0:128]
    ones_bf = cstbf[:, 128:256]
    ident_f = cst[:, 0:128]
    vecs = S("vecs_s", [128, 8, D], F32)
    bgbf = S("bgbf", [1, 3072], BF16)
    ctx.dma("sp", cst[:, :], cst_d, writes=[cb])
    ctx.dma("pool", cstbf[:, :], cst_d, writes=[cb])
    ctx.dma("sp", vecs[:, :, :].rearrange("p a d -> p (a d)"),
            vec_d.rearrange("a d -> (a d)").partition_broadcast(128), writes=[cb])
    ctx.dma("pool", bgbf[:, :], bg_d, writes=[cb])
    if moe:
        rw = S("rw_s", [128, 8, NE], F32)
        rb = S("rb_s", [1, NE], F32)
        ctx.dma("sp", rw[:, :, :], rw_d.rearrange("(k p) n -> p k n", p=128), writes=[cb])
        ctx.dma("sp", rb[:, :], rb_d, writes=[cb])

    psr = Ring(ctx, "ps", [128, 512], F32, 8, psum=True)
    wr = WRing(ctx, 5)
    small = Ring(ctx, "small", [128, 32], F32, 4)
    tmpr = Ring(ctx, "tmp", [128, 512], F32, 3)
    tmpb = Ring(ctx, "tmpb", [128, 1024], BF16, 2)

    memT = S("memT", [128, 8, 256], BF16)
    memTb = Buf("memT")
    kxT = S("kxT", [128, 8, 256], BF16)
    vx = S("vx", [128, 2, D], BF16)
    kvb = Buf("kv")
    zbuf = S("zbuf", [128, 4, D], F32); zbb = [Buf("z%d" % j) for j in range(4)]
    outr = Ring(ctx, "xo", [128, D], F32, 2)
    for mc in range(2):
        z, zb = zbuf[:, mc, :], zbb[mc]
        ctx.dma("sp", z[:, :], mem_d[mc * 128:(mc + 1) * 128, :], writes=[zb])
        o, ob = zbuf[:, 2 + mc, :], zbb[2 + mc]
        layer_norm(ctx, z[:, :], zb, o[:, :], ob, vecs[:, 0, :], vecs[:, 1, :], cb, small, 0)
        t, tb = tmpb.next()
        CP(ctx, "act", t[:, :], o[:, :], [ob], [tb])
        transpose_to(ctx, psr, t, tb, memT, memTb, ident_bf, cb, mc * 128)
    for s in range(4):
        w, wb = wr.load([(wkv_d[:, s * 512:(s + 1) * 512], 0)], 8, 512)
        if s < 2:
            for o4 in range(4):
                oc = s * 4 + o4
                ps, pb = psr.next()
                for k in range(8):
                    mm(ctx, ps[:, 0:256], pb, w[:, k, o4 * 128:(o4 + 1) * 128], memT[:, k, :], k == 0, k == 7,
                       [wb, memTb])
                CP(ctx, "dve", kxT[:, oc, :], ps[:, 0:256], [pb], [kvb])
        else:
            nb = s - 2
            for mc in range(2):
                ps, pb = psr.next()
                for k in range(8):
                    mm(ctx, ps[:, :], pb, memT[:, k, mc * 128:(mc + 1) * 128], w[:, k, :], k == 0, k == 7,
                       [wb, memTb])
                CP(ctx, "act", vx[:, mc, nb * 512:(nb + 1) * 512], ps[:, :], [pb], [kvb])

    xtok = S("xtok", [128, 4, D], F32); x1b = [Buf("x1%d" % j) for j in range(4)]
    big = S("big", [128, max(FC, 28), G], BF16); bigb = Buf("big")
    hT = big[:, 0:12, :]; qT = big[:, 12:20, :]; oT = big[:, 20:28, :]; hff = big[:, 0:FC, :]
    hTb = qTb = oTb = hfb = bigb
    merged = S("merged", [128, 4, D], F32); mgb = [Buf("mg%d" % j) for j in range(4)]
    x1 = xtok
    x2 = merged; x2b = mgb
    aT = S("aT", [128, 8, G], BF16); aTb = Buf("aT")
    xT = aT; xTb = aTb
    PT = Ring(ctx, "PT", [128, G], BF16, 4)
    if moe:
        yacc = zbuf; yb = zbb
        x2Tf = S("x2Tf", [128, 8, 128], F32); x2Tfb = Buf("x2Tf")
        comb = S("comb", [128, 4, NE], F32); combb = [Buf("comb%d" % j) for j in range(4)]

    def linear_tok(lhsT_tile, lhsT_buf, w_dram, K, ncols_total, consume, bias_row=None):
        KC = K // 128
        for nb in range(ncols_total // 512):
            w, wb = wr.load([(w_dram[:, nb * 512:(nb + 1) * 512], 0)], KC, 512)
            for j in range(4):
                ps, pb = psr.next()
                for k in range(KC):
                    mm(ctx, ps[:, :], pb, lhsT_tile[:, k, j * 128:(j + 1) * 128], w[:, k, :], k == 0,
                       k == KC - 1 and bias_row is None, [lhsT_buf, wb])
                if bias_row is not None:
                    mm(ctx, ps[:, :], pb, ones_bf[0:1, 0:128], bias_row[0:1, nb * 512:(nb + 1) * 512], False, True,
                       [cb])
                consume(j, nb, ps, pb)

    for g in range(NG):
        t0 = g * G
        ctx.dma("sp", xtok[:, :, :], x_tok[t0:t0 + G, :].rearrange("(j p) d -> p j d", p=128), writes=x1b)
        ctx.dma("pool", xT[:, :, :], xT_d.rearrange("(k p) t -> p k t", p=128)[:, :, t0:t0 + G], writes=[xTb])
        ctx.dma("pool", hT[:, :, :], hT_d.rearrange("(k p) t -> p k t", p=128)[:, :, t0:t0 + G], writes=[hTb])

        for nb in range(6):
            br, half = nb // 2, nb % 2
            wgs, wgb = wr.load([(wg_d[:, nb * 512:(nb + 1) * 512], 0)], 8, 512)
            wus, wub = wr.load([(wup_d[br * 512:(br + 1) * 512, half * 512:(half + 1) * 512], 0)], 4, 512)
            for j in range(4):
                psg, pgb = psr.next()
                for k in range(8):
                    mm(ctx, psg[:, :], pgb, xT[:, k, j * 128:(j + 1) * 128], wgs[:, k, :], k == 0, False, [xTb, wgb])
                mm(ctx, psg[:, :], pgb, ones_bf[0:1, 0:128], bgbf[0:1, nb * 512:(nb + 1) * 512], False, True, [cb])
                psu, pub = psr.next()
                for k in range(4):
                    mm(ctx, psu[:, :], pub, hT[:, br * 4 + k, j * 128:(j + 1) * 128], wus[:, k, :], k == 0, k == 3,
                       [hTb, wub])
                gt, gtb = tmpr.next()
                A_(ctx, gt[:, :], psg[:, :], AF.Sigmoid, [pgb], [gtb])
                dst = merged[:, j, half * 512:(half + 1) * 512]
                if br == 0:
                    TT(ctx, "dve", dst, gt[:, :], psu[:, :], ALU.mult, [gtb, pub], [mgb[j]])
                else:
                    TT(ctx, "dve", gt[:, :], gt[:, :], psu[:, :], ALU.mult, [gtb, pub], [gtb])
                    TT(ctx, "pool", dst, dst, gt[:, :], ALU.add, [gtb, mgb[j]], [mgb[j]])

        for j in range(4):
            t, tb = tmpb.next()
            CP(ctx, "act", t[:, :], merged[:, j, :], [mgb[j]], [tb])
            transpose_to(ctx, psr, t, tb, aT, aTb, ident_bf, cb, j * 128)
        zs = [(zbuf[:, j, :], zbb[j]) for j in range(4)]

        def cons1(j, nb, ps, pb):
            z, zb = zs[j]
            STT(ctx, "dve", z[:, nb * 512:(nb + 1) * 512], xtok[:, j, nb * 512:(nb + 1) * 512], ALPHA, ps[:, :],
                ALU.mult, ALU.add, [x1b[j], pb], [zb])
        linear_tok(aT, aTb, wout_d, D, D, cons1)
        for j in range(4):
            z, zb = zs[j]
            layer_norm(ctx, z[:, :], zb, x1[:, j, :], x1b[j], vecs[:, 2, :], vecs[:, 3, :], cb, small, 0)
            t, tb = tmpb.next()
            CP(ctx, "act", t[:, :], x1[:, j, :], [x1b[j]], [tb])
            transpose_to(ctx, psr, t, tb, aT, aTb, ident_bf, cb, j * 128)

        for s in range(2):
            w, wb = wr.load([(wq_d[:, s * 512:(s + 1) * 512], 0)], 8, 512)
            for o4 in range(4):
                oc = s * 4 + o4
                ps, pb = psr.next()
                for k in range(8):
                    mm(ctx, ps[:, :], pb, w[:, k, o4 * 128:(o4 + 1) * 128], aT[:, k, :], k == 0, k == 7, [wb, aTb])
                A_(ctx, qT[:, oc, :], ps[:, :], AF.Copy, [pb], [qTb], scale=1.0 / 16.0)
        for h in range(4):
            pts = []
            for mc in range(2):
                ps, pb = psr.next()
                for c in range(2):
                    mm(ctx, ps[:, :], pb, kxT[:, h * 2 + c, mc * 128:(mc + 1) * 128], qT[:, h * 2 + c, :], c == 0,
                       c == 1, [kvb, qTb])
                pt, ptb = PT.next()
                A_(ctx, pt[:, :], ps[:, :], AF.Exp, [pb], [ptb])
                pts.append((pt, ptb))
            psd, pdb = psr.next()
            for mc in range(2):
                mm(ctx, psd[:, :], pdb, ones_bf, pts[mc][0][:, :], mc == 0, mc == 1, [cb, pts[mc][1]])
            rd, rdb = tmpr.next()
            ctx.op("dve", lambda e, o=rd[:, :], i=psd[:, :]: e.reciprocal(o, i), reads=[pdb], writes=[rdb])
            for dc in range(2):
                pso, pob = psr.next()
                for mc in range(2):
                    mm(ctx, pso[:, :], pob, vx[:, mc, h * 256 + dc * 128:h * 256 + (dc + 1) * 128], pts[mc][0][:, :],
                       mc == 0, mc == 1, [kvb, pts[mc][1]])
                TT(ctx, "dve", oT[:, h * 2 + dc, :], pso[:, :], rd[:, :], ALU.mult, [pob, rdb], [oTb])
        zs = [(zbuf[:, j, :], zbb[j]) for j in range(4)]

        def cons2(j, nb, ps, pb):
            z, zb = zs[j]
            STT(ctx, "dve", z[:, nb * 512:(nb + 1) * 512], x1[:, j, nb * 512:(nb + 1) * 512], ALPHA, ps[:, :],
                ALU.mult, ALU.add, [x1b[j], pb], [zb])
        linear_tok(oT, oTb, wo_d, D, D, cons2)
        for j in range(4):
            z, zb = zs[j]
            layer_norm(ctx, z[:, :], zb, x2[:, j, :], x2b[j], vecs[:, 4, :], vecs[:, 5, :], cb, small, 0)
            t, tb = tmpb.next()
            CP(ctx, "act", t[:, :], x2[:, j, :], [x2b[j]], [tb])
            transpose_to(ctx, psr, t, tb, aT, aTb, ident_bf, cb, j * 128)
            if moe:
                for hh in range(2):
                    ps, pb = psr.next()
                    for q in range(4):
                        k = hh * 4 + q
                        mm(ctx, ps[:, q * 128:(q + 1) * 128], pb, x2[:, j, k * 128:(k + 1) * 128], ident_f, True, True,
                           [x2b[j], cb])
                    CP(ctx, "dve", x2Tf[:, hh * 4:(hh + 1) * 4, :], ps[:, :].rearrange("p (k t) -> p k t", k=4), [pb],
                       [x2Tfb])
                ps, pb = psr.next()
                for k in range(8):
                    mm(ctx, ps[:, 0:NE], pb, x2Tf[:, k, :], rw[:, k, :], k == 0, False, [x2Tfb, cb])
                mm(ctx, ps[:, 0:NE], pb, cst[0:1, 128:256], rb[0:1, :], False, True, [cb])
                sm, smb = small.next()
                lg = sm[:, 0:8]; m1 = sm[:, 8:9]; m2 = sm[:, 9:10]; k1 = sm[:, 10:18]; l2 = sm[:, 18:26]
                w1 = sm[:, 26:27]; w2 = sm[:, 27:28]
                CP(ctx, "dve", lg, ps[:, 0:NE], [pb], [smb])
                ctx.op("dve", lambda e, o=m1, i=lg: e.reduce_max(o, i, AX.X), reads=[smb], writes=[smb])
                TS(ctx, "dve", k1, lg, m1, None, ALU.is_equal, None, [smb], [smb])
                STT(ctx, "dve", l2, k1, -1e30, lg, ALU.mult, ALU.add, [smb], [smb])
                ctx.op("dve", lambda e, o=m2, i=l2: e.reduce_max(o, i, AX.X), reads=[smb], writes=[smb])
                TS(ctx, "dve", l2, l2, m2, None, ALU.is_equal, None, [smb], [smb])
                TT(ctx, "dve", w2, m2, m1, ALU.subtract, [smb], [smb])
                A_(ctx, w2, w2, AF.Exp, [smb], [smb])
                TS(ctx, "dve", w1, w2, 1.0, None, ALU.add, None, [smb], [smb])
                ctx.op("dve", lambda e, o=w1, i=w1: e.reciprocal(o, i), reads=[smb], writes=[smb])
                TT(ctx, "dve", w2, w2, w1, ALU.mult, [smb], [smb])
                TS(ctx, "dve", k1, k1, w1, None, ALU.mult, None, [smb], [smb])
                STT(ctx, "dve", comb[:, j, :], l2, w2, k1, ALU.mult, ALU.add, [smb], [combb[j]])

        for e_ in range(NE if moe else 1):
            wgu_e = wgu_d[e_] if moe else wgu_d
            wd_e = wd_d[e_] if moe else wd_d
            for fp in range(FC // 2):
                w, wb = wr.load([(wgu_e[:, fp * 256:(fp + 1) * 256], 0), (wgu_e[:, FF + fp * 256:FF + (fp + 1) * 256], 256)],
                                8, 512)
                for f2 in range(2):
                    fc = fp * 2 + f2
                    psg, pgb = psr.next()
                    for k in range(8):
                        mm(ctx, psg[:, :], pgb, w[:, k, f2 * 128:(f2 + 1) * 128], aT[:, k, :], k == 0, k == 7, [wb, aTb])
                    psu, pub = psr.next()
                    for k in range(8):
                        mm(ctx, psu[:, :], pub, w[:, k, 256 + f2 * 128:256 + (f2 + 1) * 128], aT[:, k, :], k == 0, k == 7,
                           [wb, aTb])
                    sg, sgb = tmpr.next()
                    A_(ctx, sg[:, :], psg[:, :], AF.Silu, [pgb], [sgb])
                    TT(ctx, "dve", hff[:, fc, :], sg[:, :], psu[:, :], ALU.mult, [sgb, pub], [hfb])
            parts = []
            k0 = 0
            while k0 < FC:
                parts.append((k0, min(8, FC - k0)))
                k0 += 8
            if not moe:
                zs = [(zbuf[:, j, :], zbb[j]) for j in range(4)]
            for nb in range(2):
                pss = [psr.next() for _ in range(4)]
                for pi, (k0, kn) in enumerate(parts):
                    w, wb = wr.load([(wd_e[k0 * 128:(k0 + kn) * 128, nb * 512:(nb + 1) * 512], 0)], kn, 512)
                    for j in range(4):
                        for k in range(kn):
                            mm(ctx, pss[j][0][:, :], pss[j][1], hff[:, k0 + k, j * 128:(j + 1) * 128], w[:, k, :],
                               k0 + k == 0, k0 + k == FC - 1, [hfb, wb])
                for j in range(4):
                    ps, pb = pss[j]
                    sl = slice(nb * 512, (nb + 1) * 512)
                    if not moe:
                        z, zb = zs[j]
                        STT(ctx, "dve", z[:, sl], x2[:, j, sl], ALPHA, ps[:, :], ALU.mult, ALU.add, [x2b[j], pb], [zb])
                    elif e_ == 0:
                        TS(ctx, "dve", yacc[:, j, sl], ps[:, :], comb[:, j, 0:1], None, ALU.mult, None,
                           [pb, combb[j]], [yb[j]])
                    else:
                        STT(ctx, "dve", yacc[:, j, sl], ps[:, :], comb[:, j, e_:e_ + 1], yacc[:, j, sl], ALU.mult, ALU.add,
                            [pb, combb[j], yb[j]], [yb[j]])
        if moe:
            zs = [(zbuf[:, j, :], zbb[j]) for j in range(4)]
            for j in range(4):
                z, zb = zs[j]
                STT(ctx, "dve", z[:, :], x2[:, j, :], ALPHA, yacc[:, j, :], ALU.mult, ALU.add, [x2b[j], yb[j]], [zb])
        for j in range(4):
            z, zb = zs[j]
            o, ob = outr.next()
            layer_norm(ctx, z[:, :], zb, o[:, :], ob, vecs[:, 6, :], vecs[:, 7, :], cb, small, 0)
            ctx.dma("sp", out_d[t0 + j * 128:t0 + (j + 1) * 128, :], o[:, :], reads=[ob])
    ctx.wait_all_dma("sp")
    ctx.emit()
    ctx.close()
    return nc, ctx


FGROUPS = ([("mq0", 64), ("mq1", 64), ("mk0", 64), ("mk1", 64), ("mo0", 128), ("mo1", 128),
            ("fq0", 128), ("fq1", 128), ("fk0", 128), ("fk1", 128)]
           + [("rr%d" % h, 64) for h in range(4)] + [("rk%d" % h, 64) for h in range(4)]
           + [("rv%d" % h, 64) for h in range(4)] + [("lw", 64), ("la", 64), ("lg", 128), ("lv", 32)])
FOFF = {}
_o = 0
for _n, _w in FGROUPS:
    FOFF[_n] = (_o, _w, len(FOFF))
    _o += _w
NF = _o
NTM = 520
PVN = (["conv%d_%s" % (j, g) for g in ("mq0", "mq1", "mk0", "mk1") for j in range(4)]
       + ["mnorm0", "mnorm1"]
       + ["%s_%d" % (n, h) for n in ("mu_r", "mu_k", "mu_v", "wbias", "abias", "vbias", "kkw", "ka", "rk", "lng", "lnb")
          for h in range(4)]
       + ["mu_lw", "mu_la", "mu_lg", "mu_lv"])
PVI = {n: i for i, n in enumerate(PVN)}


def build_l1(T=8192, vres=False, rw_level=2):
    NB = T // 512
    NQ = T // 128
    nc = bass.Bass("TRN2", target_bir_lowering=False)
    ctx = Ctx(nc)

    def din(name, shape):
        return nc.dram_tensor(name, list(shape), F32, kind="ExternalInput").ap()

    xT_d = din("xT", [D, T])
    wf_d = din("wf", [D, NF])
    wt_d = din("wt", [D, NTM])
    bf_d = din("bfm", [128, len(FGROUPS)])
    bt_d = din("btm", [1, NTM])
    pv_d = din("pv", [128, len(PVN)])
    cst_d = din("cst", [128, 1280])
    lora_d = din("lora", [128, 4, 256])
    if vres:
        vf_d = din("vfirst", [256, T])
    hout_d = nc.dram_tensor("hout", [768, T], F32, kind="ExternalOutput").ap()
    vown_d = nc.dram_tensor("vown", [256, T], F32, kind="ExternalOutput").ap()

    ctx.alloc_sems()
    S = ctx.sbuf
    cb = Buf("const")
    cst = S("cst_s", [128, 1280], F32)
    cstb = S("cst_b", [128, 1024], BF16)
    ctx.dma("sp", cst[:, :], cst_d, writes=[cb])
    ctx.dma("pool", cstb[:, :], cst_d[:, 0:1024], writes=[cb])
    mSU, mSL = cst[:, 1024:1152], cst[:, 1152:1280]
    ident_f, ones_f, mLT, utri, sel127, sel63, mneg, bdiag = [cst[:, i * 128:(i + 1) * 128] for i in range(8)]
    ident_b, ones_b = cstb[:, 0:128], cstb[:, 128:256]
    mneg_b = cstb[:, 768:896]
    wt = S("wt_s", [128, 8, NTM], BF16)
    wfr = WRing(ctx, 6, name="wfr", slot=1024)
    ctx.dma("pool", wt[:, :, :], wt_d.rearrange("(k p) n -> p k n", p=128), writes=[cb])
    bfm = S("bfm_s", [128, len(FGROUPS)], F32)
    btm = S("btm_s", [1, NTM], F32)
    pv = S("pv_s", [128, len(PVN)], F32)
    lora = S("lora_s", [128, 4, 256], F32)
    ctx.dma("sp", bfm[:, :], bf_d, writes=[cb])
    ctx.dma("sp", btm[:, :], bt_d, writes=[cb])
    ctx.dma("sp", pv[:, :], pv_d, writes=[cb])
    ctx.dma("sp", lora[:, :, :], lora_d, writes=[cb])

    def PV(name, w=128):
        i = PVI[name]
        return pv[0:w, i:i + 1]

    psr = Ring(ctx, "ps", [128, 512], F32, 5, psum=True)
    foxacc = (ctx.psum("pa_fox", [128, 512], F32), Buf("pa_fox", psum=True))
    mlacc = (ctx.psum("pa_ml", [128, 512], F32), Buf("pa_ml", psum=True))
    rwacc = (ctx.psum("pa_rw", [128, 512], F32), Buf("pa_rw", psum=True))
    xTr = Ring(ctx, "xTb", [128, 8, 512], BF16, 1)
    small = Ring(ctx, "small", [128, 64], F32, 4)
    smallR = Ring(ctx, "smallR", [128, 64], F32, 4)
    smallF = Ring(ctx, "smallF", [128, 64], F32, 4)

    fK = S("fK", [128, 2, T], BF16); fKb = Buf("fK")
    fV = S("fV", [128, NQ, 4 * 65], BF16); fVb = Buf("fV")
    fG = S("fG", [128, NQ, 4], F32); fGb = Buf("fG")
    ctx.op("pool", lambda e: e.memset(fV[:, :, :], 1.0), writes=[fVb])
    fQr = Ring(ctx, "fQ", [128, 2, 512], BF16, 2)
    PTr = Ring(ctx, "PT", [128, 128], BF16, 4)
    fbias = Ring(ctx, "fbias", [128, NQ], F32, 3)
    hbr = Ring(ctx, "hb", [128, 256], F32, 2)
    outT = Ring(ctx, "outT", [128, 256], F32, 3)

    LN8 = float(np.log(0.125))
    mU = [S("mU%d" % i, [64, 515], F32) for i in range(4)]; mUb = [Buf("mU%d" % i) for i in range(4)]
    for i in range(4):
        ctx.op("pool", lambda e, i=i: e.memset(mU[i][:, 0:3], 0.0), writes=[mUb[i]])
    mQK = [S("mQK%d" % i, [64, 512], F32) for i in range(4)]; mQKb = [Buf("mQK%d" % i) for i in range(4)]
    mVa = S("mVa", [128, 4, 2, 129], F32); mVab = [Buf("mVa%d" % j) for j in range(4)]
    ctx.op("pool", lambda e: e.memset(mVa[:, :, :, :].rearrange("p a b c -> p (a b c)"), 1.0), writes=mVab)
    mC = [S("mC%d" % i, [64, 129], F32) for i in range(2)]; mCb = [Buf("mC%d" % i) for i in range(2)]
    for i in range(2):
        ctx.op("pool", lambda e, i=i: e.memset(mC[i][:, :], 0.0), writes=[mCb[i]])
    mSig = [S("mSig%d" % i, [128, 512], F32) for i in range(2)]; mSigb = [Buf("mSig%d" % i) for i in range(2)]
    mWT = Ring(ctx, "mWT", [128, 128], F32, 2)
    mKg = Ring(ctx, "mKg", [128, 64], F32, 2)
    mH = Ring(ctx, "mH", [128, 128], F32, 2)
    mSc = Ring(ctx, "mSc", [128, 32], F32, 4)

    RG = [n for n, _ in FGROUPS if n[0] == "r" or n in ("lw", "la", "lg", "lv")]
    rHalo = S("rHalo", [128, len(RG)], F32); rHb = Buf("rHalo")
    ctx.op("pool", lambda e: e.memset(rHalo[:, :], 0.0), writes=[rHb])
    rP = Ring(ctx, "rP", [128, 513], F32, 3)
    rLo = [S("rLo%d" % i, [128, 512], F32) for i in range(4)]; rLob = [Buf("rLo%d" % i) for i in range(4)]
    NRX = 10
    rX = [[S("rX%d_%d" % (s_, i), [64, 512], F32) for i in range(NRX)] for s_ in range(1)]
    rXb = [[Buf("rX%d_%d" % (s_, i)) for i in range(NRX)] for s_ in range(1)]
    rM = [S("rM%d" % i, [64, 64], F32) for i in range(4)]; rMb = [Buf("rM%d" % i) for i in range(4)]
    for i in range(4):
        ctx.op("pool", lambda e, i=i: e.memset(rM[i][:, :], 0.0), writes=[rMb[i]])
    rE = Ring(ctx, "rE", [64, 5, 128], F32, 1)
    rF = Ring(ctx, "rF", [64, 6, 128], F32, 1)
    rTK = Ring(ctx, "rTK", [128, 256], F32, 2)
    rKX = Ring(ctx, "rKX", [128, 128], F32, 2)
    rA = Ring(ctx, "rA", [128, 4, 128], F32, 1)
    rN = Ring(ctx, "rN", [128, 128], F32, 6)
    rR = Ring(ctx, "rR", [128, 128], F32, 3)
    rWU = Ring(ctx, "rWU", [128, 128], F32, 2)
    rS64 = Ring(ctx, "rS64", [64, 256], F32, 3)
    rY = Ring(ctx, "rY", [128, 64], F32, 2)
    SIGC = 0.6065306597126334

    def fm_proj(xt, xtb, name, evac):
        off, w, gi = FOFF[name]
        wv, wvb = wfr.load([(wf_d[:, off:off + w], 0)], 8, w)
        ps, pb = psr.next()
        for k in range(8):
            mm(ctx, ps[0:w, :], pb, wv[:, k, :], xt[:, k, :], k == 0, k == 7, [wvb, xtb])
        evac(ps[0:w, :], pb, bfm[0:w, gi:gi + 1])

    for blk in range(NB):
        t0 = blk * 512
        xt, xtb = xTr.next()
        ctx.dma("pool", xt[:, :, :], xT_d.rearrange("(k p) t -> p k t", p=128)[:, :, t0:t0 + 512], writes=[xtb])

        tmv = []
        for j in range(4):
            ps, pb = psr.next()
            for k in range(8):
                mm(ctx, ps[:, :], pb, xt[:, k, j * 128:(j + 1) * 128], wt[:, k, 0:512], k == 0, False, [xtb, cb])
            mm(ctx, ps[:, :], pb, ones_f[0:1, 0:128], btm[0:1, 0:512], False, True, [cb])
            ps2, pb2 = psr.next()
            for k in range(8):
                mm(ctx, ps2[:, 0:8], pb2, xt[:, k, j * 128:(j + 1) * 128], wt[:, k, 512:520], k == 0, False, [xtb, cb])
            mm(ctx, ps2[:, 0:8], pb2, ones_f[0:1, 0:128], btm[0:1, 512:520], False, True, [cb])
            qb = blk * 4 + j
            CP(ctx, "act", fV[:, qb, :].rearrange("p (h c) -> p h c", c=65)[:, :, 0:64],
               ps[:, 256:512].rearrange("p (h c) -> p h c", c=64), [pb], [fVb])
            sm, smb = small.next()
            A_(ctx, sm[:, 0:8], ps2[:, 0:8], AF.Exp, [pb2], [smb], scale=-1.0)
            A_(ctx, sm[:, 8:16], sm[:, 0:8], AF.Ln, [smb], [smb], bias=1.0)
            psg, pgb = psr.next()
            mm(ctx, psg[:, 0:4], pgb, utri, sm[:, 12:16], True, qb == 0, [cb, smb])
            if qb > 0:
                mm(ctx, psg[:, 0:4], pgb, sel127, fG[:, qb - 1, :], False, True, [cb, fGb])
            CP(ctx, "dve", fG[:, qb, :], psg[:, 0:4], [pgb], [fGb])
            CP(ctx, "dve", mVa[:, j, :, 0:128], ps[:, 0:256].rearrange("p (h c) -> p h c", c=128), [pb], [mVab[j]])
            sc, scb = mSc.next()
            psm, pmb = psr.next()
            mm(ctx, psm[:, 0:2], pmb, utri, sm[:, 10:12], True, True, [cb, smb])
            mm(ctx, psm[:, 2:4], pmb, ones_f, sm[:, 10:12], True, True, [cb, smb])
            CP(ctx, "dve", sc[:, 0:4], psm[:, 0:4], [pmb], [scb])
            TT(ctx, "dve", sc[:, 12:14], ps2[:, 0:2], sc[:, 0:2], ALU.add, [pb2, scb], [scb])
            A_(ctx, sc[:, 4:6], sc[:, 12:14], AF.Exp, [scb], [scb], bias=LN8)
            A_(ctx, sc[:, 6:8], sc[:, 0:2], AF.Exp, [scb], [scb], scale=-1.0)
            TT(ctx, "dve", sc[:, 12:14], sc[:, 12:14], sc[:, 2:4], ALU.subtract, [scb], [scb])
            A_(ctx, sc[:, 8:10], sc[:, 12:14], AF.Exp, [scb], [scb], bias=LN8)
            A_(ctx, sc[:, 10:12], sc[:, 2:4], AF.Exp, [scb], [scb], scale=-1.0)
            tmv.append((sc, scb))

        for gi_, gname in enumerate(("mq0", "mq1", "mk0", "mk1")):
            U, Ub = mU[gi_], mUb[gi_]
            fm_proj(xt, xtb, gname, lambda ps, pb, bias, U=U, Ub=Ub: A_(ctx, U[:, 3:515], ps, AF.Identity, [pb, cb], [Ub], bias=bias))
            q_, qb_ = mQK[gi_], mQKb[gi_]
            TS(ctx, "dve", q_[:, :], U[:, 0:512], PV("conv0_" + gname, 64), None, ALU.mult, None, [Ub, cb], [qb_])
            for jj in range(1, 4):
                STT(ctx, "dve", q_[:, :], U[:, jj:jj + 512], PV("conv%d_%s" % (jj, gname), 64), q_[:, :], ALU.mult, ALU.add,
                    [Ub, cb, qb_], [qb_])
            A_(ctx, q_[:, :], q_[:, :], AF.Silu, [qb_], [qb_])
            CP(ctx, "pool", U[:, 0:3], U[:, 512:515], [Ub], [Ub])
        for i in range(2):
            fm_proj(xt, xtb, "mo%d" % i, lambda ps, pb, bias, i=i: A_(ctx, mSig[i][:, :], ps, AF.Sigmoid, [pb, cb], [mSigb[i]], bias=bias))
        def gen_mlstm(tmv=tmv, t0=t0):
          for j in range(4):
            sc, scb = tmv[j]
            cs = slice(j * 128, (j + 1) * 128)
            for i in range(2):
                qT, qTb_, kT, kTb_ = mQK[i], mQKb[i], mQK[2 + i], mQKb[2 + i]
                psG, pGb = psr.next()
                mm(ctx, psG[:, 0:128], pGb, kT[:, cs], qT[:, cs], True, True, [kTb_, qTb_])
                wt_, wtb = mWT.next()
                STT(ctx, "dve", wt_[:, :], psG[:, 0:128], sc[:, 4 + i:5 + i], mLT, ALU.mult, ALU.mult, [pGb, scb, cb], [wtb])
                yield
                psN, pNb = mlacc
                mm(ctx, psN[:, 0:129], pNb, wt_[:, :], mVa[:, j, i, :], True, False, [wtb, mVab[j]])
                mm(ctx, psN[:, 0:129], pNb, qT[:, cs], mC[i][:, :], False, True, [qTb_, mCb[i]])
                psK, pKb = psr.next()
                mm(ctx, psK[:, 0:64], pKb, kT[:, cs], ident_f[0:64, 0:64], True, True, [kTb_, cb])
                kg, kgb = mKg.next()
                TS(ctx, "dve", kg[:, :], psK[:, 0:64], sc[:, 8 + i:9 + i], None, ALU.mult, None, [pKb, scb], [kgb])
                yield
                psC, pCb = psr.next()
                mm(ctx, psC[0:64, 0:129], pCb, kg[:, :], mVa[:, j, i, :], True, True, [kgb, mVab[j]])
                STT(ctx, "dve", mC[i][:, :], mC[i][:, :], sc[0:64, 10 + i:11 + i], psC[0:64, 0:129], ALU.mult, ALU.add,
                    [mCb[i], scb, pCb], [mCb[i]])
                yield
                s2, s2b = small.next()
                TT(ctx, "dve", s2[:, 0:1], psN[:, 128:129], sc[:, 6 + i:7 + i], ALU.mult, [pNb, scb], [s2b])
                A_(ctx, s2[:, 0:1], s2[:, 0:1], AF.Abs, [s2b], [s2b])
                TS(ctx, "dve", s2[:, 0:1], s2[:, 0:1], 1.0, None, ALU.max, None, [s2b], [s2b])
                ctx.op("dve", lambda e, o=s2[:, 1:2], i_=s2[:, 0:1]: e.reciprocal(o, i_), reads=[s2b], writes=[s2b])
                TT(ctx, "dve", s2[:, 1:2], s2[:, 1:2], sc[:, 6 + i:7 + i], ALU.mult, [s2b, scb], [s2b])
                hh_, hhb = mH.next()
                TS(ctx, "dve", hh_[:, :], psN[:, 0:128], s2[:, 1:2], None, ALU.mult, None, [pNb, s2b], [hhb])
                ctx.op("dve", lambda e, o=s2[:, 8:14], i_=hh_[:, :]: e.bn_stats(o, i_), reads=[hhb], writes=[s2b])
                ctx.op("dve", lambda e, o=s2[:, 16:18], i_=s2[:, 8:14]: e.bn_aggr(o, i_), reads=[s2b], writes=[s2b])
                A_(ctx, s2[:, 18:19], s2[:, 17:18], AF.Sqrt, [s2b], [s2b], bias=1e-6)
                ctx.op("dve", lambda e, o=s2[:, 18:19]: e.reciprocal(o, o), reads=[s2b], writes=[s2b])
                TS(ctx, "dve", hh_[:, :], hh_[:, :], s2[:, 16:17], s2[:, 18:19], ALU.subtract, ALU.mult, [hhb, s2b], [hhb])
                psT, pTb = psr.next()
                mm(ctx, psT[:, 0:128], pTb, hh_[:, :], ident_f, True, True, [hhb, cb])
                ot, otb = outT.next()
                STT(ctx, "dve", ot[:, 0:128], psT[:, 0:128], PV("mnorm%d" % i), mSig[i][:, cs], ALU.mult, ALU.mult,
                    [pTb, cb, mSigb[i]], [otb])
                ctx.dma("sp", hout_d[i * 128:(i + 1) * 128, t0 + j * 128:t0 + (j + 1) * 128], ot[:, 0:128], reads=[otb])
                yield

        def lerp_group(name, dst, dstb, post=None):
            off, w, gi = FOFF[name]
            hi = RG.index(name)
            P, Pb = rP.next()
            CP(ctx, "pool", P[0:w, 0:1], rHalo[0:w, hi:hi + 1], [rHb], [Pb])
            fm_proj(xt, xtb, name, lambda ps, pb, bias: A_(ctx, P[0:w, 1:513], ps, AF.Identity, [pb, cb], [Pb], bias=bias))
            CP(ctx, "pool", rHalo[0:w, hi:hi + 1], P[0:w, 512:513], [Pb], [rHb])
            Dd, Ddb = rP.next()
            TT(ctx, "pool", Dd[0:w, 0:512], P[0:w, 0:512], P[0:w, 1:513], ALU.subtract, [Pb], [Ddb])
            muname = {"lw": "mu_lw", "la": "mu_la", "lg": "mu_lg", "lv": "mu_lv"}.get(name) or "mu_%s_%s" % (name[1], name[2])
            STT(ctx, "dve", dst, Dd[0:w, 0:512], PV(muname, w), P[0:w, 1:513], ALU.mult, ALU.add, [Pb, Ddb, cb], [dstb])
            if post is not None:
                A_(ctx, dst, dst, post, [dstb], [dstb])

        def gen_rwkv(t0=t0, lerp_group=lerp_group):
          lerp_group("lw", rLo[0][0:64, :], rLob[0], AF.Tanh)
          lerp_group("la", rLo[1][0:64, :], rLob[1])
          yield
          lerp_group("lg", rLo[2][0:128, :], rLob[2], AF.Sigmoid)
          if vres:
            lerp_group("lv", rLo[3][0:32, :], rLob[3])
          yield
          for i in range(4 if rw_level >= 1 else 0):
              X, Xb = rX[0], rXb[0]
              r_, k_, v_, nlw, a_, g_, kk, k2, bb, t1 = [X[q][:, :] for q in range(NRX)]
              r_b, k_b, v_b, nlwb, a_b, g_b, kkb, k2b, bbb, t1b = Xb
              bv, bvb = k_, k_b
              t2, t2b = kk, kkb
              lerp_group("rr%d" % i, r_, r_b)
              yield
              lerp_group("rk%d" % i, k_, k_b)
              yield
              lerp_group("rv%d" % i, v_, v_b)
              yield
              ctx.dma("sp", vown_d[i * 64:(i + 1) * 64, t0:t0 + 512], v_, reads=[v_b])
              ic = slice(i * 64, (i + 1) * 64)
              ps, pb = psr.next()
              mm(ctx, ps[0:64, :], pb, lora[0:64, 0, ic], rLo[0][0:64, :], True, True, [cb, rLob[0]])
              A_(ctx, nlw, ps[0:64, :], AF.Sigmoid, [pb, cb], [nlwb], bias=PV("wbias_%d" % i, 64))
              TS(ctx, "pool", nlw, nlw, SIGC, None, ALU.mult, None, [nlwb], [nlwb])
              ps, pb = psr.next()
              mm(ctx, ps[0:64, :], pb, lora[0:64, 1, ic], rLo[1][0:64, :], True, True, [cb, rLob[1]])
              A_(ctx, a_, ps[0:64, :], AF.Sigmoid, [pb, cb], [a_b], bias=PV("abias_%d" % i, 64))
              ps, pb = psr.next()
              mm(ctx, ps[0:64, :], pb, lora[0:128, 2, ic], rLo[2][0:128, :], True, True, [cb, rLob[2]])
              CP(ctx, "act", g_, ps[0:64, :], [pb], [g_b])
              yield
              if vres:
                  ps, pb = psr.next()
                  mm(ctx, ps[0:64, :], pb, lora[0:32, 3, ic], rLo[3][0:32, :], True, True, [cb, rLob[3]])
                  A_(ctx, t1, ps[0:64, :], AF.Sigmoid, [pb, cb], [t1b], bias=PV("vbias_%d" % i, 64))
                  ctx.dma("sp", t2, vf_d[i * 64:(i + 1) * 64, t0:t0 + 512], writes=[t2b])
                  TT(ctx, "pool", t2, t2, v_, ALU.subtract, [t2b, v_b], [t2b])
                  TT(ctx, "pool", t2, t2, t1, ALU.mult, [t2b, t1b], [t2b])
                  TT(ctx, "pool", v_, v_, t2, ALU.add, [v_b, t2b], [v_b])
              TS(ctx, "pool", kk, k_, PV("kkw_%d" % i, 64), None, ALU.mult, None, [k_b, cb], [kkb])
              TT(ctx, "pool", t1, kk, kk, ALU.mult, [kkb], [t1b])
              ps, pb = psr.next()
              mm(ctx, ps[0:64, :], pb, ones_f[0:64, 0:64], t1, True, True, [cb, t1b])
              A_(ctx, t1, ps[0:64, :], AF.Sqrt, [pb], [t1b])
              TS(ctx, "dve", t1, t1, 1e-12, None, ALU.max, None, [t1b], [t1b])
              ctx.op("dve", lambda e, o=t1: e.reciprocal(o, o), reads=[t1b], writes=[t1b])
              TT(ctx, "pool", kk, kk, t1, ALU.mult, [kkb, t1b], [kkb])
              yield
              TS(ctx, "dve", t1, a_, 1.0, PV("ka_%d" % i, 64), ALU.subtract, ALU.mult, [a_b, cb], [t1b])
              STT(ctx, "dve", k2, t1, 1.0, k_, ALU.add, ALU.mult, [t1b, k_b], [k2b])
              TT(ctx, "pool", bb, kk, a_, ALU.mult, [kkb, a_b], [bbb])
              STT(ctx, "dve", t1, r_, PV("rk_%d" % i, 64), k2, ALU.mult, ALU.mult, [r_b, cb, k2b], [t1b])
              ps, pb = psr.next()
              mm(ctx, ps[0:64, :], pb, ones_f[0:64, 0:64], t1, True, True, [cb, t1b])
              TT(ctx, "dve", bv, ps[0:64, :], v_, ALU.mult, [pb, v_b], [bvb])
              yield
              M, Mb = rM[i], rMb[i]
              for j in range(4 if rw_level >= 2 else 0):
                  cs = slice(j * 128, (j + 1) * 128)
                  E, Eb = rE.next()
                  ncl, eg, egm, ei, egl = [E[:, q, :] for q in range(5)]
                  ctx.op("dve", lambda e, o=ncl, d1=nlw[:, cs]: e.tensor_tensor_scan(o, ones_f[0:64, 0:128], d1, 0.0, ALU.mult, ALU.add),
                         reads=[cb, nlwb], writes=[Eb])
                  sm, smb = smallR.next()
                  TS(ctx, "pool", sm[0:64, 0:1], ncl[:, 127:128], -1.0, None, ALU.mult, None, [Eb], [smb])
                  TT(ctx, "pool", egm, nlw[:, cs], ncl, ALU.subtract, [nlwb, Eb], [Eb])
                  A_(ctx, eg, ncl, AF.Exp, [Eb], [Eb], scale=-1.0)
                  A_(ctx, egm, egm, AF.Exp, [Eb], [Eb])
                  A_(ctx, ei, ncl, AF.Exp, [Eb], [Eb])
                  A_(ctx, egl, ncl, AF.Exp, [Eb, smb], [Eb], bias=sm[0:64, 0:1])
                  Fm, Fb = rF.next()
                  KR = Fm[:, 0:2, :]
                  BiT, KiT, KtT, NBtT = [Fm[:, q, :] for q in range(2, 6)]
                  TT(ctx, "pool", Fm[:, 0, :], kk[:, cs], egm, ALU.mult, [kkb, Eb], [Fb])
                  TT(ctx, "pool", Fm[:, 1, :], r_[:, cs], eg, ALU.mult, [r_b, Eb], [Fb])
                  TT(ctx, "pool", BiT, bb[:, cs], ei, ALU.mult, [bbb, Eb], [Fb])
                  TT(ctx, "pool", KiT, k2[:, cs], ei, ALU.mult, [k2b, Eb], [Fb])
                  TT(ctx, "pool", KtT, k2[:, cs], egl, ALU.mult, [k2b, Eb], [Fb])
                  STT(ctx, "dve", NBtT, bb[:, cs], -1.0, egl, ALU.mult, ALU.mult, [bbb, Eb], [Fb])
                  yield
                  if rw_level == 3:
                      continue
                  psT, pTb = psr.next()
                  SUB = 9
                  for q, src, srcb in ((0, Fm[:, 0, :], Fb), (1, Fm[:, 1, :], Fb), (2, v_[:, cs], v_b), (3, KtT, Fb), (4, NBtT, Fb)):
                      if SUB == 1 and q >= 2:
                          continue
                      mm(ctx, psT[:, q * 64:(q + 1) * 64], pTb, src, ident_f[0:64, 0:64], True, True, [srcb, cb])
                  TK, TKb = rTK.next()
                  KX, KXb = rKX.next()
                  if SUB <= 2:
                      continue
                  CP(ctx, "act", TK[:, :], psT[:, 64:320], [pTb], [TKb])
                  if SUB <= 3:
                      continue
                  CP(ctx, "dve", KX[:, 0:64], psT[:, 0:64], [pTb], [KXb])
                  yield
                  Rg_tok, V_tok, Kt_tok, NBt_tok = [TK[:, q * 64:(q + 1) * 64] for q in range(4)]
                  if rw_level == 4:
                      continue
                  Am, Ab = rA.next()
                  NT_, NArbT, AkkT, ArkT = [Am[:, q, :] for q in range(4)]
                  KRf = KR.rearrange("p a t -> p (a t)")
                  ps1, p1b = psr.next()
                  mm(ctx, ps1[:, 0:256], p1b, BiT, KRf, True, True, [Fb])
                  STT(ctx, "dve", NT_, ps1[:, 0:128], -1.0, mSU, ALU.mult, ALU.mult, [p1b, cb], [Ab])
                  STT(ctx, "dve", NArbT, ps1[:, 128:256], -1.0, mLT, ALU.mult, ALU.mult, [p1b, cb], [Ab])
                  ps2_, p2b = psr.next()
                  mm(ctx, ps2_[:, 0:256], p2b, KiT, KRf, True, True, [Fb])
                  TT(ctx, "dve", AkkT, ps2_[:, 0:128], mSU, ALU.mult, [p2b, cb], [Ab])
                  TT(ctx, "dve", ArkT, ps2_[:, 128:256], mLT, ALU.mult, [p2b, cb], [Ab])
                  ps3, p3b = psr.next()
                  mm(ctx, ps3[:, 0:128], p3b, Fm[:, 0, :], BiT, True, True, [Fb])
                  A_cur, A_curb = rN.next()
                  STT(ctx, "dve", A_cur[:, :], ps3[:, 0:128], -1.0, mSL, ALU.mult, ALU.mult, [p3b, cb], [A_curb])
                  yield
                  if rw_level == 5:
                      continue
                  AT_cur, AT_curb = NT_, Ab
                  RT, RTb = rR.next()
                  TT(ctx, "pool", RT[:, :], NT_, ident_f, ALU.add, [Ab, cb], [RTb])
                  for lvl in range(6):
                      psa, pab = psr.next()
                      mm(ctx, psa[:, 0:128], pab, AT_cur, A_cur[:, :], True, True, [AT_curb, A_curb])
                      A2, A2b = rN.next()
                      CP(ctx, "act", A2[:, :], psa[:, 0:128], [pab], [A2b])
                      if lvl < 5:
                          psb_, pbb = psr.next()
                          mm(ctx, psb_[:, 0:128], pbb, A_cur[:, :], AT_cur, True, True, [AT_curb, A_curb])
                          A2T, A2Tb = rN.next()
                          CP(ctx, "act", A2T[:, :], psb_[:, 0:128], [pbb], [A2Tb])
                          yield
                      psc, pcb = psr.next()
                      mm(ctx, psc[:, 0:128], pcb, A2[:, :], RT[:, :], True, True, [A2b, RTb])
                      RT2, RT2b = rR.next()
                      TT(ctx, "dve", RT2[:, :], psc[:, 0:128], RT[:, :], ALU.add, [pcb, RTb], [RT2b])
                      yield
                      RT, RTb = RT2, RT2b
                      A_cur, A_curb = A2, A2b
                      if lvl < 5:
                          AT_cur, AT_curb = A2T[:, :], A2Tb
                  if rw_level == 6:
                      continue
                  ps4, p4b = psr.next()
                  mm(ctx, ps4[:, 0:64], p4b, AkkT, V_tok, True, True, [Ab, TKb])
                  CP(ctx, "act", KX[:, 64:128], ps4[:, 0:64], [p4b], [KXb])
                  yield
                  ps5, p5b = psr.next()
                  mm(ctx, ps5[:, 0:128], p5b, RT[:, :], KX[:, :], True, True, [RTb, KXb])
                  WU, WUb = rWU.next()
                  CP(ctx, "act", WU[:, :], ps5[:, 0:128], [p5b], [WUb])
                  yield
                  S64, S64b = rS64.next()
                  RyT, PTs = S64[:, 0:128], S64[:, 128:192]
                  ps6, p6b = psr.next()
                  mm(ctx, ps6[0:64, 0:128], p6b, WU[:, 0:64], NArbT, True, False, [WUb, Ab])
                  mm(ctx, ps6[0:64, 0:128], p6b, Rg_tok, ident_f, False, True, [TKb, cb])
                  CP(ctx, "act", RyT, ps6[0:64, 0:128], [p6b], [S64b])
                  ps7, p7b = psr.next()
                  mm(ctx, ps7[0:64, 0:64], p7b, WU[:, 0:64], NBt_tok, True, True, [WUb, TKb])
                  STT(ctx, "dve", PTs, ident_f[0:64, 0:64], eg[:, 127:128], ps7[0:64, 0:64], ALU.mult, ALU.add, [cb, Eb, p7b], [S64b])
                  yield
                  if rw_level == 7:
                      continue
                  ps9, p9b = rwacc
                  mm(ctx, ps9[:, 0:64], p9b, ArkT, V_tok, True, False, [Ab, TKb])
                  mm(ctx, ps9[:, 0:64], p9b, NArbT, WU[:, 64:128], False, False, [Ab, WUb])
                  mm(ctx, ps9[:, 0:64], p9b, RyT, M[:, :], False, True, [S64b, Mb])
                  ps8, p8b = rwacc
                  mm(ctx, ps8[0:64, 64:128], p8b, Kt_tok, V_tok, True, False, [TKb])
                  mm(ctx, ps8[0:64, 64:128], p8b, NBt_tok, WU[:, 64:128], False, False, [TKb, WUb])
                  mm(ctx, ps8[0:64, 64:128], p8b, PTs, M[:, :], False, True, [S64b, Mb])
                  CP(ctx, "act", M[:, :], ps8[0:64, 64:128], [p8b], [Mb])
                  yield
                  if rw_level == 8:
                      continue
                  s2, s2b = smallR.next()
                  yt, ytb = rY.next()
                  CP(ctx, "dve", yt[:, :], ps9[:, 0:64], [p9b], [ytb])
                  ctx.op("dve", lambda e, o=s2[:, 8:14], i_=yt[:, :]: e.bn_stats(o, i_), reads=[ytb], writes=[s2b])
                  ctx.op("dve", lambda e, o=s2[:, 16:18], i_=s2[:, 8:14]: e.bn_aggr(o, i_), reads=[s2b], writes=[s2b])
                  A_(ctx, s2[:, 18:19], s2[:, 17:18], AF.Sqrt, [s2b], [s2b], bias=64e-5)
                  ctx.op("dve", lambda e, o=s2[:, 18:19]: e.reciprocal(o, o), reads=[s2b], writes=[s2b])
                  TS(ctx, "dve", yt[:, :], yt[:, :], s2[:, 16:17], s2[:, 18:19], ALU.subtract, ALU.mult, [ytb, s2b], [ytb])
                  psy, pyb = psr.next()
                  mm(ctx, psy[0:64, 0:128], pyb, yt[:, :], ident_f, True, True, [ytb, cb])
                  ot, otb = outT.next()
                  TS(ctx, "dve", ot[0:64, 0:128], psy[0:64, 0:128], PV("lng_%d" % i, 64), PV("lnb_%d" % i, 64), ALU.mult, ALU.add,
                     [pyb, cb], [otb])
                  TT(ctx, "pool", ot[0:64, 0:128], ot[0:64, 0:128], bv[:, cs], ALU.add, [otb, bvb], [otb])
                  TT(ctx, "pool", ot[0:64, 0:128], ot[0:64, 0:128], g_[:, cs], ALU.mult, [otb, g_b], [otb])
                  ctx.dma("sp", hout_d[512 + i * 64:512 + (i + 1) * 64, t0 + j * 128:t0 + (j + 1) * 128], ot[0:64, 0:128], reads=[otb])
                  yield

        fq, fqb = fQr.next()
        for pr_ in range(2):
            fm_proj(xt, xtb, "fq%d" % pr_,
                    lambda ps, pb, bias, pr_=pr_: A_(ctx, fq[:, pr_, :], ps, AF.Identity, [pb, cb], [fqb], bias=bias, scale=1.0))
            fm_proj(xt, xtb, "fk%d" % pr_,
                    lambda ps, pb, bias, pr_=pr_: A_(ctx, fK[:, pr_, t0:t0 + 512], ps, AF.Identity, [pb, cb], [fKb], bias=bias))

        def gen_fox(blk=blk, fq=fq, fqb=fqb):
          for j in range(4):
              qb = blk * 4 + j
              pso, pob = foxacc
              for h in range(4):
                  hs = slice((h % 2) * 64, (h % 2) * 64 + 64)
                  psr_, prb = psr.next()
                  mm(ctx, psr_[:, 0:1], prb, sel63, fG[:, qb, h:h + 1], True, True, [cb, fGb])
                  sm, smb = smallF.next()
                  CP(ctx, "dve", sm[:, 0:1], psr_[:, 0:1], [prb], [smb])
                  fb, fbb = fbias.next()
                  TS(ctx, "dve", fb[:, 0:qb + 1], fG[:, 0:qb + 1, h], sm[:, 0:1], None, ALU.subtract, None, [fGb, smb], [fbb])
                  for kb in range(qb + 1):
                      ps, pb = psr.next()
                      mm(ctx, ps[:, 0:128], pb, fK[hs, h // 2, kb * 128:(kb + 1) * 128], fq[hs, h // 2, j * 128:(j + 1) * 128],
                         True, kb != qb, [fKb, fqb])
                      if kb == qb:
                          mm(ctx, ps[:, 0:128], pb, ident_b, mneg_b, False, True, [cb])
                      pt, ptb = PTr.next()
                      A_(ctx, pt[:, :], ps[:, 0:128], AF.Exp, [pb, fbb], [ptb], bias=fb[:, kb:kb + 1], scale=0.125)
                      mm(ctx, pso[:, h * 65:(h + 1) * 65], pob, pt[:, :], fV[:, kb, h * 65:(h + 1) * 65], kb == 0, kb == qb,
                         [ptb, fVb])
                      yield
              sm, smb = smallF.next()
              ctx.op("dve", lambda e, o=sm[:, 0:4], i=pso[:, 0:260].rearrange("p (h c) -> p h c", c=65)[:, :, 64]: e.reciprocal(o, i),
                     reads=[pob], writes=[smb])
              hb, hbb = hbr.next()
              for h in range(4):
                  TS(ctx, "dve", hb[:, h * 64:(h + 1) * 64], pso[:, h * 65:h * 65 + 64], sm[:, h:h + 1], None, ALU.mult, None,
                     [pob, smb], [hbb])
              pst, ptb2 = psr.next()
              for c in range(2):
                  mm(ctx, pst[:, c * 128:(c + 1) * 128], ptb2, hb[:, c * 128:(c + 1) * 128], ident_f, True, True, [hbb, cb])
              ot, otb = outT.next()
              CP(ctx, "act", ot[:, 0:256], pst[:, 0:256], [ptb2], [otb])
              for c in range(2):
                  ctx.dma("sp", hout_d[256 + c * 128:256 + (c + 1) * 128, qb * 128:(qb + 1) * 128], ot[:, c * 128:(c + 1) * 128],
                          reads=[otb])
              yield

        g_rw, g_fx, g_ml = gen_rwkv(), gen_fox(), gen_mlstm()
        n_rw = 4 * (4 * 19 + 6) + 2
        r_fx = (64 * blk + 44) / float(n_rw)
        r_ml = 34.0 / n_rw
        c_fx = c_ml = 0.0
        alive = {"fx": True, "ml": True}

        def adv(g, key):
            if alive[key]:
                try:
                    next(g)
                except StopIteration:
                    alive[key] = False

        for _ in g_rw:
            c_fx += r_fx
            c_ml += r_ml
            while c_fx >= 1.0 and alive["fx"]:
                adv(g_fx, "fx")
                c_fx -= 1.0
            while c_ml >= 1.0 and alive["ml"]:
                adv(g_ml, "ml")
                c_ml -= 1.0
        while alive["ml"]:
            adv(g_ml, "ml")
        while alive["fx"]:
            adv(g_fx, "fx")
    ctx.wait_all_dma("sp")
    ctx.emit()
    ctx.close()
    return nc, ctx


def _layout(vres):
    cols = [("m_qk", 512), ("m_v", 512), ("m_o", 512), ("m_i", 4), ("m_f", 4), ("f_q", 512), ("f_k", 512), ("f_v", 512),
            ("f_f", 8), ("gate", 3072), ("r_r", 512), ("r_k", 512), ("r_v", 512), ("r_w", 64), ("r_a", 64), ("r_g", 128)]
    if vres:
        cols.append(("r_vres", 32))
    lay, st = {}, 0
    for n, w in cols:
        lay[n] = st
        st += w
    return lay, st


def l1_consts():
    p = np.arange(128)
    ident = np.eye(128)
    ones = np.ones((128, 128))
    mLT = (p[:, None] <= p[None, :]).astype(np.float64)
    utri = mLT.copy()
    sel127 = np.zeros((128, 128)); sel127[127, :] = 1
    sel63 = np.zeros((128, 128)); sel63[63, :] = 1
    mneg = np.where(p[:, None] <= p[None, :], 0.0, -30000.0)
    bd = (p[:, None] // 64 == p[None, :] // 64).astype(np.float64)
    mSU = (p[:, None] < p[None, :]).astype(np.float64)
    mSL = (p[:, None] > p[None, :]).astype(np.float64)
    return np.concatenate([ident, ones, mLT, utri, sel127, sel63, mneg, bd, mSU, mSL], 1).astype(np.float32)


def pack_l1(inp, l, hh):
    vres = l > 0
    lay, ncol = _layout(vres)
    W = inp["w_in_%d" % l]
    Bv = inp["b_in_%d" % l]
    g = lambda n: inp["%s_%d" % (n, l)]
    mh = [2 * hh, 2 * hh + 1]
    fh = [4 * hh + i for i in range(4)]
    cols = {}
    for i, h in enumerate(mh):
        cols["mq%d" % i] = np.arange(lay["m_qk"] + h * 64, lay["m_qk"] + (h + 1) * 64)
        cols["mk%d" % i] = np.arange(lay["m_qk"] + 256 + h * 64, lay["m_qk"] + 256 + (h + 1) * 64)
        cols["mo%d" % i] = np.arange(lay["m_o"] + h * 128, lay["m_o"] + (h + 1) * 128)
    for pr in range(2):
        cols["fq%d" % pr] = np.arange(lay["f_q"] + fh[2 * pr] * 64, lay["f_q"] + (fh[2 * pr] + 2) * 64)
        cols["fk%d" % pr] = np.arange(lay["f_k"] + fh[2 * pr] * 64, lay["f_k"] + (fh[2 * pr] + 2) * 64)
    for i, h in enumerate(fh):
        cols["rr%d" % i] = np.arange(lay["r_r"] + h * 64, lay["r_r"] + (h + 1) * 64)
        cols["rk%d" % i] = np.arange(lay["r_k"] + h * 64, lay["r_k"] + (h + 1) * 64)
        cols["rv%d" % i] = np.arange(lay["r_v"] + h * 64, lay["r_v"] + (h + 1) * 64)
    cols["lw"] = np.arange(lay["r_w"], lay["r_w"] + 64)
    cols["la"] = np.arange(lay["r_a"], lay["r_a"] + 64)
    cols["lg"] = np.arange(lay["r_g"], lay["r_g"] + 128)
    cols["lv"] = np.arange(lay["r_vres"], lay["r_vres"] + 32) if vres else None
    wf = np.zeros((D, NF), np.float32)
    bfm = np.zeros((128, len(FGROUPS)), np.float32)
    for n, w in FGROUPS:
        off, _, gi = FOFF[n]
        if cols[n] is None:
            continue
        wf[:, off:off + w] = W[:, cols[n]]
        bfm[:w, gi] = Bv[cols[n]]
    tcols = np.concatenate([np.arange(lay["m_v"] + mh[0] * 128, lay["m_v"] + (mh[1] + 1) * 128),
                            np.arange(lay["f_v"] + fh[0] * 64, lay["f_v"] + (fh[3] + 1) * 64),
                            lay["m_i"] + np.array(mh), lay["m_f"] + np.array(mh), lay["f_f"] + np.array(fh)])
    wt = np.ascontiguousarray(W[:, tcols])
    btm = np.ascontiguousarray(Bv[tcols])[None, :]
    pv = np.zeros((128, len(PVN)), np.float32)
    r0 = lay["r_r"]
    mu = g("r_mu")
    mc = g("m_conv")
    for gname in ("mq0", "mq1", "mk0", "mk1"):
        for j in range(4):
            pv[:64, PVI["conv%d_%s" % (j, gname)]] = mc[j, cols[gname] - lay["m_qk"]]
    for i, h in enumerate(mh):
        pv[:128, PVI["mnorm%d" % i]] = g("m_norm")[h * 128:(h + 1) * 128]
    for i, h in enumerate(fh):
        sl = slice(h * 64, (h + 1) * 64)
        pv[:64, PVI["mu_r_%d" % i]] = mu[cols["rr%d" % i] - r0]
        pv[:64, PVI["mu_k_%d" % i]] = mu[cols["rk%d" % i] - r0]
        pv[:64, PVI["mu_v_%d" % i]] = mu[cols["rv%d" % i] - r0]
        pv[:64, PVI["wbias_%d" % i]] = g("r_wbias")[sl]
        pv[:64, PVI["abias_%d" % i]] = g("r_abias")[sl]
        if vres:
            pv[:64, PVI["vbias_%d" % i]] = g("r_vbias")[sl]
        pv[:64, PVI["kkw_%d" % i]] = g("r_kk")[sl]
        pv[:64, PVI["ka_%d" % i]] = g("r_ka")[sl]
        pv[:64, PVI["rk_%d" % i]] = g("r_rk")[sl]
        pv[:64, PVI["lng_%d" % i]] = g("r_ln_g")[sl]
        pv[:64, PVI["lnb_%d" % i]] = g("r_ln_b")[sl]
    pv[:64, PVI["mu_lw"]] = mu[cols["lw"] - r0]
    pv[:64, PVI["mu_la"]] = mu[cols["la"] - r0]
    pv[:128, PVI["mu_lg"]] = mu[cols["lg"] - r0]
    if vres:
        pv[:32, PVI["mu_lv"]] = mu[cols["lv"] - r0]
    lora = np.zeros((128, 4, 256), np.float32)
    csl = slice(fh[0] * 64, (fh[3] + 1) * 64)
    lora[:64, 0] = g("r_wB")[:, csl]
    lora[:64, 1] = g("r_aB")[:, csl]
    lora[:128, 2] = g("r_gB")[:, csl]
    if vres:
        lora[:32, 3] = g("r_vB")[:, csl]
    return dict(wf=wf, wt=wt, bfm=bfm, btm=btm, pv=pv, lora=lora, cst=l1_consts())


_PROGS = {}
SEQ = 8192
NBATCH = 4


def _prog(kind, **kw):
    key = (kind,) + tuple(sorted(kw.items()))
    if key not in _PROGS:
        if kind == "l1":
            _PROGS[key] = build_l1(T=SEQ, vres=kw["vres"])[0]
        else:
            _PROGS[key] = build_l2(kw["moe"], NT=SEQ // 2)[0]
    return _PROGS[key]


def run_l1(inp, l, cur):
    nc = _prog("l1", vres=l > 0)
    packs = [pack_l1(inp, l, hh) for hh in range(2)]
    xTs = [np.ascontiguousarray(cur[b].T) for b in range(NBATCH)]
    maps = []
    for c in range(NCORES):
        b, hh = c // 2, c % 2
        m = dict(packs[hh])
        m["xT"] = xTs[b]
        if l > 0:
            m["vfirst"] = inp["_vfirst"][c]
        maps.append(m)
    res = run_bass_kernel_spmd(nc, maps, core_ids=list(range(NCORES)))
    hT = []
    for b in range(NBATCH):
        o0, o1 = res.results[2 * b]["hout"], res.results[2 * b + 1]["hout"]
        hT.append(np.concatenate([o0[0:256], o1[0:256], o0[256:512], o1[256:512], o0[512:768], o1[512:768]], 0))
    vown = [np.ascontiguousarray(res.results[c]["vown"]) for c in range(NCORES)]
    return hT, vown


def run_l2(inp, l, cur, hT):
    moe = l % 2 == 1
    nc = _prog("l2", moe=moe)
    NT = SEQ // 2
    g = lambda n: inp["%s_%d" % (n, l)]
    lay, _ = _layout(l > 0)
    g0 = lay["gate"]
    W = g("w_in")
    shared = dict(
        cst=np.concatenate([np.eye(128), np.ones((128, 128))], 1).astype(np.float32),
        vecs=np.stack([inp["mem_ln_g"], inp["mem_ln_b"], g("ln1_g"), g("ln1_b"), g("ln2_g"), g("ln2_b"),
                       g("ln3_g"), g("ln3_b")]).astype(np.float32),
        wg=np.ascontiguousarray(W[:, g0:g0 + 3072]),
        bg=np.ascontiguousarray(g("b_in")[g0:g0 + 3072])[None, :],
        wup=np.concatenate([g("m_up"), g("f_up"), g("r_up")], 0),
        wout=g("w_out"), wq=g("x_wq"), wkv=g("x_wkv"), wo=g("x_wo"))
    if moe:
        shared.update(rw=g("ex_router"), rb=g("ex_router_b")[None, :], wgu=g("ex_wgu"), wd=g("ex_wd"))
    else:
        shared.update(wgu=g("ff_wgu"), wd=g("ff_wd"))
    maps = []
    for c in range(NCORES):
        b, half = c // 2, c % 2
        sl = slice(half * NT, (half + 1) * NT)
        m = dict(shared)
        m["x_tok"] = np.ascontiguousarray(cur[b, sl])
        m["xT"] = np.ascontiguousarray(cur[b, sl].T)
        m["hT"] = np.ascontiguousarray(hT[b][:, sl])
        m["mem"] = np.ascontiguousarray(inp["mem"][b])
        maps.append(m)
    res = run_bass_kernel_spmd(nc, maps, core_ids=list(range(NCORES)))
    out = np.empty((NBATCH, SEQ, D), np.float32)
    for c in range(NCORES):
        b, half = c // 2, c % 2
        out[b, half * NT:(half + 1) * NT] = res.results[c]["x3"]
    return out


_INPUT_NAMES = (
    "x", "mem", "mem_ln_g", "mem_ln_b", "w_in_0", "b_in_0", "m_conv_0", "m_norm_0", "m_up_0", "f_up_0",
    "r_mu_0", "r_wbias_0", "r_wB_0", "r_abias_0", "r_aB_0", "r_gB_0", "r_kk_0", "r_ka_0", "r_rk_0",
    "r_ln_g_0", "r_ln_b_0", "r_up_0", "w_out_0", "ln1_g_0", "ln1_b_0", "x_wq_0", "x_wkv_0", "x_wo_0",
    "ln2_g_0", "ln2_b_0", "ff_wgu_0", "ff_wd_0", "ln3_g_0", "ln3_b_0", "w_in_1", "b_in_1", "m_conv_1",
    "m_norm_1", "m_up_1", "f_up_1", "r_mu_1", "r_wbias_1", "r_wB_1", "r_abias_1", "r_aB_1", "r_vbias_1",
    "r_vB_1", "r_gB_1", "r_kk_1", "r_ka_1", "r_rk_1", "r_ln_g_1", "r_ln_b_1", "r_up_1", "w_out_1", "ln1_g_1",
    "ln1_b_1", "x_wq_1", "x_wkv_1", "x_wo_1", "ln2_g_1", "ln2_b_1", "ex_router_1", "ex_router_b_1",
    "ex_wgu_1", "ex_wd_1", "ln3_g_1", "ln3_b_1",
)


def kernel(**inputs):
    inp = {k: np.asarray(inputs[k], dtype=np.float32) for k in _INPUT_NAMES}
    cur = inp["x"]
    for l in range(2):
        hT, vown = run_l1(inp, l, cur)
        if l == 0:
            inp["_vfirst"] = vown
        cur = run_l2(inp, l, cur, hT)
    return cur
```

```python
import numpy as np
import concourse.bass as bass
import concourse.mybir as mybir
from concourse.bass_utils import run_bass_kernel_spmd

F32 = mybir.dt.float32
BF16 = mybir.dt.bfloat16
AF = mybir.ActivationFunctionType
ALU = mybir.AluOpType
AX = mybir.AxisListType

D = 1024
NCORES = 8
ALPHA = (2.0 * 2) ** 0.25
LN_EPS = 1e-5


class Buf:
    __slots__ = ("name", "w", "r", "psum")

    def __init__(self, name="", psum=False):
        self.name = name
        self.psum = psum
        self.w = None
        self.r = {}


class Ctx:
    ENGS = ("pe", "act", "dve", "pool", "sp")
    NDMASEM = 24

    def __init__(self, nc):
        self.nc = nc
        self.q = {e: [] for e in self.ENGS}
        self.cnt = {e: 0 for e in self.ENGS}
        self.seen = {e: {} for e in self.ENGS}
        self.sems = {}
        self.dma_uses = [0] * self.NDMASEM
        self.dma_rr = 0
        self.dma_rr_sw = 0
        self._stack = []
        self.ninst = 0

    def enter(self, cm):
        v = cm.__enter__()
        self._stack.append(cm)
        return v

    def close(self):
        while self._stack:
            self._stack.pop().__exit__(None, None, None)

    def alloc_sems(self):
        for e in self.ENGS:
            self.sems[e] = self.enter(self.nc.semaphore("s_" + e))
        for i in range(self.NDMASEM):
            self.sems[("dma", i)] = self.enter(self.nc.semaphore("s_dma%d" % i))

    def sbuf(self, name, shape, dt):
        return self.enter(self.nc.sbuf_tensor(name, list(shape), dt))

    def psum(self, name, shape, dt=F32):
        return self.enter(self.nc.psum_tensor(name, list(shape), dt))

    def _need(self, eng, tok, waits):
        if tok is None:
            return
        key, val = tok
        if key == "pe" and eng == "pe":
            return
        if self.seen[eng].get(key, 0) >= val:
            return
        waits[key] = max(waits.get(key, 0), val)

    def _deps(self, eng, reads, writes):
        waits = {}
        for b in reads:
            self._need(eng, b.w, waits)
            if b.psum:
                for key, val in b.r.items():
                    if key != eng:
                        self._need(eng, (key, val), waits)
        for b in writes:
            self._need(eng, b.w, waits)
            for key, val in b.r.items():
                if key == eng and eng != "pool":
                    continue
                self._need(eng, (key, val), waits)
        for key, val in waits.items():
            self.seen[eng][key] = val
        return list(waits.items())

    def op(self, eng, fn, reads=(), writes=()):
        waits = self._deps(eng, reads, writes)
        self.cnt[eng] += 1
        tok = (eng, self.cnt[eng])
        for b in reads:
            if b.r.get(eng, 0) < tok[1]:
                b.r[eng] = tok[1]
        for b in writes:
            b.w = tok
            b.r = {}
        self.q[eng].append((waits, fn, (eng, 1)))
        self.ninst += 1
        return tok

    def dma(self, eng, out, in_, reads=(), writes=()):
        half = self.NDMASEM // 2
        if eng == "pool":
            s = half + self.dma_rr_sw
            self.dma_rr_sw = (self.dma_rr_sw + 1) % half
        else:
            s = self.dma_rr
            self.dma_rr = (self.dma_rr + 1) % half
        key = ("dma", s)
        waits = dict(self._deps(eng, reads, writes))
        prev = self.dma_uses[s] * 16
        if prev and self.seen[eng].get(key, 0) < prev:
            waits[key] = prev
            self.seen[eng][key] = prev
        self.dma_uses[s] += 1
        tok = (key, self.dma_uses[s] * 16)
        for b in reads:
            if b.r.get(key, 0) < tok[1]:
                b.r[key] = tok[1]
        for b in writes:
            b.w = tok
            b.r = {}
        self.q[eng].append((list(waits.items()), lambda e, o=out, i=in_: e.dma_start(out=o, in_=i), (key, 16)))
        self.ninst += 1
        return tok

    def wait_all_dma(self, eng):
        waits = []
        for s in range(self.NDMASEM):
            if self.dma_uses[s]:
                waits.append((("dma", s), self.dma_uses[s] * 16))
        self.q[eng].append((waits, None, None))

    def emit(self):
        nc = self.nc
        with nc.Block() as block:
            def run(e, name):
                for waits, fn, inc in self.q[name]:
                    for key, val in waits:
                        e.wait_ge(self.sems[key], val)
                    if fn is None:
                        continue
                    ins = fn(e)
                    ins.then_inc(self.sems[inc[0]], inc[1])

            @block.tensor
            def _(e):
                run(e, "pe")

            @block.scalar
            def _(e):
                run(e, "act")

            @block.vector
            def _(e):
                run(e, "dve")

            @block.gpsimd
            def _(e):
                run(e, "pool")

            @block.sync
            def _(e):
                run(e, "sp")


class Ring:
    def __init__(self, ctx, name, shape, dt, n, psum=False):
        self.tiles = []
        for i in range(n):
            t = ctx.psum("%s%d" % (name, i), shape, dt) if psum else ctx.sbuf("%s%d" % (name, i), shape, dt)
            self.tiles.append((t, Buf("%s%d" % (name, i), psum=psum)))
        self.i = 0

    def next(self):
        t = self.tiles[self.i]
        self.i = (self.i + 1) % len(self.tiles)
        return t


class WRing:
    SLOT = 4096

    def __init__(self, ctx, n, name="wr", dt=BF16, slot=None):
        self.ctx = ctx
        self.slot = slot or self.SLOT
        self.ring = Ring(ctx, name, [128, self.slot], dt, n)
        self.qi = 0

    def load(self, pieces, kc, ncols):
        t, b = self.ring.next()
        v = t[:, : kc * ncols].rearrange("p (k n) -> p k n", k=kc)
        for ap, off in pieces:
            w = ap.shape[1]
            src = ap.rearrange("(k p) n -> p k n", p=128)
            self.ctx.dma("pool", v[:, :, off:off + w], src, writes=[b])
        return v, b


def mm(ctx, ps, pb, lhsT, rhs, start, stop, reads):
    ctx.op("pe", lambda e: e.matmul(ps, lhsT, rhs, start=start, stop=stop), reads=reads, writes=[pb])


def layer_norm(ctx, z, zb, out, ob, gbc, bbc, pbuf, small, idx, F=1024):
    st, stb = small.next()
    nch = F // 512
    for c in range(nch):
        ctx.op("dve", lambda e, c=c: e.bn_stats(st[:, c * 6:(c + 1) * 6], z[:, c * 512:(c + 1) * 512]),
               reads=[zb], writes=[stb])
    ctx.op("dve", lambda e: e.bn_aggr(st[:, 16:18], st[:, 0:6 * nch]), reads=[stb], writes=[stb])
    A_(ctx, st[:, 18:19], st[:, 17:18], AF.Sqrt, [stb], [stb], bias=LN_EPS)
    ctx.op("dve", lambda e: e.reciprocal(st[:, 18:19], st[:, 18:19]), reads=[stb], writes=[stb])
    ctx.op("dve", lambda e: e.tensor_scalar(z, z, st[:, 16:17], st[:, 18:19], ALU.subtract, ALU.mult),
           reads=[zb, stb], writes=[zb])
    ctx.op("pool", lambda e: e.tensor_tensor(z, z, gbc, ALU.mult), reads=[zb, pbuf], writes=[zb])
    ctx.op("pool", lambda e: e.tensor_tensor(out, z, bbc, ALU.add), reads=[zb, pbuf], writes=[ob])


def transpose_to(ctx, psring, src_bf, srcb, dstT, dstb, ident_bf, cb, tok0, nk=8, evac=("act", "dve")):
    for h in range(nk // 4):
        ps, pb = psring.next()
        for q in range(4):
            k = h * 4 + q
            mm(ctx, ps[:, q * 128:(q + 1) * 128], pb, src_bf[:, k * 128:(k + 1) * 128], ident_bf, True, True,
               [srcb, cb])
        eng = evac[h % len(evac)]
        dst = dstT[:, h * 4:(h + 1) * 4, tok0:tok0 + 128]
        src = ps[:, :].rearrange("p (k t) -> p k t", k=4)
        if eng == "act":
            ctx.op("act", lambda e, d=dst, s=src: e.copy(d, s), reads=[pb], writes=[dstb])
        else:
            ctx.op("dve", lambda e, d=dst, s=src: e.tensor_copy(d, s), reads=[pb], writes=[dstb])


def A_(ctx, out, in_, func, reads, writes, bias=None, scale=None):
    kw = {}
    if bias is not None:
        kw["bias"] = bias
    if scale is not None:
        kw["scale"] = scale
    ctx.op("act", lambda e: e.activation(out, in_, func, **kw), reads=reads, writes=writes)


def TT(ctx, eng, out, in0, in1, op, reads, writes):
    ctx.op(eng, lambda e: e.tensor_tensor(out, in0, in1, op), reads=reads, writes=writes)


def TS(ctx, eng, out, in0, s1, s2, op0, op1, reads, writes):
    if s2 is None:
        ctx.op(eng, lambda e: e.tensor_scalar(out, in0, s1, None, op0), reads=reads, writes=writes)
    else:
        ctx.op(eng, lambda e: e.tensor_scalar(out, in0, s1, s2, op0, op1), reads=reads, writes=writes)


def STT(ctx, eng, out, in0, scalar, in1, op0, op1, reads, writes):
    ctx.op(eng, lambda e: e.scalar_tensor_tensor(out, in0, scalar, in1, op0, op1), reads=reads, writes=writes)


def CP(ctx, eng, out, in_, reads, writes):
    if eng == "act":
        ctx.op("act", lambda e: e.copy(out, in_), reads=reads, writes=writes)
    else:
        ctx.op(eng, lambda e: e.tensor_copy(out, in_), reads=reads, writes=writes)


def build_l2(moe, NT=4096, G=512, FF=None, NE=8):
    FF = FF or (3584 if moe else 2816)
    FC = FF // 128
    NG = NT // G
    nc = bass.Bass("TRN2", target_bir_lowering=False)
    ctx = Ctx(nc)

    def din(name, shape):
        return nc.dram_tensor(name, list(shape), F32, kind="ExternalInput").ap()

    x_tok = din("x_tok", [NT, D])
    xT_d = din("xT", [D, NT])
    hT_d = din("hT", [1536, NT])
    mem_d = din("mem", [256, D])
    cst_d = din("cst", [128, 256])
    vec_d = din("vecs", [8, D])
    wg_d = din("wg", [D, 3072])
    bg_d = din("bg", [1, 3072])
    wup_d = din("wup", [1536, D])
    wout_d = din("wout", [D, D])
    wq_d = din("wq", [D, D])
    wkv_d = din("wkv", [D, 2 * D])
    wo_d = din("wo", [D, D])
    if moe:
        rw_d = din("rw", [D, NE])
        rb_d = din("rb", [1, NE])
        wgu_d = din("wgu", [NE, D, 2 * FF])
        wd_d = din("wd", [NE, FF, D])
    else:
        wgu_d = din("wgu", [D, 2 * FF])
        wd_d = din("wd", [FF, D])
    out_d = nc.dram_tensor("x3", [NT, D], F32, kind="ExternalOutput").ap()

    ctx.alloc_sems()
    S = ctx.sbuf
    cst = S("cst_s", [128, 256], F32)
    cstbf = S("cstbf", [128, 256], BF16)
    cb = Buf("cst")
    ident_bf = cstbf[:, 0:128]
    ones_bf = cstbf[:, 128:256]
    ident_f = cst[:, 0:128]
    vecs = S("vecs_s", [128, 8, D], F32)
    bgbf = S("bgbf", [1, 3072], BF16)
    ctx.dma("sp", cst[:, :], cst_d, writes=[cb])
    ctx.dma("pool", cstbf[:, :], cst_d, writes=[cb])
    ctx.dma("sp", vecs[:, :, :].rearrange("p a d -> p (a d)"),
            vec_d.rearrange("a d -> (a d)").partition_broadcast(128), writes=[cb])
    ctx.dma("pool", bgbf[:, :], bg_d, writes=[cb])
    if moe:
        rw = S("rw_s", [128, 8, NE], F32)
        rb = S("rb_s", [1, NE], F32)
        ctx.dma("sp", rw[:, :, :], rw_d.rearrange("(k p) n -> p k n", p=128), writes=[cb])
        ctx.dma("sp", rb[:, :], rb_d, writes=[cb])

    psr = Ring(ctx, "ps", [128, 512], F32, 8, psum=True)
    wr = WRing(ctx, 5)
    small = Ring(ctx, "small", [128, 32], F32, 4)
    tmpr = Ring(ctx, "tmp", [128, 512], F32, 3)
    tmpb = Ring(ctx, "tmpb", [128, 1024], BF16, 2)

    memT = S("memT", [128, 8, 256], BF16)
    memTb = Buf("memT")
    kxT = S("kxT", [128, 8, 256], BF16)
    vx = S("vx", [128, 2, D], BF16)
    kvb = Buf("kv")
    zbuf = S("zbuf", [128, 4, D], F32); zbb = [Buf("z%d" % j) for j in range(4)]
    outr = Ring(ctx, "xo", [128, D], F32, 2)
    for mc in range(2):
        z, zb = zbuf[:, mc, :], zbb[mc]
        ctx.dma("sp", z[:, :], mem_d[mc * 128:(mc + 1) * 128, :], writes=[zb])
        o, ob = zbuf[:, 2 + mc, :], zbb[2 + mc]
        layer_norm(ctx, z[:, :], zb, o[:, :], ob, vecs[:, 0, :], vecs[:, 1, :], cb, small, 0)
        t, tb = tmpb.next()
        CP(ctx, "act", t[:, :], o[:, :], [ob], [tb])
        transpose_to(ctx, psr, t, tb, memT, memTb, ident_bf, cb, mc * 128)
    for s in range(4):
        w, wb = wr.load([(wkv_d[:, s * 512:(s + 1) * 512], 0)], 8, 512)
        if s < 2:
            for o4 in range(4):
                oc = s * 4 + o4
                ps, pb = psr.next()
                for k in range(8):
                    mm(ctx, ps[:, 0:256], pb, w[:, k, o4 * 128:(o4 + 1) * 128], memT[:, k, :], k == 0, k == 7,
                       [wb, memTb])
                CP(ctx, "dve", kxT[:, oc, :], ps[:, 0:256], [pb], [kvb])
        else:
            nb = s - 2
            for mc in range(2):
                ps, pb = psr.next()
                for k in range(8):
                    mm(ctx, ps[:, :], pb, memT[:, k, mc * 128:(mc + 1) * 128], w[:, k, :], k == 0, k == 7,
                       [wb, memTb])
                CP(ctx, "act", vx[:, mc, nb * 512:(nb + 1) * 512], ps[:, :], [pb], [kvb])

    xtok = S("xtok", [128, 4, D], F32); x1b = [Buf("x1%d" % j) for j in range(4)]
    big = S("big", [128, max(FC, 28), G], BF16); bigb = Buf("big")
    hT = big[:, 0:12, :]; qT = big[:, 12:20, :]; oT = big[:, 20:28, :]; hff = big[:, 0:FC, :]
    hTb = qTb = oTb = hfb = bigb
    merged = S("merged", [128, 4, D], F32); mgb = [Buf("mg%d" % j) for j in range(4)]
    x1 = xtok
    x2 = merged; x2b = mgb
    aT = S("aT", [128, 8, G], BF16); aTb = Buf("aT")
    xT = aT; xTb = aTb
    PT = Ring(ctx, "PT", [128, G], BF16, 4)
    if moe:
        yacc = zbuf; yb = zbb
        x2Tf = S("x2Tf", [128, 8, 128], F32); x2Tfb = Buf("x2Tf")
        comb = S("comb", [128, 4, NE], F32); combb = [Buf("comb%d" % j) for j in range(4)]

    def linear_tok(lhsT_tile, lhsT_buf, w_dram, K, ncols_total, consume, bias_row=None):
        KC = K // 128
        for nb in range(ncols_total // 512):
            w, wb = wr.load([(w_dram[:, nb * 512:(nb + 1) * 512], 0)], KC, 512)
            for j in range(4):
                ps, pb = psr.next()
                for k in range(KC):
                    mm(ctx, ps[:, :], pb, lhsT_tile[:, k, j * 128:(j + 1) * 128], w[:, k, :], k == 0,
                       k == KC - 1 and bias_row is None, [lhsT_buf, wb])
                if bias_row is not None:
                    mm(ctx, ps[:, :], pb, ones_bf[0:1, 0:128], bias_row[0:1, nb * 512:(nb + 1) * 512], False, True,
                       [cb])
                consume(j, nb, ps, pb)

    for g in range(NG):
        t0 = g * G
        ctx.dma("sp", xtok[:, :, :], x_tok[t0:t0 + G, :].rearrange("(j p) d -> p j d", p=128), writes=x1b)
        ctx.dma("pool", xT[:, :, :], xT_d.rearrange("(k p) t -> p k t", p=128)[:, :, t0:t0 + G], writes=[xTb])
        ctx.dma("pool", hT[:, :, :], hT_d.rearrange("(k p) t -> p k t", p=128)[:, :, t0:t0 + G], writes=[hTb])

        for nb in range(6):
            br, half = nb // 2, nb % 2
            wgs, wgb = wr.load([(wg_d[:, nb * 512:(nb + 1) * 512], 0)], 8, 512)
            wus, wub = wr.load([(wup_d[br * 512:(br + 1) * 512, half * 512:(half + 1) * 512], 0)], 4, 512)
            for j in range(4):
                psg, pgb = psr.next()
                for k in range(8):
                    mm(ctx, psg[:, :], pgb, xT[:, k, j * 128:(j + 1) * 128], wgs[:, k, :], k == 0, False, [xTb, wgb])
                mm(ctx, psg[:, :], pgb, ones_bf[0:1, 0:128], bgbf[0:1, nb * 512:(nb + 1) * 512], False, True, [cb])
                psu, pub = psr.next()
                for k in range(4):
                    mm(ctx, psu[:, :], pub, hT[:, br * 4 + k, j * 128:(j + 1) * 128], wus[:, k, :], k == 0, k == 3,
                       [hTb, wub])
                gt, gtb = tmpr.next()
                A_(ctx, gt[:, :], psg[:, :], AF.Sigmoid, [pgb], [gtb])
                dst = merged[:, j, half * 512:(half + 1) * 512]
                if br == 0:
                    TT(ctx, "dve", dst, gt[:, :], psu[:, :], ALU.mult, [gtb, pub], [mgb[j]])
                else:
                    TT(ctx, "dve", gt[:, :], gt[:, :], psu[:, :], ALU.mult, [gtb, pub], [gtb])
                    TT(ctx, "pool", dst, dst, gt[:, :], ALU.add, [gtb, mgb[j]], [mgb[j]])

        for j in range(4):
            t, tb = tmpb.next()
            CP(ctx, "act", t[:, :], merged[:, j, :], [mgb[j]], [tb])
            transpose_to(ctx, psr, t, tb, aT, aTb, ident_bf, cb, j * 128)
        zs = [(zbuf[:, j, :], zbb[j]) for j in range(4)]

        def cons1(j, nb, ps, pb):
            z, zb = zs[j]
            STT(ctx, "dve", z[:, nb * 512:(nb + 1) * 512], xtok[:, j, nb * 512:(nb + 1) * 512], ALPHA, ps[:, :],
                ALU.mult, ALU.add, [x1b[j], pb], [zb])
        linear_tok(aT, aTb, wout_d, D, D, cons1)
        for j in range(4):
            z, zb = zs[j]
            layer_norm(ctx, z[:, :], zb, x1[:, j, :], x1b[j], vecs[:, 2, :], vecs[:, 3, :], cb, small, 0)
            t, tb = tmpb.next()
            CP(ctx, "act", t[:, :], x1[:, j, :], [x1b[j]], [tb])
            transpose_to(ctx, psr, t, tb, aT, aTb, ident_bf, cb, j * 128)

        for s in range(2):
            w, wb = wr.load([(wq_d[:, s * 512:(s + 1) * 512], 0)], 8, 512)
            for o4 in range(4):
                oc = s * 4 + o4
                ps, pb = psr.next()
                for k in range(8):
                    mm(ctx, ps[:, :], pb, w[:, k, o4 * 128:(o4 + 1) * 128], aT[:, k, :], k == 0, k == 7, [wb, aTb])
                A_(ctx, qT[:, oc, :], ps[:, :], AF.Copy, [pb], [qTb], scale=1.0 / 16.0)
        for h in range(4):
            pts = []
            for mc in range(2):
                ps, pb = psr.next()
                for c in range(2):
                    mm(ctx, ps[:, :], pb, kxT[:, h * 2 + c, mc * 128:(mc + 1) * 128], qT[:, h * 2 + c, :], c == 0,
                       c == 1, [kvb, qTb])
                pt, ptb = PT.next()
                A_(ctx, pt[:, :], ps[:, :], AF.Exp, [pb], [ptb])
                pts.append((pt, ptb))
            psd, pdb = psr.next()
            for mc in range(2):
                mm(ctx, psd[:, :], pdb, ones_bf, pts[mc][0][:, :], mc == 0, mc == 1, [cb, pts[mc][1]])
            rd, rdb = tmpr.next()
            ctx.op("dve", lambda e, o=rd[:, :], i=psd[:, :]: e.reciprocal(o, i), reads=[pdb], writes=[rdb])
            for dc in range(2):
                pso, pob = psr.next()
                for mc in range(2):
                    mm(ctx, pso[:, :], pob, vx[:, mc, h * 256 + dc * 128:h * 256 + (dc + 1) * 128], pts[mc][0][:, :],
                       mc == 0, mc == 1, [kvb, pts[mc][1]])
                TT(ctx, "dve", oT[:, h * 2 + dc, :], pso[:, :], rd[:, :], ALU.mult, [pob, rdb], [oTb])
        zs = [(zbuf[:, j, :], zbb[j]) for j in range(4)]

        def cons2(j, nb, ps, pb):
            z, zb = zs[j]
            STT(ctx, "dve", z[:, nb * 512:(nb + 1) * 512], x1[:, j, nb * 512:(nb + 1) * 512], ALPHA, ps[:, :],
                ALU.mult, ALU.add, [x1b[j], pb], [zb])
        linear_tok(oT, oTb, wo_d, D, D, cons2)
        for j in range(4):
            z, zb = zs[j]
            layer_norm(ctx, z[:, :], zb, x2[:, j, :], x2b[j], vecs[:, 4, :], vecs[:, 5, :], cb, small, 0)
            t, tb = tmpb.next()
            CP(ctx, "act", t[:, :], x2[:, j, :], [x2b[j]], [tb])
            transpose_to(ctx, psr, t, tb, aT, aTb, ident_bf, cb, j * 128)
            if moe:
                for hh in range(2):
                    ps, pb = psr.next()
                    for q in range(4):
                        k = hh * 4 + q
                        mm(ctx, ps[:, q * 128:(q + 1) * 128], pb, x2[:, j, k * 128:(k + 1) * 128], ident_f, True, True,
                           [x2b[j], cb])
                    CP(ctx, "dve", x2Tf[:, hh * 4:(hh + 1) * 4, :], ps[:, :].rearrange("p (k t) -> p k t", k=4), [pb],
                       [x2Tfb])
                ps, pb = psr.next()
                for k in range(8):
                    mm(ctx, ps[:, 0:NE], pb, x2Tf[:, k, :], rw[:, k, :], k == 0, False, [x2Tfb, cb])
                mm(ctx, ps[:, 0:NE], pb, cst[0:1, 128:256], rb[0:1, :], False, True, [cb])
                sm, smb = small.next()
                lg = sm[:, 0:8]; m1 = sm[:, 8:9]; m2 = sm[:, 9:10]; k1 = sm[:, 10:18]; l2 = sm[:, 18:26]
                w1 = sm[:, 26:27]; w2 = sm[:, 27:28]
                CP(ctx, "dve", lg, ps[:, 0:NE], [pb], [smb])
                ctx.op("dve", lambda e, o=m1, i=lg: e.reduce_max(o, i, AX.X), reads=[smb], writes=[smb])
                TS(ctx, "dve", k1, lg, m1, None, ALU.is_equal, None, [smb], [smb])
                STT(ctx, "dve", l2, k1, -1e30, lg, ALU.mult, ALU.add, [smb], [smb])
                ctx.op("dve", lambda e, o=m2, i=l2: e.reduce_max(o, i, AX.X), reads=[smb], writes=[smb])
                TS(ctx, "dve", l2, l2, m2, None, ALU.is_equal, None, [smb], [smb])
                TT(ctx, "dve", w2, m2, m1, ALU.subtract, [smb], [smb])
                A_(ctx, w2, w2, AF.Exp, [smb], [smb])
                TS(ctx, "dve", w1, w2, 1.0, None, ALU.add, None, [smb], [smb])
                ctx.op("dve", lambda e, o=w1, i=w1: e.reciprocal(o, i), reads=[smb], writes=[smb])
                TT(ctx, "dve", w2, w2, w1, ALU.mult, [smb], [smb])
                TS(ctx, "dve", k1, k1, w1, None, ALU.mult, None, [smb], [smb])
                STT(ctx, "dve", comb[:, j, :], l2, w2, k1, ALU.mult, ALU.add, [smb], [combb[j]])

        for e_ in range(NE if moe else 1):
            wgu_e = wgu_d[e_] if moe else wgu_d
            wd_e = wd_d[e_] if moe else wd_d
            for fp in range(FC // 2):
                w, wb = wr.load([(wgu_e[:, fp * 256:(fp + 1) * 256], 0), (wgu_e[:, FF + fp * 256:FF + (fp + 1) * 256], 256)],
                                8, 512)
                for f2 in range(2):
                    fc = fp * 2 + f2
                    psg, pgb = psr.next()
                    for k in range(8):
                        mm(ctx, psg[:, :], pgb, w[:, k, f2 * 128:(f2 + 1) * 128], aT[:, k, :], k == 0, k == 7, [wb, aTb])
                    psu, pub = psr.next()
                    for k in range(8):
                        mm(ctx, psu[:, :], pub, w[:, k, 256 + f2 * 128:256 + (f2 + 1) * 128], aT[:, k, :], k == 0, k == 7,
                           [wb, aTb])
                    sg, sgb = tmpr.next()
                    A_(ctx, sg[:, :], psg[:, :], AF.Silu, [pgb], [sgb])
                    TT(ctx, "dve", hff[:, fc, :], sg[:, :], psu[:, :], ALU.mult, [sgb, pub], [hfb])
            parts = []
            k0 = 0
            while k0 < FC:
                parts.append((k0, min(8, FC - k0)))
                k0 += 8
            if not moe:
                zs = [(zbuf[:, j, :], zbb[j]) for j in range(4)]
            for nb in range(2):
                pss = [psr.next() for _ in range(4)]
                for pi, (k0, kn) in enumerate(parts):
                    w, wb = wr.load([(wd_e[k0 * 128:(k0 + kn) * 128, nb * 512:(nb + 1) * 512], 0)], kn, 512)
                    for j in range(4):
                        for k in range(kn):
                            mm(ctx, pss[j][0][:, :], pss[j][1], hff[:, k0 + k, j * 128:(j + 1) * 128], w[:, k, :],
                               k0 + k == 0, k0 + k == FC - 1, [hfb, wb])
                for j in range(4):
                    ps, pb = pss[j]
                    sl = slice(nb * 512, (nb + 1) * 512)
                    if not moe:
                        z, zb = zs[j]
                        STT(ctx, "dve", z[:, sl], x2[:, j, sl], ALPHA, ps[:, :], ALU.mult, ALU.add, [x2b[j], pb], [zb])
                    elif e_ == 0:
                        TS(ctx, "dve", yacc[:, j, sl], ps[:, :], comb[:, j, 0:1], None, ALU.mult, None,
                           [pb, combb[j]], [yb[j]])
                    else:
                        STT(ctx, "dve", yacc[:, j, sl], ps[:, :], comb[:, j, e_:e_ + 1], yacc[:, j, sl], ALU.mult, ALU.add,
                            [pb, combb[j], yb[j]], [yb[j]])
        if moe:
            zs = [(zbuf[:, j, :], zbb[j]) for j in range(4)]
            for j in range(4):
                z, zb = zs[j]
                STT(ctx, "dve", z[:, :], x2[:, j, :], ALPHA, yacc[:, j, :], ALU.mult, ALU.add, [x2b[j], yb[j]], [zb])
        for j in range(4):
            z, zb = zs[j]
            o, ob = outr.next()
            layer_norm(ctx, z[:, :], zb, o[:, :], ob, vecs[:, 6, :], vecs[:, 7, :], cb, small, 0)
            ctx.dma("sp", out_d[t0 + j * 128:t0 + (j + 1) * 128, :], o[:, :], reads=[ob])
    ctx.wait_all_dma("sp")
    ctx.emit()
    ctx.close()
    return nc, ctx


FGROUPS = ([("mq0", 64), ("mq1", 64), ("mk0", 64), ("mk1", 64), ("mo0", 128), ("mo1", 128),
            ("fq0", 128), ("fq1", 128), ("fk0", 128), ("fk1", 128)]
           + [("rr%d" % h, 64) for h in range(4)] + [("rk%d" % h, 64) for h in range(4)]
           + [("rv%d" % h, 64) for h in range(4)] + [("lw", 64), ("la", 64), ("lg", 128), ("lv", 32)])
FOFF = {}
_o = 0
for _n, _w in FGROUPS:
    FOFF[_n] = (_o, _w, len(FOFF))
    _o += _w
NF = _o
NTM = 520
PVN = (["conv%d_%s" % (j, g) for g in ("mq0", "mq1", "mk0", "mk1") for j in range(4)]
       + ["mnorm0", "mnorm1"]
       + ["%s_%d" % (n, h) for n in ("mu_r", "mu_k", "mu_v", "wbias", "abias", "vbias", "kkw", "ka", "rk", "lng", "lnb")
          for h in range(4)]
       + ["mu_lw", "mu_la", "mu_lg", "mu_lv"])
PVI = {n: i for i, n in enumerate(PVN)}


def build_l1(T=8192, vres=False, rw_level=2):
    NB = T // 512
    NQ = T // 128
    nc = bass.Bass("TRN2", target_bir_lowering=False)
    ctx = Ctx(nc)

    def din(name, shape):
        return nc.dram_tensor(name, list(shape), F32, kind="ExternalInput").ap()

    xT_d = din("xT", [D, T])
    wf_d = din("wf", [D, NF])
    wt_d = din("wt", [D, NTM])
    bf_d = din("bfm", [128, len(FGROUPS)])
    bt_d = din("btm", [1, NTM])
    pv_d = din("pv", [128, len(PVN)])
    cst_d = din("cst", [128, 1280])
    lora_d = din("lora", [128, 4, 256])
    if vres:
        vf_d = din("vfirst", [256, T])
    hout_d = nc.dram_tensor("hout", [768, T], F32, kind="ExternalOutput").ap()
    vown_d = nc.dram_tensor("vown", [256, T], F32, kind="ExternalOutput").ap()

    ctx.alloc_sems()
    S = ctx.sbuf
    cb = Buf("const")
    cst = S("cst_s", [128, 1280], F32)
    cstb = S("cst_b", [128, 1024], BF16)
    ctx.dma("sp", cst[:, :], cst_d, writes=[cb])
    ctx.dma("pool", cstb[:, :], cst_d[:, 0:1024], writes=[cb])
    mSU, mSL = cst[:, 1024:1152], cst[:, 1152:1280]
    ident_f, ones_f, mLT, utri, sel127, sel63, mneg, bdiag = [cst[:, i * 128:(i + 1) * 128] for i in range(8)]
    ident_b, ones_b = cstb[:, 0:128], cstb[:, 128:256]
    mneg_b = cstb[:, 768:896]
    wt = S("wt_s", [128, 8, NTM], BF16)
    wfr = WRing(ctx, 5, name="wfr", slot=1024)
    ctx.dma("pool", wt[:, :, :], wt_d.rearrange("(k p) n -> p k n", p=128), writes=[cb])
    bfm = S("bfm_s", [128, len(FGROUPS)], F32)
    btm = S("btm_s", [1, NTM], F32)
    pv = S("pv_s", [128, len(PVN)], F32)
    lora = S("lora_s", [128, 4, 256], F32)
    ctx.dma("sp", bfm[:, :], bf_d, writes=[cb])
    ctx.dma("sp", btm[:, :], bt_d, writes=[cb])
    ctx.dma("sp", pv[:, :], pv_d, writes=[cb])
    ctx.dma("sp", lora[:, :, :], lora_d, writes=[cb])

    def PV(name, w=128):
        i = PVI[name]
        return pv[0:w, i:i + 1]

    psr = Ring(ctx, "ps", [128, 512], F32, 5, psum=True)
    foxacc = (ctx.psum("pa_fox", [128, 512], F32), Buf("pa_fox", psum=True))
    mlacc = (ctx.psum("pa_ml", [128, 512], F32), Buf("pa_ml", psum=True))
    rwacc = (ctx.psum("pa_rw", [128, 512], F32), Buf("pa_rw", psum=True))
    xTr = Ring(ctx, "xTb", [128, 8, 512], BF16, 1)
    small = Ring(ctx, "small", [128, 64], F32, 4)
    smallR = Ring(ctx, "smallR", [128, 64], F32, 4)
    smallF = Ring(ctx, "smallF", [128, 64], F32, 4)

    fK = S("fK", [128, 2, T], BF16); fKb = Buf("fK")
    fV = S("fV", [128, NQ, 4 * 65], BF16); fVb = Buf("fV")
    fG = S("fG", [128, NQ, 4], F32); fGb = Buf("fG")
    ctx.op("pool", lambda e: e.memset(fV[:, :, :], 1.0), writes=[fVb])
    fQr = Ring(ctx, "fQ", [128, 2, 512], BF16, 2)
    PTr = Ring(ctx, "PT", [128, 512], BF16, 3)
    fmisc = Ring(ctx, "fmisc", [128, 512], F32, 2)
    fnum = Ring(ctx, "fnum", [64, 512], F32, 1)
    neg8 = S("neg8", [1, 128], F32)
    TS(ctx, "dve", neg8[0:1, :], ones_f[0:1, 0:128], -8.0, None, ALU.mult, None, [cb], [cb])
    fbias = Ring(ctx, "fbias", [128, NQ], F32, 3)
    outT = Ring(ctx, "outT", [128, 256], F32, 2)

    LN8 = float(np.log(0.125))
    mU = [S("mU%d" % i, [64, 515], F32) for i in range(4)]; mUb = [Buf("mU%d" % i) for i in range(4)]
    for i in range(4):
        ctx.op("pool", lambda e, i=i: e.memset(mU[i][:, 0:3], 0.0), writes=[mUb[i]])
    mQK = [S("mQK%d" % i, [64, 512], F32) for i in range(4)]; mQKb = [Buf("mQK%d" % i) for i in range(4)]
    mVa = S("mVa", [128, 4, 2, 129], F32); mVab = [Buf("mVa%d" % j) for j in range(4)]
    ctx.op("pool", lambda e: e.memset(mVa[:, :, :, :].rearrange("p a b c -> p (a b c)"), 1.0), writes=mVab)
    mC = [S("mC%d" % i, [64, 129], F32) for i in range(2)]; mCb = [Buf("mC%d" % i) for i in range(2)]
    for i in range(2):
        ctx.op("pool", lambda e, i=i: e.memset(mC[i][:, :], 0.0), writes=[mCb[i]])
    mSig = [S("mSig%d" % i, [128, 512], F32) for i in range(2)]; mSigb = [Buf("mSig%d" % i) for i in range(2)]
    mWT = Ring(ctx, "mWT", [128, 128], F32, 2)
    mKg = Ring(ctx, "mKg", [128, 64], F32, 2)
    mH = Ring(ctx, "mH", [128, 128], F32, 2)
    mSc = Ring(ctx, "mSc", [128, 32], F32, 4)

    RG = [n for n, _ in FGROUPS if n[0] == "r" or n in ("lw", "la", "lg", "lv")]
    rHalo = S("rHalo", [128, len(RG)], F32); rHb = Buf("rHalo")
    ctx.op("pool", lambda e: e.memset(rHalo[:, :], 0.0), writes=[rHb])
    rP = Ring(ctx, "rP", [128, 513], F32, 3)
    rLo = [S("rLo%d" % i, [128, 512], F32) for i in range(4)]; rLob = [Buf("rLo%d" % i) for i in range(4)]
    NRX = 10
    rX = [[S("rX%d_%d" % (s_, i), [64, 512], F32) for i in range(NRX)] for s_ in range(1)]
    rXb = [[Buf("rX%d_%d" % (s_, i)) for i in range(NRX)] for s_ in range(1)]
    rM = [S("rM%d" % i, [64, 64], F32) for i in range(4)]; rMb = [Buf("rM%d" % i) for i in range(4)]
    for i in range(4):
        ctx.op("pool", lambda e, i=i: e.memset(rM[i][:, :], 0.0), writes=[rMb[i]])
    rE = Ring(ctx, "rE", [64, 5, 128], F32, 1)
    rF = Ring(ctx, "rF", [64, 6, 128], F32, 1)
    rTK = Ring(ctx, "rTK", [128, 256], F32, 2)
    rKX = Ring(ctx, "rKX", [128, 128], F32, 2)
    rA = Ring(ctx, "rA", [128, 4, 128], F32, 1)
    rN = Ring(ctx, "rN", [128, 128], F32, 6)
    rR = Ring(ctx, "rR", [128, 128], F32, 3)
    rWU = Ring(ctx, "rWU", [128, 128], F32, 2)
    rS64 = Ring(ctx, "rS64", [64, 256], F32, 3)
    rY = Ring(ctx, "rY", [128, 64], F32, 2)
    SIGC = 0.6065306597126334

    def fm_proj(xt, xtb, name, evac):
        off, w, gi = FOFF[name]
        wv, wvb = wfr.load([(wf_d[:, off:off + w], 0)], 8, w)
        ps, pb = psr.next()
        for k in range(8):
            mm(ctx, ps[0:w, :], pb, wv[:, k, :], xt[:, k, :], k == 0, k == 7, [wvb, xtb])
        evac(ps[0:w, :], pb, bfm[0:w, gi:gi + 1])

    for blk in range(NB):
        t0 = blk * 512
        xt, xtb = xTr.next()
        ctx.dma("pool", xt[:, :, :], xT_d.rearrange("(k p) t -> p k t", p=128)[:, :, t0:t0 + 512], writes=[xtb])

        tmv = []
        for j in range(4):
            ps, pb = psr.next()
            for k in range(8):
                mm(ctx, ps[:, :], pb, xt[:, k, j * 128:(j + 1) * 128], wt[:, k, 0:512], k == 0, False, [xtb, cb])
            mm(ctx, ps[:, :], pb, ones_f[0:1, 0:128], btm[0:1, 0:512], False, True, [cb])
            ps2, pb2 = psr.next()
            for k in range(8):
                mm(ctx, ps2[:, 0:8], pb2, xt[:, k, j * 128:(j + 1) * 128], wt[:, k, 512:520], k == 0, False, [xtb, cb])
            mm(ctx, ps2[:, 0:8], pb2, ones_f[0:1, 0:128], btm[0:1, 512:520], False, True, [cb])
            qb = blk * 4 + j
            CP(ctx, "act", fV[:, qb, :].rearrange("p (h c) -> p h c", c=65)[:, :, 0:64],
               ps[:, 256:512].rearrange("p (h c) -> p h c", c=64), [pb], [fVb])
            sm, smb = small.next()
            A_(ctx, sm[:, 0:8], ps2[:, 0:8], AF.Exp, [pb2], [smb], scale=-1.0)
            A_(ctx, sm[:, 8:16], sm[:, 0:8], AF.Ln, [smb], [smb], bias=1.0)
            psg, pgb = psr.next()
            mm(ctx, psg[:, 0:4], pgb, utri, sm[:, 12:16], True, qb == 0, [cb, smb])
            if qb > 0:
                mm(ctx, psg[:, 0:4], pgb, sel127, fG[:, qb - 1, :], False, True, [cb, fGb])
            CP(ctx, "dve", fG[:, qb, :], psg[:, 0:4], [pgb], [fGb])
            CP(ctx, "dve", mVa[:, j, :, 0:128], ps[:, 0:256].rearrange("p (h c) -> p h c", c=128), [pb], [mVab[j]])
            sc, scb = mSc.next()
            psm, pmb = psr.next()
            mm(ctx, psm[:, 0:2], pmb, utri, sm[:, 10:12], True, True, [cb, smb])
            mm(ctx, psm[:, 2:4], pmb, ones_f, sm[:, 10:12], True, True, [cb, smb])
            CP(ctx, "dve", sc[:, 0:4], psm[:, 0:4], [pmb], [scb])
            TT(ctx, "dve", sc[:, 12:14], ps2[:, 0:2], sc[:, 0:2], ALU.add, [pb2, scb], [scb])
            A_(ctx, sc[:, 4:6], sc[:, 12:14], AF.Exp, [scb], [scb], bias=LN8)
            A_(ctx, sc[:, 6:8], sc[:, 0:2], AF.Exp, [scb], [scb], scale=-1.0)
            TT(ctx, "dve", sc[:, 12:14], sc[:, 12:14], sc[:, 2:4], ALU.subtract, [scb], [scb])
            A_(ctx, sc[:, 8:10], sc[:, 12:14], AF.Exp, [scb], [scb], bias=LN8)
            A_(ctx, sc[:, 10:12], sc[:, 2:4], AF.Exp, [scb], [scb], scale=-1.0)
            tmv.append((sc, scb))

        for gi_, gname in enumerate(("mq0", "mq1", "mk0", "mk1")):
            U, Ub = mU[gi_], mUb[gi_]
            fm_proj(xt, xtb, gname, lambda ps, pb, bias, U=U, Ub=Ub: A_(ctx, U[:, 3:515], ps, AF.Identity, [pb, cb], [Ub], bias=bias))
            q_, qb_ = mQK[gi_], mQKb[gi_]
            TS(ctx, "dve", q_[:, :], U[:, 0:512], PV("conv0_" + gname, 64), None, ALU.mult, None, [Ub, cb], [qb_])
            for jj in range(1, 4):
                STT(ctx, "dve", q_[:, :], U[:, jj:jj + 512], PV("conv%d_%s" % (jj, gname), 64), q_[:, :], ALU.mult, ALU.add,
                    [Ub, cb, qb_], [qb_])
            A_(ctx, q_[:, :], q_[:, :], AF.Silu, [qb_], [qb_])
            CP(ctx, "pool", U[:, 0:3], U[:, 512:515], [Ub], [Ub])
        for i in range(2):
            fm_proj(xt, xtb, "mo%d" % i, lambda ps, pb, bias, i=i: A_(ctx, mSig[i][:, :], ps, AF.Sigmoid, [pb, cb], [mSigb[i]], bias=bias))
        def gen_mlstm(tmv=tmv, t0=t0):
          for j in range(4):
            sc, scb = tmv[j]
            cs = slice(j * 128, (j + 1) * 128)
            for i in range(2):
                qT, qTb_, kT, kTb_ = mQK[i], mQKb[i], mQK[2 + i], mQKb[2 + i]
                psG, pGb = psr.next()
                mm(ctx, psG[:, 0:128], pGb, kT[:, cs], qT[:, cs], True, True, [kTb_, qTb_])
                wt_, wtb = mWT.next()
                STT(ctx, "dve", wt_[:, :], psG[:, 0:128], sc[:, 4 + i:5 + i], mLT, ALU.mult, ALU.mult, [pGb, scb, cb], [wtb])
                yield
                psN, pNb = mlacc
                mm(ctx, psN[:, 0:129], pNb, wt_[:, :], mVa[:, j, i, :], True, False, [wtb, mVab[j]])
                mm(ctx, psN[:, 0:129], pNb, qT[:, cs], mC[i][:, :], False, True, [qTb_, mCb[i]])
                psK, pKb = psr.next()
                mm(ctx, psK[:, 0:64], pKb, kT[:, cs], ident_f[0:64, 0:64], True, True, [kTb_, cb])
                kg, kgb = mKg.next()
                TS(ctx, "dve", kg[:, :], psK[:, 0:64], sc[:, 8 + i:9 + i], None, ALU.mult, None, [pKb, scb], [kgb])
                yield
                psC, pCb = psr.next()
                mm(ctx, psC[0:64, 0:129], pCb, kg[:, :], mVa[:, j, i, :], True, True, [kgb, mVab[j]])
                STT(ctx, "dve", mC[i][:, :], mC[i][:, :], sc[0:64, 10 + i:11 + i], psC[0:64, 0:129], ALU.mult, ALU.add,
                    [mCb[i], scb, pCb], [mCb[i]])
                yield
                s2, s2b = small.next()
                TT(ctx, "dve", s2[:, 0:1], psN[:, 128:129], sc[:, 6 + i:7 + i], ALU.mult, [pNb, scb], [s2b])
                A_(ctx, s2[:, 0:1], s2[:, 0:1], AF.Abs, [s2b], [s2b])
                TS(ctx, "dve", s2[:, 0:1], s2[:, 0:1], 1.0, None, ALU.max, None, [s2b], [s2b])
                ctx.op("dve", lambda e, o=s2[:, 1:2], i_=s2[:, 0:1]: e.reciprocal(o, i_), reads=[s2b], writes=[s2b])
                TT(ctx, "dve", s2[:, 1:2], s2[:, 1:2], sc[:, 6 + i:7 + i], ALU.mult, [s2b, scb], [s2b])
                hh_, hhb = mH.next()
                TS(ctx, "dve", hh_[:, :], psN[:, 0:128], s2[:, 1:2], None, ALU.mult, None, [pNb, s2b], [hhb])
                ctx.op("dve", lambda e, o=s2[:, 8:14], i_=hh_[:, :]: e.bn_stats(o, i_), reads=[hhb], writes=[s2b])
                ctx.op("dve", lambda e, o=s2[:, 16:18], i_=s2[:, 8:14]: e.bn_aggr(o, i_), reads=[s2b], writes=[s2b])
                A_(ctx, s2[:, 18:19], s2[:, 17:18], AF.Sqrt, [s2b], [s2b], bias=1e-6)
                ctx.op("dve", lambda e, o=s2[:, 18:19]: e.reciprocal(o, o), reads=[s2b], writes=[s2b])
                TS(ctx, "dve", hh_[:, :], hh_[:, :], s2[:, 16:17], s2[:, 18:19], ALU.subtract, ALU.mult, [hhb, s2b], [hhb])
                psT, pTb = psr.next()
                mm(ctx, psT[:, 0:128], pTb, hh_[:, :], ident_f, True, True, [hhb, cb])
                ot, otb = outT.next()
                STT(ctx, "dve", ot[:, 0:128], psT[:, 0:128], PV("mnorm%d" % i), mSig[i][:, cs], ALU.mult, ALU.mult,
                    [pTb, cb, mSigb[i]], [otb])
                ctx.dma("sp", hout_d[i * 128:(i + 1) * 128, t0 + j * 128:t0 + (j + 1) * 128], ot[:, 0:128], reads=[otb])
                yield

        def lerp_group(name, dst, dstb, post=None):
            off, w, gi = FOFF[name]
            hi = RG.index(name)
            P, Pb = rP.next()
            CP(ctx, "pool", P[0:w, 0:1], rHalo[0:w, hi:hi + 1], [rHb], [Pb])
            fm_proj(xt, xtb, name, lambda ps, pb, bias: A_(ctx, P[0:w, 1:513], ps, AF.Identity, [pb, cb], [Pb], bias=bias))
            CP(ctx, "pool", rHalo[0:w, hi:hi + 1], P[0:w, 512:513], [Pb], [rHb])
            Dd, Ddb = rP.next()
            TT(ctx, "pool", Dd[0:w, 0:512], P[0:w, 0:512], P[0:w, 1:513], ALU.subtract, [Pb], [Ddb])
            muname = {"lw": "mu_lw", "la": "mu_la", "lg": "mu_lg", "lv": "mu_lv"}.get(name) or "mu_%s_%s" % (name[1], name[2])
            STT(ctx, "dve", dst, Dd[0:w, 0:512], PV(muname, w), P[0:w, 1:513], ALU.mult, ALU.add, [Pb, Ddb, cb], [dstb])
            if post is not None:
                A_(ctx, dst, dst, post, [dstb], [dstb])

        def gen_rwkv(t0=t0, lerp_group=lerp_group):
          lerp_group("lw", rLo[0][0:64, :], rLob[0], AF.Tanh)
          lerp_group("la", rLo[1][0:64, :], rLob[1])
          yield
          lerp_group("lg", rLo[2][0:128, :], rLob[2], AF.Sigmoid)
          if vres:
            lerp_group("lv", rLo[3][0:32, :], rLob[3])
          yield
          for i in range(4 if rw_level >= 1 else 0):
              X, Xb = rX[0], rXb[0]
              r_, k_, v_, nlw, a_, g_, kk, k2, bb, t1 = [X[q][:, :] for q in range(NRX)]
              r_b, k_b, v_b, nlwb, a_b, g_b, kkb, k2b, bbb, t1b = Xb
              bv, bvb = k_, k_b
              t2, t2b = kk, kkb
              lerp_group("rr%d" % i, r_, r_b)
              yield
              lerp_group("rk%d" % i, k_, k_b)
              yield
              lerp_group("rv%d" % i, v_, v_b)
              yield
              ctx.dma("sp", vown_d[i * 64:(i + 1) * 64, t0:t0 + 512], v_, reads=[v_b])
              ic = slice(i * 64, (i + 1) * 64)
              ps, pb = psr.next()
              mm(ctx, ps[0:64, :], pb, lora[0:64, 0, ic], rLo[0][0:64, :], True, True, [cb, rLob[0]])
              A_(ctx, nlw, ps[0:64, :], AF.Sigmoid, [pb, cb], [nlwb], bias=PV("wbias_%d" % i, 64))
              TS(ctx, "pool", nlw, nlw, SIGC, None, ALU.mult, None, [nlwb], [nlwb])
              ps, pb = psr.next()
              mm(ctx, ps[0:64, :], pb, lora[0:64, 1, ic], rLo[1][0:64, :], True, True, [cb, rLob[1]])
              A_(ctx, a_, ps[0:64, :], AF.Sigmoid, [pb, cb], [a_b], bias=PV("abias_%d" % i, 64))
              ps, pb = psr.next()
              mm(ctx, ps[0:64, :], pb, lora[0:128, 2, ic], rLo[2][0:128, :], True, True, [cb, rLob[2]])
              CP(ctx, "act", g_, ps[0:64, :], [pb], [g_b])
              yield
              if vres:
                  ps, pb = psr.next()
                  mm(ctx, ps[0:64, :], pb, lora[0:32, 3, ic], rLo[3][0:32, :], True, True, [cb, rLob[3]])
                  A_(ctx, t1, ps[0:64, :], AF.Sigmoid, [pb, cb], [t1b], bias=PV("vbias_%d" % i, 64))
                  ctx.dma("sp", t2, vf_d[i * 64:(i + 1) * 64, t0:t0 + 512], writes=[t2b])
                  TT(ctx, "pool", t2, t2, v_, ALU.subtract, [t2b, v_b], [t2b])
                  TT(ctx, "pool", t2, t2, t1, ALU.mult, [t2b, t1b], [t2b])
                  TT(ctx, "pool", v_, v_, t2, ALU.add, [v_b, t2b], [v_b])
              TS(ctx, "pool", kk, k_, PV("kkw_%d" % i, 64), None, ALU.mult, None, [k_b, cb], [kkb])
              TT(ctx, "pool", t1, kk, kk, ALU.mult, [kkb], [t1b])
              ps, pb = psr.next()
              mm(ctx, ps[0:64, :], pb, ones_f[0:64, 0:64], t1, True, True, [cb, t1b])
              A_(ctx, t1, ps[0:64, :], AF.Sqrt, [pb], [t1b])
              TS(ctx, "dve", t1, t1, 1e-12, None, ALU.max, None, [t1b], [t1b])
              ctx.op("dve", lambda e, o=t1: e.reciprocal(o, o), reads=[t1b], writes=[t1b])
              TT(ctx, "pool", kk, kk, t1, ALU.mult, [kkb, t1b], [kkb])
              yield
              TS(ctx, "dve", t1, a_, 1.0, PV("ka_%d" % i, 64), ALU.subtract, ALU.mult, [a_b, cb], [t1b])
              STT(ctx, "dve", k2, t1, 1.0, k_, ALU.add, ALU.mult, [t1b, k_b], [k2b])
              TT(ctx, "pool", bb, kk, a_, ALU.mult, [kkb, a_b], [bbb])
              STT(ctx, "dve", t1, r_, PV("rk_%d" % i, 64), k2, ALU.mult, ALU.mult, [r_b, cb, k2b], [t1b])
              ps, pb = psr.next()
              mm(ctx, ps[0:64, :], pb, ones_f[0:64, 0:64], t1, True, True, [cb, t1b])
              TT(ctx, "dve", bv, ps[0:64, :], v_, ALU.mult, [pb, v_b], [bvb])
              yield
              M, Mb = rM[i], rMb[i]
              for j in range(4 if rw_level >= 2 else 0):
                  cs = slice(j * 128, (j + 1) * 128)
                  E, Eb = rE.next()
                  ncl, eg, egm, ei, egl = [E[:, q, :] for q in range(5)]
                  ctx.op("dve", lambda e, o=ncl, d1=nlw[:, cs]: e.tensor_tensor_scan(o, ones_f[0:64, 0:128], d1, 0.0, ALU.mult, ALU.add),
                         reads=[cb, nlwb], writes=[Eb])
                  sm, smb = smallR.next()
                  TS(ctx, "pool", sm[0:64, 0:1], ncl[:, 127:128], -1.0, None, ALU.mult, None, [Eb], [smb])
                  TT(ctx, "pool", egm, nlw[:, cs], ncl, ALU.subtract, [nlwb, Eb], [Eb])
                  A_(ctx, eg, ncl, AF.Exp, [Eb], [Eb], scale=-1.0)
                  A_(ctx, egm, egm, AF.Exp, [Eb], [Eb])
                  A_(ctx, ei, ncl, AF.Exp, [Eb], [Eb])
                  A_(ctx, egl, ncl, AF.Exp, [Eb, smb], [Eb], bias=sm[0:64, 0:1])
                  Fm, Fb = rF.next()
                  KR = Fm[:, 0:2, :]
                  BiT, KiT, KtT, NBtT = [Fm[:, q, :] for q in range(2, 6)]
                  TT(ctx, "pool", Fm[:, 0, :], kk[:, cs], egm, ALU.mult, [kkb, Eb], [Fb])
                  TT(ctx, "pool", Fm[:, 1, :], r_[:, cs], eg, ALU.mult, [r_b, Eb], [Fb])
                  TT(ctx, "pool", BiT, bb[:, cs], ei, ALU.mult, [bbb, Eb], [Fb])
                  TT(ctx, "pool", KiT, k2[:, cs], ei, ALU.mult, [k2b, Eb], [Fb])
                  TT(ctx, "pool", KtT, k2[:, cs], egl, ALU.mult, [k2b, Eb], [Fb])
                  STT(ctx, "dve", NBtT, bb[:, cs], -1.0, egl, ALU.mult, ALU.mult, [bbb, Eb], [Fb])
                  yield
                  if rw_level == 3:
                      continue
                  psT, pTb = psr.next()
                  SUB = 9
                  for q, src, srcb in ((0, Fm[:, 0, :], Fb), (1, Fm[:, 1, :], Fb), (2, v_[:, cs], v_b), (3, KtT, Fb), (4, NBtT, Fb)):
                      if SUB == 1 and q >= 2:
                          continue
                      mm(ctx, psT[:, q * 64:(q + 1) * 64], pTb, src, ident_f[0:64, 0:64], True, True, [srcb, cb])
                  TK, TKb = rTK.next()
                  KX, KXb = rKX.next()
                  if SUB <= 2:
                      continue
                  CP(ctx, "act", TK[:, :], psT[:, 64:320], [pTb], [TKb])
                  if SUB <= 3:
                      continue
                  CP(ctx, "dve", KX[:, 0:64], psT[:, 0:64], [pTb], [KXb])
                  yield
                  Rg_tok, V_tok, Kt_tok, NBt_tok = [TK[:, q * 64:(q + 1) * 64] for q in range(4)]
                  if rw_level == 4:
                      continue
                  Am, Ab = rA.next()
                  NT_, NArbT, AkkT, ArkT = [Am[:, q, :] for q in range(4)]
                  KRf = KR.rearrange("p a t -> p (a t)")
                  ps1, p1b = psr.next()
                  mm(ctx, ps1[:, 0:256], p1b, BiT, KRf, True, True, [Fb])
                  STT(ctx, "dve", NT_, ps1[:, 0:128], -1.0, mSU, ALU.mult, ALU.mult, [p1b, cb], [Ab])
                  STT(ctx, "dve", NArbT, ps1[:, 128:256], -1.0, mLT, ALU.mult, ALU.mult, [p1b, cb], [Ab])
                  ps2_, p2b = psr.next()
                  mm(ctx, ps2_[:, 0:256], p2b, KiT, KRf, True, True, [Fb])
                  TT(ctx, "dve", AkkT, ps2_[:, 0:128], mSU, ALU.mult, [p2b, cb], [Ab])
                  TT(ctx, "dve", ArkT, ps2_[:, 128:256], mLT, ALU.mult, [p2b, cb], [Ab])
                  ps3, p3b = psr.next()
                  mm(ctx, ps3[:, 0:128], p3b, Fm[:, 0, :], BiT, True, True, [Fb])
                  A_cur, A_curb = rN.next()
                  STT(ctx, "dve", A_cur[:, :], ps3[:, 0:128], -1.0, mSL, ALU.mult, ALU.mult, [p3b, cb], [A_curb])
                  yield
                  if rw_level == 5:
                      continue
                  AT_cur, AT_curb = NT_, Ab
                  RT, RTb = rR.next()
                  TT(ctx, "pool", RT[:, :], NT_, ident_f, ALU.add, [Ab, cb], [RTb])
                  for lvl in range(6):
                      psa, pab = psr.next()
                      mm(ctx, psa[:, 0:128], pab, AT_cur, A_cur[:, :], True, True, [AT_curb, A_curb])
                      A2, A2b = rN.next()
                      CP(ctx, "act", A2[:, :], psa[:, 0:128], [pab], [A2b])
                      if lvl < 5:
                          psb_, pbb = psr.next()
                          mm(ctx, psb_[:, 0:128], pbb, A_cur[:, :], AT_cur, True, True, [AT_curb, A_curb])
                          A2T, A2Tb = rN.next()
                          CP(ctx, "act", A2T[:, :], psb_[:, 0:128], [pbb], [A2Tb])
                          yield
                      psc, pcb = psr.next()
                      mm(ctx, psc[:, 0:128], pcb, A2[:, :], RT[:, :], True, True, [A2b, RTb])
                      RT2, RT2b = rR.next()
                      TT(ctx, "dve", RT2[:, :], psc[:, 0:128], RT[:, :], ALU.add, [pcb, RTb], [RT2b])
                      yield
                      RT, RTb = RT2, RT2b
                      A_cur, A_curb = A2, A2b
                      if lvl < 5:
                          AT_cur, AT_curb = A2T[:, :], A2Tb
                  if rw_level == 6:
                      continue
                  ps4, p4b = psr.next()
                  mm(ctx, ps4[:, 0:64], p4b, AkkT, V_tok, True, True, [Ab, TKb])
                  CP(ctx, "act", KX[:, 64:128], ps4[:, 0:64], [p4b], [KXb])
                  yield
                  ps5, p5b = psr.next()
                  mm(ctx, ps5[:, 0:128], p5b, RT[:, :], KX[:, :], True, True, [RTb, KXb])
                  WU, WUb = rWU.next()
                  CP(ctx, "act", WU[:, :], ps5[:, 0:128], [p5b], [WUb])
                  yield
                  S64, S64b = rS64.next()
                  RyT, PTs = S64[:, 0:128], S64[:, 128:192]
                  ps6, p6b = psr.next()
                  mm(ctx, ps6[0:64, 0:128], p6b, WU[:, 0:64], NArbT, True, False, [WUb, Ab])
                  mm(ctx, ps6[0:64, 0:128], p6b, Rg_tok, ident_f, False, True, [TKb, cb])
                  CP(ctx, "act", RyT, ps6[0:64, 0:128], [p6b], [S64b])
                  ps7, p7b = psr.next()
                  mm(ctx, ps7[0:64, 0:64], p7b, WU[:, 0:64], NBt_tok, True, True, [WUb, TKb])
                  STT(ctx, "dve", PTs, ident_f[0:64, 0:64], eg[:, 127:128], ps7[0:64, 0:64], ALU.mult, ALU.add, [cb, Eb, p7b], [S64b])
                  yield
                  if rw_level == 7:
                      continue
                  ps9, p9b = rwacc
                  mm(ctx, ps9[:, 0:64], p9b, ArkT, V_tok, True, False, [Ab, TKb])
                  mm(ctx, ps9[:, 0:64], p9b, NArbT, WU[:, 64:128], False, False, [Ab, WUb])
                  mm(ctx, ps9[:, 0:64], p9b, RyT, M[:, :], False, True, [S64b, Mb])
                  ps8, p8b = rwacc
                  mm(ctx, ps8[0:64, 64:128], p8b, Kt_tok, V_tok, True, False, [TKb])
                  mm(ctx, ps8[0:64, 64:128], p8b, NBt_tok, WU[:, 64:128], False, False, [TKb, WUb])
                  mm(ctx, ps8[0:64, 64:128], p8b, PTs, M[:, :], False, True, [S64b, Mb])
                  CP(ctx, "act", M[:, :], ps8[0:64, 64:128], [p8b], [Mb])
                  yield
                  if rw_level == 8:
                      continue
                  s2, s2b = smallR.next()
                  yt, ytb = rY.next()
                  CP(ctx, "dve", yt[:, :], ps9[:, 0:64], [p9b], [ytb])
                  ctx.op("dve", lambda e, o=s2[:, 8:14], i_=yt[:, :]: e.bn_stats(o, i_), reads=[ytb], writes=[s2b])
                  ctx.op("dve", lambda e, o=s2[:, 16:18], i_=s2[:, 8:14]: e.bn_aggr(o, i_), reads=[s2b], writes=[s2b])
                  A_(ctx, s2[:, 18:19], s2[:, 17:18], AF.Sqrt, [s2b], [s2b], bias=64e-5)
                  ctx.op("dve", lambda e, o=s2[:, 18:19]: e.reciprocal(o, o), reads=[s2b], writes=[s2b])
                  TS(ctx, "dve", yt[:, :], yt[:, :], s2[:, 16:17], s2[:, 18:19], ALU.subtract, ALU.mult, [ytb, s2b], [ytb])
                  psy, pyb = psr.next()
                  mm(ctx, psy[0:64, 0:128], pyb, yt[:, :], ident_f, True, True, [ytb, cb])
                  ot, otb = outT.next()
                  TS(ctx, "dve", ot[0:64, 0:128], psy[0:64, 0:128], PV("lng_%d" % i, 64), PV("lnb_%d" % i, 64), ALU.mult, ALU.add,
                     [pyb, cb], [otb])
                  TT(ctx, "pool", ot[0:64, 0:128], ot[0:64, 0:128], bv[:, cs], ALU.add, [otb, bvb], [otb])
                  TT(ctx, "pool", ot[0:64, 0:128], ot[0:64, 0:128], g_[:, cs], ALU.mult, [otb, g_b], [otb])
                  ctx.dma("sp", hout_d[512 + i * 64:512 + (i + 1) * 64, t0 + j * 128:t0 + (j + 1) * 128], ot[0:64, 0:128], reads=[otb])
                  yield

        fq, fqb = fQr.next()
        for pr_ in range(2):
            fm_proj(xt, xtb, "fq%d" % pr_,
                    lambda ps, pb, bias, pr_=pr_: A_(ctx, fq[:, pr_, :], ps, AF.Identity, [pb, cb], [fqb], bias=bias, scale=1.0))
            fm_proj(xt, xtb, "fk%d" % pr_,
                    lambda ps, pb, bias, pr_=pr_: A_(ctx, fK[:, pr_, t0:t0 + 512], ps, AF.Identity, [pb, cb], [fKb], bias=bias))

        def gen_fox(blk=blk, fq=fq, fqb=fqb, t0=t0):
          qb0 = blk * 4
          nk = qb0 + 4
          pso, pob = foxacc
          for h in range(4):
              hs = slice((h % 2) * 64, (h % 2) * 64 + 64)
              psr_, prb = psr.next()
              mm(ctx, psr_[:, 0:1], prb, sel63, fG[:, qb0, h:h + 1], True, True, [cb, fGb])
              mm(ctx, psr_[0:1, 4:8], prb, sel63[:, 0:1], fG[:, qb0:qb0 + 4, h], True, True, [cb, fGb])
              sm, smb = smallF.next()
              CP(ctx, "dve", sm[:, 0:1], psr_[:, 0:1], [prb], [smb])
              CP(ctx, "dve", sm[0:1, 4:8], psr_[0:1, 4:8], [prb], [smb])
              fb, fbb = fbias.next()
              TS(ctx, "dve", fb[:, 0:nk], fG[:, 0:nk, h], sm[:, 0:1], None, ALU.subtract, None, [fGb, smb], [fbb])
              fm_, fmb = fmisc.next()
              rr = fm_[0:1, :]
              for j in range(4):
                  TS(ctx, "dve", rr[0:1, j * 128:(j + 1) * 128], ones_f[0:1, 0:128], sm[0:1, 4 + j:5 + j], sm[0:1, 4:5],
                     ALU.mult, ALU.subtract, [cb, smb], [fmb])
              for kb in range(nk):
                  c = kb - qb0
                  col0 = max(c, 0) * 128
                  n = 512 - col0
                  ps, pb = psr.next()
                  mm(ctx, ps[:, 0:n], pb, fK[hs, h // 2, kb * 128:(kb + 1) * 128], fq[hs, h // 2, col0:512], True, False,
                     [fKb, fqb])
                  if c >= 0:
                      mm(ctx, ps[:, 0:128], pb, ident_b, mneg_b, False, False, [cb])
                  mm(ctx, ps[:, 0:n], pb, neg8[0:1, 0:128], rr[0:1, col0:512], False, True, [cb, fmb])
                  pt, ptb = PTr.next()
                  A_(ctx, pt[:, 0:n], ps[:, 0:n], AF.Exp, [pb, fbb], [ptb], bias=fb[:, kb:kb + 1], scale=0.125)
                  mm(ctx, pso[0:65, col0:512], pob, fV[:, kb, h * 65:(h + 1) * 65], pt[:, 0:n], kb == 0, kb == nk - 1,
                     [ptb, fVb])
                  yield
              ctx.op("dve", lambda e, o=fm_[64:65, :], i=pso[64:65, :]: e.reciprocal(o, i), reads=[pob], writes=[fmb])
              psb, pbb = psr.next()
              mm(ctx, psb[0:64, :], pbb, ones_f[64:65, 0:64], fm_[64:65, :], True, True, [cb, fmb])
              nu, nub = fnum.next()
              CP(ctx, "act", nu[:, :], pso[0:64, :], [pob], [nub])
              TT(ctx, "dve", nu[:, :], nu[:, :], psb[0:64, :], ALU.mult, [nub, pbb], [nub])
              ctx.dma("sp", hout_d[256 + h * 64:256 + (h + 1) * 64, t0:t0 + 512], nu[:, :], reads=[nub])
              yield

        g_rw, g_fx, g_ml = gen_rwkv(), gen_fox(), gen_mlstm()
        n_rw = 4 * (4 * 19 + 6) + 2
        r_fx = (16 * blk + 20) / float(n_rw)
        r_ml = 34.0 / n_rw
        c_fx = c_ml = 0.0
        alive = {"fx": True, "ml": True}

        def adv(g, key):
            if alive[key]:
                try:
                    next(g)
                except StopIteration:
                    alive[key] = False

        for _ in g_rw:
            c_fx += r_fx
            c_ml += r_ml
            while c_fx >= 1.0 and alive["fx"]:
                adv(g_fx, "fx")
                c_fx -= 1.0
            while c_ml >= 1.0 and alive["ml"]:
                adv(g_ml, "ml")
                c_ml -= 1.0
        while alive["ml"]:
            adv(g_ml, "ml")
        while alive["fx"]:
            adv(g_fx, "fx")
    ctx.wait_all_dma("sp")
    ctx.emit()
    ctx.close()
    return nc, ctx


def _layout(vres):
    cols = [("m_qk", 512), ("m_v", 512), ("m_o", 512), ("m_i", 4), ("m_f", 4), ("f_q", 512), ("f_k", 512), ("f_v", 512),
            ("f_f", 8), ("gate", 3072), ("r_r", 512), ("r_k", 512), ("r_v", 512), ("r_w", 64), ("r_a", 64), ("r_g", 128)]
    if vres:
        cols.append(("r_vres", 32))
    lay, st = {}, 0
    for n, w in cols:
        lay[n] = st
        st += w
    return lay, st


def l1_consts():
    p = np.arange(128)
    ident = np.eye(128)
    ones = np.ones((128, 128))
    mLT = (p[:, None] <= p[None, :]).astype(np.float64)
    utri = mLT.copy()
    sel127 = np.zeros((128, 128)); sel127[127, :] = 1
    sel63 = np.zeros((128, 128)); sel63[63, :] = 1
    mneg = np.where(p[:, None] <= p[None, :], 0.0, -30000.0)
    bd = (p[:, None] // 64 == p[None, :] // 64).astype(np.float64)
    mSU = (p[:, None] < p[None, :]).astype(np.float64)
    mSL = (p[:, None] > p[None, :]).astype(np.float64)
    return np.concatenate([ident, ones, mLT, utri, sel127, sel63, mneg, bd, mSU, mSL], 1).astype(np.float32)


def pack_l1(inp, l, hh):
    vres = l > 0
    lay, ncol = _layout(vres)
    W = inp["w_in_%d" % l]
    Bv = inp["b_in_%d" % l]
    g = lambda n: inp["%s_%d" % (n, l)]
    mh = [2 * hh, 2 * hh + 1]
    fh = [4 * hh + i for i in range(4)]
    cols = {}
    for i, h in enumerate(mh):
        cols["mq%d" % i] = np.arange(lay["m_qk"] + h * 64, lay["m_qk"] + (h + 1) * 64)
        cols["mk%d" % i] = np.arange(lay["m_qk"] + 256 + h * 64, lay["m_qk"] + 256 + (h + 1) * 64)
        cols["mo%d" % i] = np.arange(lay["m_o"] + h * 128, lay["m_o"] + (h + 1) * 128)
    for pr in range(2):
        cols["fq%d" % pr] = np.arange(lay["f_q"] + fh[2 * pr] * 64, lay["f_q"] + (fh[2 * pr] + 2) * 64)
        cols["fk%d" % pr] = np.arange(lay["f_k"] + fh[2 * pr] * 64, lay["f_k"] + (fh[2 * pr] + 2) * 64)
    for i, h in enumerate(fh):
        cols["rr%d" % i] = np.arange(lay["r_r"] + h * 64, lay["r_r"] + (h + 1) * 64)
        cols["rk%d" % i] = np.arange(lay["r_k"] + h * 64, lay["r_k"] + (h + 1) * 64)
        cols["rv%d" % i] = np.arange(lay["r_v"] + h * 64, lay["r_v"] + (h + 1) * 64)
    cols["lw"] = np.arange(lay["r_w"], lay["r_w"] + 64)
    cols["la"] = np.arange(lay["r_a"], lay["r_a"] + 64)
    cols["lg"] = np.arange(lay["r_g"], lay["r_g"] + 128)
    cols["lv"] = np.arange(lay["r_vres"], lay["r_vres"] + 32) if vres else None
    wf = np.zeros((D, NF), np.float32)
    bfm = np.zeros((128, len(FGROUPS)), np.float32)
    for n, w in FGROUPS:
        off, _, gi = FOFF[n]
        if cols[n] is None:
            continue
        wf[:, off:off + w] = W[:, cols[n]]
        bfm[:w, gi] = Bv[cols[n]]
    tcols = np.concatenate([np.arange(lay["m_v"] + mh[0] * 128, lay["m_v"] + (mh[1] + 1) * 128),
                            np.arange(lay["f_v"] + fh[0] * 64, lay["f_v"] + (fh[3] + 1) * 64),
                            lay["m_i"] + np.array(mh), lay["m_f"] + np.array(mh), lay["f_f"] + np.array(fh)])
    wt = np.ascontiguousarray(W[:, tcols])
    btm = np.ascontiguousarray(Bv[tcols])[None, :]
    pv = np.zeros((128, len(PVN)), np.float32)
    r0 = lay["r_r"]
    mu = g("r_mu")
    mc = g("m_conv")
    for gname in ("mq0", "mq1", "mk0", "mk1"):
        for j in range(4):
            pv[:64, PVI["conv%d_%s" % (j, gname)]] = mc[j, cols[gname] - lay["m_qk"]]
    for i, h in enumerate(mh):
        pv[:128, PVI["mnorm%d" % i]] = g("m_norm")[h * 128:(h + 1) * 128]
    for i, h in enumerate(fh):
        sl = slice(h * 64, (h + 1) * 64)
        pv[:64, PVI["mu_r_%d" % i]] = mu[cols["rr%d" % i] - r0]
        pv[:64, PVI["mu_k_%d" % i]] = mu[cols["rk%d" % i] - r0]
        pv[:64, PVI["mu_v_%d" % i]] = mu[cols["rv%d" % i] - r0]
        pv[:64, PVI["wbias_%d" % i]] = g("r_wbias")[sl]
        pv[:64, PVI["abias_%d" % i]] = g("r_abias")[sl]
        if vres:
            pv[:64, PVI["vbias_%d" % i]] = g("r_vbias")[sl]
        pv[:64, PVI["kkw_%d" % i]] = g("r_kk")[sl]
        pv[:64, PVI["ka_%d" % i]] = g("r_ka")[sl]
        pv[:64, PVI["rk_%d" % i]] = g("r_rk")[sl]
        pv[:64, PVI["lng_%d" % i]] = g("r_ln_g")[sl]
        pv[:64, PVI["lnb_%d" % i]] = g("r_ln_b")[sl]
    pv[:64, PVI["mu_lw"]] = mu[cols["lw"] - r0]
    pv[:64, PVI["mu_la"]] = mu[cols["la"] - r0]
    pv[:128, PVI["mu_lg"]] = mu[cols["lg"] - r0]
    if vres:
        pv[:32, PVI["mu_lv"]] = mu[cols["lv"] - r0]
    lora = np.zeros((128, 4, 256), np.float32)
    csl = slice(fh[0] * 64, (fh[3] + 1) * 64)
    lora[:64, 0] = g("r_wB")[:, csl]
    lora[:64, 1] = g("r_aB")[:, csl]
    lora[:128, 2] = g("r_gB")[:, csl]
    if vres:
        lora[:32, 3] = g("r_vB")[:, csl]
    return dict(wf=wf, wt=wt, bfm=bfm, btm=btm, pv=pv, lora=lora, cst=l1_consts())


_PROGS = {}
SEQ = 8192
NBATCH = 4


def _prog(kind, **kw):
    key = (kind,) + tuple(sorted(kw.items()))
    if key not in _PROGS:
        if kind == "l1":
            _PROGS[key] = build_l1(T=SEQ, vres=kw["vres"])[0]
        else:
            _PROGS[key] = build_l2(kw["moe"], NT=SEQ // 2)[0]
    return _PROGS[key]


def run_l1(inp, l, cur):
    nc = _prog("l1", vres=l > 0)
    packs = [pack_l1(inp, l, hh) for hh in range(2)]
    xTs = [np.ascontiguousarray(cur[b].T) for b in range(NBATCH)]
    maps = []
    for c in range(NCORES):
        b, hh = c // 2, c % 2
        m = dict(packs[hh])
        m["xT"] = xTs[b]
        if l > 0:
            m["vfirst"] = inp["_vfirst"][c]
        maps.append(m)
    res = run_bass_kernel_spmd(nc, maps, core_ids=list(range(NCORES)))
    hT = []
    for b in range(NBATCH):
        o0, o1 = res.results[2 * b]["hout"], res.results[2 * b + 1]["hout"]
        hT.append(np.concatenate([o0[0:256], o1[0:256], o0[256:512], o1[256:512], o0[512:768], o1[512:768]], 0))
    vown = [np.ascontiguousarray(res.results[c]["vown"]) for c in range(NCORES)]
    return hT, vown


def run_l2(inp, l, cur, hT):
    moe = l % 2 == 1
    nc = _prog("l2", moe=moe)
    NT = SEQ // 2
    g = lambda n: inp["%s_%d" % (n, l)]
    lay, _ = _layout(l > 0)
    g0 = lay["gate"]
    W = g("w_in")
    shared = dict(
        cst=np.concatenate([np.eye(128), np.ones((128, 128))], 1).astype(np.float32),
        vecs=np.stack([inp["mem_ln_g"], inp["mem_ln_b"], g("ln1_g"), g("ln1_b"), g("ln2_g"), g("ln2_b"),
                       g("ln3_g"), g("ln3_b")]).astype(np.float32),
        wg=np.ascontiguousarray(W[:, g0:g0 + 3072]),
        bg=np.ascontiguousarray(g("b_in")[g0:g0 + 3072])[None, :],
        wup=np.concatenate([g("m_up"), g("f_up"), g("r_up")], 0),
        wout=g("w_out"), wq=g("x_wq"), wkv=g("x_wkv"), wo=g("x_wo"))
    if moe:
        shared.update(rw=g("ex_router"), rb=g("ex_router_b")[None, :], wgu=g("ex_wgu"), wd=g("ex_wd"))
    else:
        shared.update(wgu=g("ff_wgu"), wd=g("ff_wd"))
    maps = []
    for c in range(NCORES):
        b, half = c // 2, c % 2
        sl = slice(half * NT, (half + 1) * NT)
        m = dict(shared)
        m["x_tok"] = np.ascontiguousarray(cur[b, sl])
        m["xT"] = np.ascontiguousarray(cur[b, sl].T)
        m["hT"] = np.ascontiguousarray(hT[b][:, sl])
        m["mem"] = np.ascontiguousarray(inp["mem"][b])
        maps.append(m)
    res = run_bass_kernel_spmd(nc, maps, core_ids=list(range(NCORES)))
    out = np.empty((NBATCH, SEQ, D), np.float32)
    for c in range(NCORES):
        b, half = c // 2, c % 2
        out[b, half * NT:(half + 1) * NT] = res.results[c]["x3"]
    return out


_INPUT_NAMES = (
    "x", "mem", "mem_ln_g", "mem_ln_b", "w_in_0", "b_in_0", "m_conv_0", "m_norm_0", "m_up_0", "f_up_0",
    "r_mu_0", "r_wbias_0", "r_wB_0", "r_abias_0", "r_aB_0", "r_gB_0", "r_kk_0", "r_ka_0", "r_rk_0",
    "r_ln_g_0", "r_ln_b_0", "r_up_0", "w_out_0", "ln1_g_0", "ln1_b_0", "x_wq_0", "x_wkv_0", "x_wo_0",
    "ln2_g_0", "ln2_b_0", "ff_wgu_0", "ff_wd_0", "ln3_g_0", "ln3_b_0", "w_in_1", "b_in_1", "m_conv_1",
    "m_norm_1", "m_up_1", "f_up_1", "r_mu_1", "r_wbias_1", "r_wB_1", "r_abias_1", "r_aB_1", "r_vbias_1",
    "r_vB_1", "r_gB_1", "r_kk_1", "r_ka_1", "r_rk_1", "r_ln_g_1", "r_ln_b_1", "r_up_1", "w_out_1", "ln1_g_1",
    "ln1_b_1", "x_wq_1", "x_wkv_1", "x_wo_1", "ln2_g_1", "ln2_b_1", "ex_router_1", "ex_router_b_1",
    "ex_wgu_1", "ex_wd_1", "ln3_g_1", "ln3_b_1",
)


def kernel(**inputs):
    inp = {k: np.asarray(inputs[k], dtype=np.float32) for k in _INPUT_NAMES}
    cur = inp["x"]
    for l in range(2):
        hT, vown = run_l1(inp, l, cur)
        if l == 0:
            inp["_vfirst"] = vown
        cur = run_l2(inp, l, cur, hT)
    return cur
```
